# Optimizing a Trainium2 kernel written in Bass

```python
import jax
import jax.numpy as jnp
from jax import lax
import numpy as np

D_MODEL = 1024
BATCH = 8
SEQ = 2048
DEPTH = 4

GRID_W = 64
CTX_LEN = 256
QBLK = 128
ROPE_THETA = 10000.0
NORM_EPS = 1e-6
N_BRANCH = 4
BRANCH_W = D_MODEL // 4
SGU_CHUNK = 128
SGU_GROUPS = 4
MLA_HEADS = 4
MLA_Q_RANK = D_MODEL // 4
MLA_KV_RANK = D_MODEL // 8
MLA_NOPE = 64
MLA_ROPE = 32
MLA_V = BRANCH_W // MLA_HEADS
CONV_W = 3
GQA_HEADS = 4
GQA_KV_HEADS = 2
GQA_HEAD_DIM = BRANCH_W // GQA_HEADS
N_GROUPS = 4
EXPERTS_PER_GROUP = 8
N_EXPERTS = N_GROUPS * EXPERTS_PER_GROUP
TOP_K = 2
D_EXPERT = D_MODEL // 4
MOE_BLK = 128
SPLIT_SIZES = (BRANCH_W, BRANCH_W,
               MLA_Q_RANK, MLA_KV_RANK, MLA_ROPE,
               BRANCH_W, BRANCH_W, BRANCH_W,
               GQA_HEADS * GQA_HEAD_DIM, GQA_KV_HEADS * GQA_HEAD_DIM, GQA_KV_HEADS * GQA_HEAD_DIM)
D_IN = sum(SPLIT_SIZES)

kernel_name = 'hybrid_gated_mixers_hmoe_dit'


def _rmsnorm(x, gain=None):
    xf = x.astype(jnp.float32)
    y = xf * lax.rsqrt(jnp.mean(xf * xf, axis=-1, keepdims=True) + NORM_EPS)
    if gain is not None:
        y = y * gain.astype(jnp.float32)
    return y.astype(x.dtype)


def _split_cols(z, sizes):
    out, o = [], 0
    for s in sizes:
        out.append(z[..., o:o + s])
        o += s
    return out


def _axial_rope_tables(n_ctx, rows, rot_dim):
    n_freq = rot_dim // 4
    freqs = ROPE_THETA ** (-jnp.arange(n_freq, dtype=jnp.float32) / n_freq)
    row = jnp.repeat(jnp.arange(rows, dtype=jnp.float32), GRID_W)
    col = (jnp.arange(rows * GRID_W) % GRID_W).astype(jnp.float32)
    ang = jnp.concatenate([row[:, None] * freqs, col[:, None] * freqs], axis=-1)
    ang = jnp.concatenate([jnp.zeros((n_ctx, rot_dim // 2), jnp.float32), ang], axis=0)
    return jnp.cos(ang), jnp.sin(ang)


def _rope(x, cos, sin):
    half = x.shape[-1] // 2
    xf = x.astype(jnp.float32)
    x1, x2 = xf[..., :half], xf[..., half:]
    cc, ss = cos[None, :, None, :], sin[None, :, None, :]
    return jnp.concatenate([x1 * cc - x2 * ss, x1 * ss + x2 * cc], axis=-1).astype(x.dtype)


def _sdpa(q, k, v):
    b, tq, h, dk = q.shape
    hk = k.shape[2]
    qg = q.reshape(b, tq, hk, h // hk, dk)
    s = jnp.einsum('bqgrd,bkgd->bgrqk', qg, k).astype(jnp.float32) * (dk ** -0.5)
    p = jax.nn.softmax(s, axis=-1).astype(v.dtype)
    o = jnp.einsum('bgrqk,bkgd->bqgrd', p, v)
    return o.reshape(b, tq, h, v.shape[-1])


def _attention(q, k, v, n_ctx):
    b, n, h, dk = q.shape
    n_lat = n - n_ctx
    o_ctx = _sdpa(q[:, :n_ctx], k[:, :n_ctx], v[:, :n_ctx])
    qb = q[:, n_ctx:].reshape(b, n_lat // QBLK, QBLK, h, dk).transpose(1, 0, 2, 3, 4)
    o_lat = lax.map(lambda qq: _sdpa(qq, k, v), qb)
    o_lat = o_lat.transpose(1, 0, 2, 3, 4).reshape(b, n_lat, h, v.shape[-1])
    return jnp.concatenate([o_ctx, o_lat], axis=1)


def _per_seq(fn, z, n_ctx):
    return jnp.concatenate([fn(z[:, :n_ctx]), fn(z[:, n_ctx:])], axis=1)


def _chunk_mix(v, w_s, b_s):
    b, t, w = v.shape
    vc = v.reshape(b, t // SGU_CHUNK, SGU_CHUNK, SGU_GROUPS, w // SGU_GROUPS)
    m = jnp.einsum('gpq,bnqgc->bnpgc', w_s, vc) + b_s.T[None, None, :, :, None]
    return m.reshape(b, t, w)


def _dwconv(z, w):
    return lax.conv_general_dilated(z, w[:, None, :], (1,), ((CONV_W // 2, CONV_W // 2),),
                                    dimension_numbers=('NWC', 'WIO', 'NWC'),
                                    feature_group_count=z.shape[-1])


def _modulate(xn, shift_c, scale_c, shift_l, scale_l, n_ctx):
    hc = xn[:, :n_ctx] * (1 + scale_c) + shift_c
    hl = xn[:, n_ctx:] * (1 + scale_l[:, None]) + shift_l[:, None]
    return jnp.concatenate([hc, hl], axis=1)


def _gated(y, gate_c, gate_l, n_ctx):
    return jnp.concatenate([y[:, :n_ctx] * gate_c, y[:, n_ctx:] * gate_l[:, None]], axis=1)


def _mixer(h, n_ctx, rope_mla, rope_gqa, w_in, sgu_gain, w_sgu, b_sgu, mla_q_gain, w_uq,
           mla_kv_gain, w_ukv, w_conv, gqa_q_gain, gqa_k_gain, w_gate, b_gate, w_branch, w_out):
    b, n, _ = h.shape
    (a_u, a_v, m_q, m_kv, m_kpe, c_b, c_c, c_x, g_q, g_k, g_v) = _split_cols(h @ w_in, SPLIT_SIZES)

    a_v = _rmsnorm(jax.nn.gelu(a_v), sgu_gain)
    y_a = jax.nn.gelu(a_u) * _per_seq(lambda s: _chunk_mix(s, w_sgu, b_sgu), a_v, n_ctx)

    cos_m, sin_m = rope_mla
    q = (_rmsnorm(m_q, mla_q_gain) @ w_uq).reshape(b, n, MLA_HEADS, MLA_NOPE + MLA_ROPE)
    q = jnp.concatenate([q[..., :MLA_NOPE], _rope(q[..., MLA_NOPE:], cos_m, sin_m)], axis=-1)
    kv = (_rmsnorm(m_kv, mla_kv_gain) @ w_ukv).reshape(b, n, MLA_HEADS, MLA_NOPE + MLA_V)
    k_pe = _rope(m_kpe[:, :, None, :], cos_m, sin_m)
    k = jnp.concatenate([kv[..., :MLA_NOPE],
                         jnp.broadcast_to(k_pe, (b, n, MLA_HEADS, MLA_ROPE))], axis=-1)
    y_b = _attention(q, k, kv[..., MLA_NOPE:], n_ctx).reshape(b, n, BRANCH_W)

    y_c = c_b * _per_seq(lambda s: _dwconv(s, w_conv), c_c * c_x, n_ctx)

    cos_g, sin_g = rope_gqa
    qd = _rope(_rmsnorm(g_q.reshape(b, n, GQA_HEADS, GQA_HEAD_DIM), gqa_q_gain), cos_g, sin_g)
    kd = _rope(_rmsnorm(g_k.reshape(b, n, GQA_KV_HEADS, GQA_HEAD_DIM), gqa_k_gain), cos_g, sin_g)
    vd = g_v.reshape(b, n, GQA_KV_HEADS, GQA_HEAD_DIM)
    y_d = _attention(qd, kd, vd, n_ctx).reshape(b, n, BRANCH_W)

    merged = None
    for i, y in enumerate((y_a, y_b, y_c, y_d)):
        term = jax.nn.sigmoid(h @ w_gate[i] + b_gate[i]) * (y @ w_branch[i])
        merged = term if merged is None else merged + term
    return merged @ w_out


def _moe(h, w_gr, b_gr, w_er, b_er, w_gu, w_dn):
    shp = h.shape
    t = h.reshape(-1, shp[-1])
    n = t.shape[0]
    pg = jax.nn.softmax((t @ w_gr).astype(jnp.float32) + b_gr.astype(jnp.float32), axis=-1)
    pg_top, g_sel = lax.top_k(pg, 1)
    le = ((t @ w_er).astype(jnp.float32) + b_er.astype(jnp.float32)).reshape(n, N_GROUPS, EXPERTS_PER_GROUP)
    le = le[jnp.arange(n), g_sel[:, 0]]
    pe_top, e_sel = lax.top_k(jax.nn.softmax(le, axis=-1), TOP_K)
    wts = pg_top * pe_top / jnp.sum(pe_top, axis=-1, keepdims=True)
    eid = g_sel * EXPERTS_PER_GROUP + e_sel

    a = n * TOP_K
    flat_e = eid.reshape(-1)
    flat_t = jnp.repeat(jnp.arange(n, dtype=jnp.int32), TOP_K)
    flat_w = wts.reshape(-1)
    order = jnp.argsort(flat_e)
    e_s, t_s, w_s = flat_e[order], flat_t[order], flat_w[order]
    counts = jnp.bincount(flat_e, length=N_EXPERTS)
    start = jnp.cumsum(counts) - counts
    padded = (counts + MOE_BLK - 1) // MOE_BLK * MOE_BLK
    pend = jnp.cumsum(padded)
    pstart = pend - padded
    dest = pstart[e_s] + jnp.arange(a, dtype=jnp.int32) - start[e_s]
    cap = -(-a // MOE_BLK) * MOE_BLK + N_EXPERTS * MOE_BLK
    nblk = cap // MOE_BLK
    tok_buf = jnp.zeros((cap,), jnp.int32).at[dest].set(t_s)
    w_buf = jnp.zeros((cap,), t.dtype).at[dest].set(w_s.astype(t.dtype))
    blk_e = jnp.minimum(jnp.searchsorted(pend, jnp.arange(nblk, dtype=jnp.int32) * MOE_BLK, side='right'),
                        N_EXPERTS - 1)
    xb = t[tok_buf].reshape(nblk, MOE_BLK, shp[-1])

    def expert_block(args):
        xe, e = args
        gu = xe @ w_gu[e]
        return (jax.nn.silu(gu[:, :D_EXPERT]) * gu[:, D_EXPERT:]) @ w_dn[e]

    yb = lax.map(expert_block, (xb, blk_e)).reshape(cap, shp[-1])
    out = jnp.zeros_like(t).at[tok_buf].add(yb * w_buf[:, None])
    return out.reshape(shp)


def setup_inputs(seed: int = 0) -> dict:
    key = jax.random.key(seed)
    ks = iter(jax.random.split(key, 40))
    D = D_MODEL
    L = DEPTH

    def nrm(shape, scale):
        return jax.random.normal(next(ks), shape, jnp.float32) * scale

    def gain(shape):
        return 1.0 + nrm(shape, 0.02)

    return {
        'x': nrm((BATCH, SEQ, D), 1.0),
        'c': nrm((BATCH, D), 1.0),
        'ctx': nrm((BATCH, CTX_LEN, D), 1.0),
        'c_ctx': nrm((D,), 1.0),
        'w_ada': nrm((L, D, 6 * D), 0.5 * D ** -0.5),
        'b_ada': nrm((L, 6 * D), 0.02),
        'w_in': nrm((L, D, D_IN), D ** -0.5),
        'sgu_gain': gain((L, BRANCH_W)),
        'w_sgu': nrm((L, SGU_GROUPS, SGU_CHUNK, SGU_CHUNK), SGU_CHUNK ** -0.5),
        'b_sgu': gain((L, SGU_GROUPS, SGU_CHUNK)),
        'mla_q_gain': gain((L, MLA_Q_RANK)),
        'w_uq': nrm((L, MLA_Q_RANK, MLA_HEADS * (MLA_NOPE + MLA_ROPE)), MLA_Q_RANK ** -0.5),
        'mla_kv_gain': gain((L, MLA_KV_RANK)),
        'w_ukv': nrm((L, MLA_KV_RANK, MLA_HEADS * (MLA_NOPE + MLA_V)), MLA_KV_RANK ** -0.5),
        'w_conv': nrm((L, CONV_W, BRANCH_W), CONV_W ** -0.5),
        'gqa_q_gain': gain((L, GQA_HEAD_DIM)),
        'gqa_k_gain': gain((L, GQA_HEAD_DIM)),
        'w_gate': nrm((L, N_BRANCH, D, D), D ** -0.5),
        'b_gate': nrm((L, N_BRANCH, D), 0.02),
        'w_branch': nrm((L, N_BRANCH, BRANCH_W, D), BRANCH_W ** -0.5),
        'w_out': nrm((L, D, D), D ** -0.5),
        'w_group_router': nrm((L, D, N_GROUPS), D ** -0.5),
        'b_group_router': nrm((L, N_GROUPS), 0.01),
        'w_expert_router': nrm((L, D, N_EXPERTS), D ** -0.5),
        'b_expert_router': nrm((L, N_EXPERTS), 0.01),
        'w_expert_gate_up': nrm((L, N_EXPERTS, D, 2 * D_EXPERT), D ** -0.5),
        'w_expert_down': nrm((L, N_EXPERTS, D_EXPERT, D), D_EXPERT ** -0.5),
        'final_gain': gain((D,)),
    }


def reference(x, c, ctx, c_ctx, w_ada, b_ada, w_in, sgu_gain, w_sgu, b_sgu, mla_q_gain, w_uq,
              mla_kv_gain, w_ukv, w_conv, gqa_q_gain, gqa_k_gain, w_gate, b_gate, w_branch, w_out,
              w_group_router, b_group_router, w_expert_router, b_expert_router,
              w_expert_gate_up, w_expert_down, final_gain):
    seq = x.shape[1]
    n_ctx = ctx.shape[1]
    rows = seq // GRID_W
    rope_mla = _axial_rope_tables(n_ctx, rows, MLA_ROPE)
    rope_gqa = _axial_rope_tables(n_ctx, rows, GQA_HEAD_DIM)
    s_lat = jax.nn.silu(c)
    s_ctx = jax.nn.silu(c_ctx)
    hs = jnp.concatenate([ctx, x], axis=1)
    for l in range(DEPTH):
        ml = jnp.split(s_lat @ w_ada[l] + b_ada[l], 6, axis=-1)
        mc = jnp.split(s_ctx @ w_ada[l] + b_ada[l], 6, axis=-1)
        hn = _modulate(_rmsnorm(hs), mc[0], mc[1], ml[0], ml[1], n_ctx)
        y = _mixer(hn, n_ctx, rope_mla, rope_gqa, w_in[l], sgu_gain[l], w_sgu[l], b_sgu[l],
                   mla_q_gain[l], w_uq[l], mla_kv_gain[l], w_ukv[l], w_conv[l], gqa_q_gain[l],
                   gqa_k_gain[l], w_gate[l], b_gate[l], w_branch[l], w_out[l])
        hs = hs + _gated(y, mc[2], ml[2], n_ctx)
        if l == DEPTH - 1:
            hs = hs[:, n_ctx:]
            n_keep = 0
        else:
            n_keep = n_ctx
        hn = _modulate(_rmsnorm(hs), mc[3], mc[4], ml[3], ml[4], n_keep)
        f = _moe(hn, w_group_router[l], b_group_router[l], w_expert_router[l], b_expert_router[l],
                 w_expert_gate_up[l], w_expert_down[l])
        hs = hs + _gated(f, mc[5], ml[5], n_keep)
    return _rmsnorm(hs, final_gain)
```

```python
import numpy as np
from contextlib import ExitStack
import concourse.bass as bass
import concourse.mybir as mybir
from concourse.bass_utils import run_bass_kernel_spmd

F32 = mybir.dt.float32
I32 = mybir.dt.int32
BF16 = mybir.dt.bfloat16
AF = mybir.ActivationFunctionType
ALU = mybir.AluOpType
AX = mybir.AxisListType

ENGS = ('pe', 'act', 'dve', 'pool', 'sp')
ENGOBJ = {'pe': 'tensor', 'act': 'scalar', 'dve': 'vector', 'pool': 'gpsimd', 'sp': 'sync'}

D = 1024
T = 2304
NCTX = 256
NT = 18
DEPTH = 4
EPS = 1e-6
NB = 68
CAP = NB * 128
BLOCKS = [(0, 256), (256, 512), (768, 512), (1280, 512), (1792, 512)]


STRICT = True


class Prog:
    def __init__(self, nc, st):
        self.nc = nc
        self.st = st
        self.ops = []
        self.buf = {}
        self.sems = {}
        self.cnt = {}
        self.val = {}
        self.seen = {e: {} for e in ENGS}
        self.flushed = 0
        self.nwaits = 0

    def _deps(self, eng, r, w, is_dma):
        deps = set()
        rw = set()
        for k in r:
            b = self.buf.get(k)
            if b is not None and b[0] is not None:
                deps.add(b[0])
                rw.add(b[0])
        for k in w:
            b = self.buf.get(k)
            if b is not None:
                if b[0] is not None:
                    deps.add(b[0])
                for ri in b[1].values():
                    deps.add(ri)
        out = []
        for d in deps:
            o = self.ops[d]
            if (not o['dma']) and (not is_dma) and o['eng'] == eng:
                if eng == 'pe':
                    continue
                if (not STRICT) and d not in rw:
                    continue
            out.append(d)
        return out

    def _commit(self, idx, semname, r, w):
        for k in r:
            b = self.buf.setdefault(k, [None, {}])
            b[1][semname] = idx
        for k in w:
            self.buf[k] = [idx, {}]

    def op(self, eng, fn, r=(), w=()):
        deps = self._deps(eng, r, w, False)
        idx = len(self.ops)
        self.ops.append(dict(eng=eng, fn=fn, deps=deps, dma=False, sem='E' + eng, sig=False))
        for d in deps:
            self.ops[d]['sig'] = True
        self._commit(idx, 'E' + eng, r, w)
        return idx

    def dma(self, q, fn, semkey, r=(), w=()):
        deps = self._deps(q, r, w, True)
        idx = len(self.ops)
        self.ops.append(dict(eng=q, fn=fn, deps=deps, dma=True, sem='D' + str(semkey), sig=True))
        for d in deps:
            self.ops[d]['sig'] = True
        self._commit(idx, 'D' + str(semkey), r, w)
        return idx

    def flush(self, final_keys=()):
        nc = self.nc
        ops = self.ops
        lo = self.flushed
        hi = len(ops)
        if lo == hi:
            return
        dma_last = {}
        for i in range(lo, hi):
            if ops[i]['dma']:
                dma_last[ops[i]['sem']] = i
        ops.append(dict(eng='sp', fn=None, deps=list(dma_last.values()), dma=False, sem='Esp', sig=False))
        hi = len(ops)
        for i in range(lo, hi):
            o = ops[i]
            s = o['sem']
            if s not in self.sems:
                self.sems[s] = self.st.enter_context(nc.semaphore(s))
                self.cnt[s] = 0
            if o['fn'] is None:
                continue
            if o['dma']:
                self.cnt[s] += 16
                self.val[i] = self.cnt[s]
            else:
                if o['sig']:
                    self.cnt[s] += 1
                    self.val[i] = self.cnt[s]
        per = {e: [] for e in ENGS}
        for i in range(lo, hi):
            per[ops[i]['eng']].append(i)
        prog = self

        def body(ename):
            def f(e):
                seen = prog.seen[ename]
                for i in per[ename]:
                    o = ops[i]
                    need = {}
                    for d in o['deps']:
                        if d < lo and not ops[d]['dma'] and d not in prog.val:
                            continue
                        if d not in prog.val:
                            continue
                        s = ops[d]['sem']
                        need[s] = max(need.get(s, 0), prog.val[d])
                    for s, v in need.items():
                        if seen.get(s, 0) < v:
                            e.wait_ge(prog.sems[s], v)
                            seen[s] = v
                            prog.nwaits += 1
                    if o['fn'] is None:
                        continue
                    ins = o['fn'](e)
                    if o['dma']:
                        ins.then_inc(prog.sems[o['sem']], 16)
                    elif i in prog.val:
                        ins.then_inc(prog.sems[o['sem']], 1)
            return f

        with nc.Block() as block:
            for ename in ENGS:
                if per[ename]:
                    getattr(block, ENGOBJ[ename])(body(ename))
        self.flushed = hi
        self.buf = {}


class Rot:
    def __init__(self, name, tiles):
        self.name = name
        self.tiles = tiles
        self.i = 0

    def next(self):
        k = self.i % len(self.tiles)
        self.i += 1
        return self.tiles[k], f"{self.name}{k}"


def build(n_layers, final):
    nc = bass.Bass("TRN2", target_bir_lowering=False)
    L = n_layers

    def din(name, shape):
        return nc.dram_tensor(name, list(shape), F32, kind="ExternalInput").ap()

    hs0 = din("hs0", [T, D])
    cvec = din("cvec", [128, 8, 2])
    w_ada = din("w_ada", [L, D, 6 * D])
    b_adac = din("b_adac", [L, 128, 48])
    w_inx = din("w_inx", [L, D, 3072])
    sgu_wT = din("sgu_wT", [L, 128, 4, 128])
    sgu_b = din("sgu_b", [L, 128, 4])
    sgu_gain = din("sgu_gain", [L, 256])
    mla_qg = din("mla_qg", [L, 128, 2])
    mla_kvg = din("mla_kvg", [L, 128, 1])
    w_uqx = din("w_uqx", [L, 256, 768])
    w_ukvk = din("w_ukvk", [L, 128, 256])
    w_ukvv = din("w_ukvv", [L, 128, 256])
    convw = din("convw", [L, 128, 2, 3])
    gqag = din("gqag", [L, 128, 4])
    w_gate = din("w_gate", [L, 4, D, D])
    b_gatec = din("b_gatec", [L, 128, 4, 8])
    w_branch = din("w_branch", [L, 4, 256, D])
    w_out = din("w_out", [L, D, D])
    w_r = din("w_r", [L, D, 36])
    b_r = din("b_r", [L, 36])
    w_gu_r = din("w_gu_r", [L, 32 * 128, 4096])
    w_dn_r = din("w_dn_r", [L, 32 * 128, 2048])
    wgu_bf = nc.dram_tensor("wgu_bf", [32 * 128, 4096], BF16).ap()
    wdn_bf = nc.dram_tensor("wdn_bf", [32 * 128, 2048], BF16).ap()
    final_gain = din("final_gain", [D])
    ropeG = din("ropeG", [2, 128, T])
    ropeM = din("ropeM", [2, 128, T])
    xs_d = nc.dram_tensor("xs_scr", [CAP, D], BF16).ap()
    ys_d = nc.dram_tensor("ys_scr", [CAP, D], F32).ap()
    if final:
        out_d = nc.dram_tensor("out", [T - NCTX, D], F32, kind="ExternalOutput").ap()
        hs_d = nc.dram_tensor("hs_scr", [T, D], F32).ap()
    else:
        hs_d = nc.dram_tensor("hs_out", [T, D], F32, kind="ExternalOutput").ap()

    with ExitStack() as st:
        P = Prog(nc, st)

        uid = [0]

        def SB(scope, name, shape, dt=F32):
            uid[0] += 1
            return scope.enter_context(nc.sbuf_tensor(f"{name}_{uid[0]}", list(shape), dt))

        psbig = [st.enter_context(nc.psum_tensor(f"psb{i}", [128, 1024], F32)) for i in range(4)]
        ps = [psbig[i // 2][:, (i % 2) * 512:(i % 2 + 1) * 512] for i in range(8)]
        PSK = [f"ps{i}" for i in range(8)]
        hnT = SB(st, "hnT", [128, 8, T], BF16)
        identf = SB(st, "identf", [128, 128], F32)
        identb = SB(st, "identb", [128, 128], BF16)
        psb0 = psbig[0][:, :].bitcast(BF16)
        onesb = SB(st, "onesb", [128, 128], BF16)
        blk1b = SB(st, "blk1b", [128, 128], BF16)
        onesf = SB(st, "onesf", [128, 128], F32)
        modc = SB(st, "modc", [128, L, 6, 8, 2], F32)
        sT = SB(st, "sT", [128, 8, 2], F32)

        def hk(b):
            t0, n = BLOCKS[b]
            return [f"hnT{t}" for t in range(t0 // 128, (t0 + n) // 128)]

        def bsl(b):
            t0, n = BLOCKS[b]
            return slice(t0, t0 + n)

        P.op('pool', lambda e: e.memset(identf[:], 0.0), w=['identf'])
        P.op('pool', lambda e: e.affine_select(out=identf[:], in_=identf[:], pattern=[[-1, 128]], base=0,
                                                channel_multiplier=1, compare_op=ALU.not_equal, fill=1.0),
             r=['identf'], w=['identf'])
        P.op('pool', lambda e: e.tensor_copy(out=identb[:], in_=identf[:]), r=['identf'], w=['identb'])
        P.op('pool', lambda e: e.memset(onesb[:], 1.0), w=['onesb'])
        P.op('pool', lambda e: e.memset(onesf[:], 1.0), w=['onesf'])
        P.op('pool', lambda e: e.memset(blk1b[:], 0.0), w=['blk1b'])
        P.op('pool', lambda e: e.memset(blk1b[0:64, 0:64], 1.0), r=['blk1b'], w=['blk1b'])
        P.op('pool', lambda e: e.memset(blk1b[64:128, 64:128], 1.0), r=['blk1b'], w=['blk1b'])

        if True:
            with ExitStack() as sc:
                hs0v = hs0.rearrange("(n p) d -> n p d", p=128)
                hsv = hs_d.rearrange("(n p) d -> n p d", p=128)
                zt = SB(sc, "zt", [128, D], BF16)
                P.op('pool', lambda e: e.memset(zt[:], 0.0), w=['zt'])
                xsv0 = xs_d.rearrange("(n p) d -> n p d", p=128)
                for b in range(NB):
                    P.dma('sp', lambda e, b=b: e.dma_start(out=xsv0[b], in_=zt[:]), 'zinit', r=['zt'], w=[f"xsz{b}"])
                P.dma('sp', lambda e: e.dma_start(out=sT[:], in_=cvec), 'sT', w=['sT'])
                P.op('act', lambda e: e.activation(out=sT[:], in_=sT[:], func=AF.Silu), r=['sT'], w=['sT'])
                wa = Rot("wa", [SB(sc, f"wa{i}", [128, 8, 1024], BF16) for i in range(3)])
                sTb = SB(sc, "sTb", [128, 8, 2], BF16)
                P.op('dve', lambda e: e.tensor_copy(out=sTb[:], in_=sT[:]), r=['sT'], w=['sTb'])
                bad = SB(sc, "bad", [128, L, 48])
                P.dma('sp', lambda e: e.dma_start(out=bad[:], in_=b_adac.rearrange("l p n -> p l n")), 'bad',
                      w=['bad'])
                for l in range(L):
                    for j in range(6):
                        wt, k = wa.next()
                        src = w_ada[l, :, j * 1024:(j + 1) * 1024].rearrange("(kc p) n -> p kc n", p=128)
                        P.dma('pool', lambda e, wt=wt, src=src: e.dma_start(out=wt[:], in_=src), k, w=[k])
                        pk = PSK[j % 2]
                        pt = ps[j % 2]
                        for fo in range(8):
                            for kc in range(8):
                                P.op('pe', lambda e, pt=pt, wt=wt, fo=fo, kc=kc: e.matmul(
                                    pt[:, fo * 2:fo * 2 + 2], lhsT=wt[:, kc, fo * 128:(fo + 1) * 128],
                                    rhs=sTb[:, kc, :], start=(kc == 0), stop=(kc == 7)),
                                    r=[k, 'sTb'], w=[pk])
                        for s in range(2):
                            P.op('dve', lambda e, pt=pt, l=l, j=j, s=s: e.tensor_tensor(
                                out=modc[:, l, j, :, s], in0=pt[:, s:16:2], in1=bad[:, l, j * 8:(j + 1) * 8],
                                op=ALU.add), r=[pk, 'bad'], w=['modc'])
                    for j in (1, 4):
                        P.op('dve', lambda e, l=l, j=j: e.tensor_scalar(
                            out=modc[:, l, j], in0=modc[:, l, j], scalar1=1.0, scalar2=None, op0=ALU.add),
                            r=['modc'], w=['modc'])
                P.flush()

        rawX = SB(st, "rawX", [128, 9216], BF16)
        rawY = SB(st, "rawY", [128, 10240], BF16)
        v8 = lambda buf, a, n: buf[:, a:a + 8 * n].rearrange("p (k n) -> p k n", k=8)
        VA = dict(wA=v8(rawX, 0, 512), wsT=rawX[:, 4096:4608].rearrange("p (k n) -> p k n", k=4))
        VM = dict(wg=v8(rawY, 0, 1024), wb=rawY[:, 8192:10240].rearrange("p (k n) -> p k n", k=2))
        VC = dict(wC=v8(rawX, 0, 768))
        VD = dict(wD=v8(rawX, 0, 1024), wV=v8(rawX, 8192, 128))
        VB = dict(wB=v8(rawX, 0, 640), wuq=rawX[:, 5120:6656].rearrange("p (k n) -> p k n", k=2),
                  wkk=rawX[:, 6656:6912], wkv=rawX[:, 6912:7168])
        VO = dict(wo=v8(rawX, 0, 1024))

        def pl(dst, src, key):
            P.dma('pool', lambda e: e.dma_start(out=dst, in_=src), key, w=[key])

        def winv(l, c0, c1):
            return w_inx[l, :, c0:c1].rearrange("(kc p) n -> p kc n", p=128)

        def ld_A(l):
            pl(VA['wA'], winv(l, 0, 512), "wA")
            pl(VA['wsT'], sgu_wT[l], "wsT")

        def ld_M(l, i):
            for h2 in range(2):
                pl(VM['wg'][:, :, h2 * 512:(h2 + 1) * 512],
                   w_gate[l, i, :, h2 * 512:(h2 + 1) * 512].rearrange("(kc p) n -> p kc n", p=128), f"wg{h2}")
            pl(VM['wb'], w_branch[l, i].rearrange("(kc p) n -> p kc n", p=128), "wb")

        def ld_C(l):
            pl(VC['wC'], winv(l, 1280, 2048), "wC")

        def ld_D(l):
            pl(VD['wD'], winv(l, 2048, 3072), "wD")
            pl(VD['wV'], winv(l, 512, 640), "wV")

        def ld_B(l):
            pl(VB['wB'], winv(l, 640, 1280), "wB")
            pl(VB['wuq'], w_uqx[l].rearrange("(c p) n -> p c n", p=128), "wuq")
            pl(VB['wkk'], w_ukvk[l], "wkk")
            pl(VB['wkv'], w_ukvv[l], "wkv")

        def ld_O(l):
            for h2 in range(2):
                pl(VO['wo'][:, :, h2 * 512:(h2 + 1) * 512],
                   w_out[l, :, h2 * 512:(h2 + 1) * 512].rearrange("(kc p) n -> p kc n", p=128), f"wo{h2}")

        def precast(l, part):
            gsrc = w_gu_r[l].rearrange("r (h c) -> (r h) c", h=2)
            gdst = wgu_bf.rearrange("r (h c) -> (r h) c", h=2)
            jobs = [(gsrc, gdst, i) for i in range(16)] + [(w_dn_r[l], wdn_bf, i) for i in range(8)]
            lo_, hi_ = [(0, 4), (4, 14), (14, 24)][part]
            for j, (src, dst, i) in enumerate(jobs[lo_:hi_]):
                P.dma('pool', lambda e, src=src, dst=dst, i=i: e.dma_start(
                    out=dst[i * 512:(i + 1) * 512, :], in_=src[i * 512:(i + 1) * 512, :]), 'pc', w=[f"pc{part}_{j}"])

        def swpipe(items, stage1, stage2):
            prev = None
            for it in items:
                ctx = stage1(it)
                if prev is not None:
                    stage2(*prev)
                prev = (it, ctx)
            if prev is not None:
                stage2(*prev)

        def rstd_from_ssq(ssq_ap, out_ap, n, keys_r, key_w):
            P.op('dve', lambda e: e.tensor_scalar(out=out_ap, in0=ssq_ap, scalar1=1.0 / n, scalar2=EPS,
                                                  op0=ALU.mult, op1=ALU.add), r=keys_r, w=[key_w])
            P.op('act', lambda e: e.activation(out=out_ap, in_=out_ap, func=AF.Sqrt), r=[key_w], w=[key_w])
            P.op('dve', lambda e: e.reciprocal(out=out_ap, in_=out_ap), r=[key_w], w=[key_w])

        def gate_bcast(sc, l, j, name):
            gb = SB(sc, name, [128, 2, D])
            dgs = [SB(sc, f"{name}dg{i}", [128, 4, 128]) for i in range(2)]
            it = 0
            for s in range(2):
                for half in range(2):
                    dg, dk = dgs[it % 2], f"{name}dg{it % 2}"
                    pt, pk = ps[it % 2], PSK[it % 2]
                    it += 1
                    P.op('dve', lambda e, dg=dg, half=half, s=s: e.tensor_tensor(
                        out=dg[:], in0=identf[:].unsqueeze(1).broadcast_to([128, 4, 128]),
                        in1=modc[:, l, j, half * 4:(half + 1) * 4, s].unsqueeze(2).broadcast_to([128, 4, 128]),
                        op=ALU.mult), r=['identf', 'modc'], w=[dk])
                    P.op('pe', lambda e, pt=pt, dg=dg: e.matmul(pt[:, :], lhsT=onesf[:],
                                                                rhs=dg[:].rearrange("p a b -> p (a b)"),
                                                                start=True, stop=True), r=['onesf', dk], w=[pk])
                    P.op('act', lambda e, pt=pt, half=half, s=s: e.copy(out=gb[:, s, half * 512:(half + 1) * 512],
                                                                        in_=pt[:, :]), r=[pk], w=[name])
            return gb

        def norm_phase(l, jsh, jsc, hntok=None, pre=None, src=None):
            with ExitStack() as sc:
                if pre is not None:
                    pre()
                if hntok is not None:
                    scb = gate_bcast(sc, l, jsc, "nscb")
                    shb = gate_bcast(sc, l, jsh, "nshb")
                xin = Rot("nx", [SB(sc, f"nx{i}", [128, D]) for i in range(3)])
                junk = SB(sc, "njunk", [128, D], BF16)
                xn = Rot("nxn", [SB(sc, f"nxn{i}", [128, D]) for i in range(2)])
                if hntok is not None:
                    htmp = Rot("nht", [SB(sc, f"nht{i}", [128, D]) for i in range(2)])
                st_ = SB(sc, "nst", [128, NT, 2])
                def n1(tt):
                    xt, kx = xin.next()
                    P.dma('sp', lambda e: e.dma_start(out=xt[:], in_=(hsv if src is None else src)[tt]), kx,
                          r=[f"hs{tt}"], w=[kx])
                    P.op('act', lambda e: e.activation(out=junk[:], in_=xt[:], func=AF.Square,
                                                       accum_out=st_[:, tt, 0:1]),
                         r=[kx], w=['njunk', f"nst{tt}"])
                    rstd_from_ssq(st_[:, tt, 0:1], st_[:, tt, 1:2], D, [f"nst{tt}"], f"nrs{tt}")
                    xo, ko = xn.next()
                    P.op('act', lambda e: e.activation(out=xo[:], in_=xt[:], func=AF.Identity, scale=st_[:, tt, 1:2]),
                         r=[kx, f"nrs{tt}"], w=[ko])
                    s = 1 if tt < 2 else 0
                    if hntok is not None:
                        ht_, htk = htmp.next()
                        P.op('dve', lambda e: e.tensor_tensor(
                            out=ht_[:], in0=xo[:], in1=scb[:, s, :], op=ALU.mult),
                            r=[ko, 'nscb'], w=[htk])
                        P.op('dve', lambda e: e.tensor_tensor(
                            out=hntok[:, tt, :], in0=ht_[:], in1=shb[:, s, :], op=ALU.add),
                            r=[htk, 'nshb'], w=[f"hntok{tt}"])
                    return xo, ko

                def n2(tt, ctx):
                    xo, ko = ctx
                    s = 1 if tt < 2 else 0
                    for half in range(2):
                        pt = ps[(tt % 2) * 2 + half]
                        pk = PSK[(tt % 2) * 2 + half]
                        for q in range(4):
                            kc = half * 4 + q
                            P.op('pe', lambda e, pt=pt, kc=kc, q=q: e.transpose(
                                out=pt[:, q * 128:(q + 1) * 128], in_=xo[:, kc * 128:(kc + 1) * 128],
                                identity=identf[:]), r=[ko, 'identf'], w=[pk])
                        for q in range(4):
                            kc = half * 4 + q
                            if q % 2 == 0:
                                P.op('act', lambda e, pt=pt, kc=kc, q=q: e.activation(
                                    out=hnT[:, kc, tt * 128:(tt + 1) * 128], in_=pt[:, q * 128:(q + 1) * 128],
                                    func=AF.Identity, scale=modc[:, l, jsc, kc, s:s + 1],
                                    bias=modc[:, l, jsh, kc, s:s + 1]), r=[pk, 'modc'], w=[f"hnT{tt}"])
                            else:
                                P.op('dve', lambda e, pt=pt, kc=kc, q=q: e.tensor_scalar(
                                    out=hnT[:, kc, tt * 128:(tt + 1) * 128], in0=pt[:, q * 128:(q + 1) * 128],
                                    scalar1=modc[:, l, jsc, kc, s:s + 1], scalar2=modc[:, l, jsh, kc, s:s + 1],
                                    op0=ALU.mult, op1=ALU.add), r=[pk, 'modc'], w=[f"hnT{tt}"])
                swpipe(range(NT), n1, n2)
                P.flush()

        def wload(dst, src, key):
            P.dma('pool', lambda e: e.dma_start(out=dst, in_=src), key, w=[key])

        def fload(dst, src, key):
            P.dma('sp', lambda e: e.dma_start(out=dst, in_=src), key, w=[key])

        def win_view(l, c0, c1):
            return w_inx[l, :, c0:c1].rearrange("(kc p) n -> p kc n", p=128)

        def attention(sc, nm, nheads, dk, QTf, KTf, Vf, qk_keys, yT, scale):
            Pt = Rot(nm + "Pt", [SB(sc, f"{nm}Pt{i}", [128, 2, 512], BF16) for i in range(2)])
            rec = Rot(nm + "rc", [SB(sc, f"{nm}rc{i}", [128, 512]) for i in range(1)])
            def do_blk(h, b, t0, n, po, pok):
                nkt = 2 if b == 0 else NT
                npair = nkt // 2
                qs = QTf(h)[:, t0:t0 + n]

                def smm(j):
                    for a_ in range(2):
                        kt = 2 * j + a_
                        pt, pk = ps[(j % 2) * 2 + a_], PSK[(j % 2) * 2 + a_]
                        P.op('pe', lambda e, pt=pt, kt=kt: e.matmul(
                            pt[:, 0:n], lhsT=KTf(h)[:, kt * 128:(kt + 1) * 128], rhs=qs,
                            start=True, stop=True), r=qk_keys, w=[pk])

                def pv(j):
                    big = psbig[j % 2]
                    pe_, pek = Pt.next()
                    P.op('act', lambda e: e.activation(
                        out=pe_[:, :, 0:n], in_=big[:, :].rearrange("p (a b) -> p a b", a=2)[:, :, 0:n],
                        func=AF.Exp, scale=scale), r=[PSK[(j % 2) * 2], PSK[(j % 2) * 2 + 1]], w=[pek])
                    for a_ in range(2):
                        kt = 2 * j + a_
                        P.op('pe', lambda e, a_=a_, kt=kt: e.matmul(
                            po[:, 0:n], lhsT=Vf(h, kt), rhs=pe_[:, a_, 0:n],
                            start=(kt == 0), stop=(kt == nkt - 1)), r=[pek] + qk_keys, w=[pok])
                smm(0)
                for j in range(npair):
                    if j + 1 < npair:
                        smm(j + 1)
                    pv(j)
                rc, rck = rec.next()
                P.op('dve', lambda e: e.reciprocal(out=rc[64:128, 0:n], in_=po[64:128, 0:n]), r=[pok], w=[rck])
                p0 = (h % 2) * 64
                P.op('dve', lambda e: e.tensor_tensor(
                    out=yT[p0:p0 + 64, h // 2, t0:t0 + n], in0=po[0:64, 0:n], in1=rc[64:128, 0:n],
                    op=ALU.mult), r=[pok, rck], w=[f"yT{b}"])
            it = 0
            for h in range(nheads):
                for b, (t0, n) in enumerate(BLOCKS):
                    do_blk(h, b, t0, n, ps[4 + (it % 2)], PSK[4 + (it % 2)])
                    it += 1

        def merge(i, l, yT, mergedT, first, pre=None):
            with ExitStack() as sc:
                if pre is not None:
                    pre()
                wg = VM['wg']
                wb = VM['wb']
                bg = SB(sc, "bg", [128, 8])
                sg = Rot("sg", [SB(sc, f"sg{i_}", [128, 512]) for i_ in range(2)])
                tm = Rot("tm", [SB(sc, f"tm{i_}", [128, 512]) for i_ in range(2)])
                fload(bg[:], b_gatec[l, :, i, :], "bg")
                def mblk(b, t0, n, fo, pg, pgk, pb, pbk):
                    for kc in range(8):
                        P.op('pe', lambda e, kc=kc: e.matmul(
                            pg[:, 0:n], lhsT=wg[:, kc, fo * 128:(fo + 1) * 128], rhs=hnT[:, kc, t0:t0 + n],
                            start=(kc == 0), stop=(kc == 7)), r=[f"wg{fo // 4}"] + hk(b), w=[pgk])
                    for c in range(2):
                        P.op('pe', lambda e, c=c: e.matmul(
                            pb[:, 0:n], lhsT=wb[:, c, fo * 128:(fo + 1) * 128], rhs=yT[:, c, t0:t0 + n],
                            start=(c == 0), stop=(c == 1)), r=["wb", f"yT{b}"], w=[pbk])
                    s_, sk = sg.next()
                    P.op('act', lambda e: e.activation(
                        out=s_[:, 0:n], in_=pg[:, 0:n], func=AF.Sigmoid, bias=bg[:, fo:fo + 1]),
                        r=[pgk, 'bg'], w=[sk])
                    if first:
                        P.op('dve', lambda e: e.tensor_tensor(
                            out=mergedT[:, fo, t0:t0 + n], in0=pb[:, 0:n], in1=s_[:, 0:n], op=ALU.mult),
                            r=[pbk, sk], w=[f"mg{b}"])
                    else:
                        t_, tk = tm.next()
                        P.op('dve', lambda e: e.tensor_tensor(
                            out=t_[:, 0:n], in0=pb[:, 0:n], in1=s_[:, 0:n], op=ALU.mult),
                            r=[pbk, sk], w=[tk])
                        P.op('pool', lambda e: e.tensor_tensor(
                            out=mergedT[:, fo, t0:t0 + n], in0=mergedT[:, fo, t0:t0 + n], in1=t_[:, 0:n],
                            op=ALU.add), r=[tk, f"mg{b}"], w=[f"mg{b}"])
                it = 0
                for b, (t0, n) in enumerate(BLOCKS):
                    for fo in range(8):
                        mblk(b, t0, n, fo, ps[it % 4], PSK[it % 4], ps[4 + it % 4], PSK[4 + it % 4])
                        it += 1
                P.flush()

        rnc = [0]

        def rope_norm_chunk(wq, blkq, blksw, gcol, gswcol, ssq_lhsT, nfeat, dst, dstkey, tmp):
            (tabC, tabS, sqR, rsbR, t1R, t2R, t3R) = tmp

            def r1(it):
                b, (t0, n) = it
                st_ = (rnc[0] % 2) * 3
                rnc[0] += 1
                pz, pzk = ps[st_], PSK[st_]
                pw, pwk = ps[st_ + 1], PSK[st_ + 1]
                pss, pssk = ps[st_ + 2], PSK[st_ + 2]
                sq, sqk = sqR.next()
                rsb, rsk = rsbR.next()
                for kc in range(8):
                    P.op('pe', lambda e, kc=kc: e.matmul(pz[:, 0:n], lhsT=wq[:, kc, blkq * 128:(blkq + 1) * 128],
                                                         rhs=hnT[:, kc, t0:t0 + n], start=(kc == 0), stop=(kc == 7)),
                         r=['wD'] + hk(b), w=[pzk])
                for kc in range(8):
                    P.op('pe', lambda e, kc=kc: e.matmul(pw[:, 0:n], lhsT=wq[:, kc, blksw * 128:(blksw + 1) * 128],
                                                         rhs=hnT[:, kc, t0:t0 + n], start=(kc == 0), stop=(kc == 7)),
                         r=['wD'] + hk(b), w=[pwk])
                tc_, tck = tabC.next()
                ts_, tsk = tabS.next()
                fload(tc_[:, 0:n], ropeG[0, :, t0:t0 + n], tck)
                fload(ts_[:, 0:n], ropeG[1, :, t0:t0 + n], tsk)
                P.op('act', lambda e: e.activation(out=sq[:, 0:n], in_=pz[:, 0:n], func=AF.Square), r=[pzk], w=[sqk])
                P.op('pe', lambda e: e.matmul(pss[:, 0:n], lhsT=ssq_lhsT, rhs=sq[:, 0:n], start=True, stop=True),
                     r=[sqk, 'blk1b'], w=[pssk])
                P.op('dve', lambda e: e.tensor_scalar(out=rsb[:, 0:n], in0=pss[:, 0:n], scalar1=1.0 / nfeat,
                                                      scalar2=EPS, op0=ALU.mult, op1=ALU.add), r=[pssk], w=[rsk])
                P.op('act', lambda e: e.activation(out=rsb[:, 0:n], in_=rsb[:, 0:n], func=AF.Sqrt), r=[rsk], w=[rsk])
                P.op('dve', lambda e: e.reciprocal(out=rsb[:, 0:n], in_=rsb[:, 0:n]), r=[rsk], w=[rsk])
                return (pz, pzk, pw, pwk, rsb, rsk, tc_, tck, ts_, tsk)

            def r2(it, ctx):
                b, (t0, n) = it
                (pz, pzk, pw, pwk, rsb, rsk, tc_, tck, ts_, tsk) = ctx
                t1, t1k = t1R.next()
                t2, t2k = t2R.next()
                t3, t3k = t3R.next()
                P.op('dve', lambda e: e.scalar_tensor_tensor(out=t1[:, 0:n], in0=pz[:, 0:n], scalar=gcol,
                                                             in1=tc_[:, 0:n], op0=ALU.mult, op1=ALU.mult),
                     r=[pzk, tck, 'gq'], w=[t1k])
                P.op('dve', lambda e: e.scalar_tensor_tensor(out=t2[:, 0:n], in0=pw[:, 0:n], scalar=gswcol,
                                                             in1=ts_[:, 0:n], op0=ALU.mult, op1=ALU.mult),
                     r=[pwk, tsk, 'gq'], w=[t2k])
                P.op('pool', lambda e: e.tensor_tensor(out=t3[:, 0:n], in0=t1[:, 0:n], in1=t2[:, 0:n], op=ALU.add),
                     r=[t1k, t2k], w=[t3k])
                if isinstance(dst, tuple):
                    for hh, d_ in enumerate(dst):
                        P.op('dve', lambda e, hh=hh, d_=d_: e.tensor_tensor(
                            out=d_[hh * 64:(hh + 1) * 64, t0:t0 + n], in0=t3[hh * 64:(hh + 1) * 64, 0:n],
                            in1=rsb[hh * 64:(hh + 1) * 64, 0:n], op=ALU.mult), r=[t3k, rsk], w=[dstkey])
                else:
                    P.op('dve', lambda e: e.tensor_tensor(out=dst[:, t0:t0 + n], in0=t3[:, 0:n], in1=rsb[:, 0:n],
                                                          op=ALU.mult), r=[t3k, rsk], w=[dstkey])
            for it in enumerate(BLOCKS):
                r2(it, r1(it))

        for l in range(L):
            last = final and (l == L - 1)
            if l == 0:
                norm_phase(l, 0, 1, pre=lambda: ld_A(l), src=hs0v)
            with ExitStack() as mx:
                mergedT = SB(mx, "mergedT", [128, 8, T], BF16)
                yT = SB(mx, "yT", [128, 2, T], BF16)

                with ExitStack() as sc:
                    ld_M(l, 0)
                    precast(l, 0)
                    wA = VA['wA']
                    wsT = VA['wsT']
                    bs = SB(sc, "bs", [128, 4])
                    gnb = SB(sc, "gnb", [128, 256])
                    g = Rot("ag", [SB(sc, f"ag{i}", [128, 512]) for i in range(2)])
                    junk = SB(sc, "ajunk", [128, 256], BF16)
                    stA = SB(sc, "stA", [128, NT, 2])
                    vn = Rot("avn", [SB(sc, f"avn{i}", [128, 256], BF16) for i in range(2)])
                    ya = Rot("aya", [SB(sc, f"aya{i}", [128, 256]) for i in range(2)])
                    fload(bs[:], sgu_b[l], "bs")
                    fload(gnb[:], sgu_gain[l].partition_broadcast(128), "gnb")
                    def a1(tt):
                        pa, pak = ps[tt % 2], PSK[tt % 2]
                        for kc in range(8):
                            P.op('pe', lambda e, kc=kc: e.matmul(
                                pa[:, :], lhsT=hnT[:, kc, tt * 128:(tt + 1) * 128], rhs=wA[:, kc, :],
                                start=(kc == 0), stop=(kc == 7)), r=['wA', f"hnT{tt}"], w=[pak])
                        g_, gk = g.next()
                        P.op('act', lambda e: e.activation(out=g_[:], in_=pa[:], func=AF.Gelu_apprx_tanh),
                             r=[pak], w=[gk])
                        P.op('act', lambda e: e.activation(out=junk[:], in_=g_[:, 256:512], func=AF.Square,
                                                           accum_out=stA[:, tt, 0:1]),
                             r=[gk], w=['ajunk', f"stA{tt}"])
                        rstd_from_ssq(stA[:, tt, 0:1], stA[:, tt, 1:2], 256, [f"stA{tt}"], f"rsA{tt}")
                        v_, vk = vn.next()
                        P.op('dve', lambda e: e.scalar_tensor_tensor(
                            out=v_[:], in0=g_[:, 256:512], scalar=stA[:, tt, 1:2], in1=gnb[:], op0=ALU.mult,
                            op1=ALU.mult), r=[gk, f"rsA{tt}", 'gnb'], w=[vk])
                        return g_, gk, v_, vk

                    def a2(tt, ctx):
                        g_, gk, v_, vk = ctx
                        pm, pmk = ps[2 + tt % 2], PSK[2 + tt % 2]
                        pT_, pTk = ps[4 + tt % 2], PSK[4 + tt % 2]
                        for gg in range(4):
                            P.op('pe', lambda e, gg=gg: e.matmul(
                                pm[:, gg * 64:(gg + 1) * 64], lhsT=wsT[:, gg, :], rhs=v_[:, gg * 64:(gg + 1) * 64],
                                start=True, stop=True), r=['wsT', vk], w=[pmk])
                        y_, yk = ya.next()
                        for gg in range(4):
                            P.op('dve', lambda e, gg=gg: e.scalar_tensor_tensor(
                                out=y_[:, gg * 64:(gg + 1) * 64], in0=pm[:, gg * 64:(gg + 1) * 64],
                                scalar=bs[:, gg:gg + 1], in1=g_[:, gg * 64:(gg + 1) * 64], op0=ALU.add, op1=ALU.mult),
                                r=[pmk, gk, 'bs'], w=[yk])
                        for c in range(2):
                            P.op('pe', lambda e, c=c: e.transpose(
                                out=pT_[:, c * 128:(c + 1) * 128], in_=y_[:, c * 128:(c + 1) * 128],
                                identity=identf[:]), r=[yk, 'identf'], w=[pTk])
                        bi = [i for i, (t0, n) in enumerate(BLOCKS) if t0 <= tt * 128 < t0 + n][0]
                        for c in range(2):
                            P.op('act', lambda e, c=c: e.copy(
                                out=yT[:, c, tt * 128:(tt + 1) * 128], in_=pT_[:, c * 128:(c + 1) * 128]),
                                r=[pTk], w=[f"yT{bi}"])
                    swpipe(range(NT), a1, a2)
                    P.flush()
                merge(0, l, yT, mergedT, True, pre=lambda: ld_C(l))

                with ExitStack() as sc:
                    ld_M(l, 2)
                    wC = VC['wC']
                    cw = SB(sc, "cw", [128, 2, 3])
                    U = SB(sc, "cU", [128, T + 3])
                    acc = SB(sc, "cacc", [128, T + 3])
                    CB = SB(sc, "cCB", [128, T + 3], BF16)
                    tx = Rot("ctx", [SB(sc, f"ctx{i}", [128, 512]) for i in range(2)])
                    fload(cw[:], convw[l], "cw")
                    off = lambda t0: t0 + 1 if t0 < NCTX else t0 + 2
                    def cblk(c, b, t0, n):
                        pb_, pc_, px_ = ps[(b % 2) * 3], ps[(b % 2) * 3 + 1], ps[(b % 2) * 3 + 2]
                        kb_, kc_, kx_ = PSK[(b % 2) * 3], PSK[(b % 2) * 3 + 1], PSK[(b % 2) * 3 + 2]
                        for (pp, kk, blk) in ((pb_, kb_, c), (pc_, kc_, 2 + c), (px_, kx_, 4 + c)):
                            for kc in range(8):
                                P.op('pe', lambda e, pp=pp, kc=kc, blk=blk: e.matmul(
                                    pp[:, 0:n], lhsT=wC[:, kc, blk * 128:(blk + 1) * 128],
                                    rhs=hnT[:, kc, t0:t0 + n], start=(kc == 0), stop=(kc == 7)),
                                    r=['wC'] + hk(b), w=[kk])
                        o = off(t0)
                        t_, tk = tx.next()
                        P.op('act', lambda e: e.copy(out=t_[:, 0:n], in_=px_[:, 0:n]), r=[kx_], w=[tk])
                        P.op('act', lambda e: e.copy(out=CB[:, o:o + n], in_=pb_[:, 0:n]), r=[kb_], w=['cCB'])
                        P.op('dve', lambda e: e.tensor_tensor(
                            out=U[:, o:o + n], in0=pc_[:, 0:n], in1=t_[:, 0:n], op=ALU.mult), r=[kc_, tk], w=['cU'])

                    def cout(c, b, t0, n):
                        o = off(t0)
                        P.op('pool', lambda e: e.tensor_tensor(
                            out=yT[:, c, t0:t0 + n], in0=CB[:, o:o + n], in1=acc[:, o:o + n], op=ALU.mult),
                            r=['cCB', 'cacc'], w=[f"yT{b}"])

                    def cchunk(c):
                        P.op('pool', lambda e: e.memset(U[:], 0.0), w=['cU'])
                        for b, (t0, n) in enumerate(BLOCKS):
                            cblk(c, b, t0, n)
                        NN = T + 1
                        P.op('dve', lambda e: e.tensor_scalar(out=acc[:, 1:1 + NN], in0=U[:, 0:NN],
                                                              scalar1=cw[:, c, 0:1], scalar2=None, op0=ALU.mult),
                             r=['cU', 'cw'], w=['cacc'])
                        P.op('dve', lambda e: e.scalar_tensor_tensor(
                            out=acc[:, 1:1 + NN], in0=U[:, 1:1 + NN], scalar=cw[:, c, 1:2], in1=acc[:, 1:1 + NN],
                            op0=ALU.mult, op1=ALU.add), r=['cU', 'cw', 'cacc'], w=['cacc'])
                        P.op('dve', lambda e: e.scalar_tensor_tensor(
                            out=acc[:, 1:1 + NN], in0=U[:, 2:2 + NN], scalar=cw[:, c, 2:3], in1=acc[:, 1:1 + NN],
                            op0=ALU.mult, op1=ALU.add), r=['cU', 'cw', 'cacc'], w=['cacc'])
                        for b, (t0, n) in enumerate(BLOCKS):
                            cout(c, b, t0, n)
                    for c in range(2):
                        cchunk(c)
                    P.flush()
                merge(2, l, yT, mergedT, False, pre=lambda: ld_D(l))

                with ExitStack() as sc:
                    ld_M(l, 3)
                    wD = VD['wD']
                    wV = VD['wV']
                    gq = SB(sc, "gq", [128, 4])
                    Vd = SB(sc, "Vd", [128, NT, 2, 128], BF16)
                    KT = SB(sc, "dKT", [128, 2, T], BF16)
                    tmp = (Rot("tbC", [SB(sc, f"tbC{i}", [128, 512]) for i in range(2)]),
                           Rot("tbS", [SB(sc, f"tbS{i}", [128, 512]) for i in range(2)]),
                           Rot("sq", [SB(sc, f"sq{i}", [128, 512], BF16) for i in range(2)]),
                           Rot("rsb", [SB(sc, f"rsb{i}", [128, 512]) for i in range(2)]),
                           Rot("t1", [SB(sc, f"t1{i}", [128, 512]) for i in range(2)]),
                           Rot("t2", [SB(sc, f"t2{i}", [128, 512]) for i in range(2)]),
                           Rot("t3", [SB(sc, f"t3{i}", [128, 512]) for i in range(2)]))
                    fload(gq[:], gqag[l], "gq")
                    P.op('pool', lambda e: e.memset(Vd[:, :, :, 64:128], 1.0), w=['Vd'])
                    for tt in range(NT):
                        pv, pvk = ps[6 + tt % 2], PSK[6 + tt % 2]
                        for kc in range(8):
                            P.op('pe', lambda e, pv=pv, kc=kc, tt=tt: e.matmul(
                                pv[:, 0:128], lhsT=hnT[:, kc, tt * 128:(tt + 1) * 128], rhs=wV[:, kc, :],
                                start=(kc == 0), stop=(kc == 7)), r=['wV', f"hnT{tt}"], w=[pvk])
                        P.op('act', lambda e, pv=pv, tt=tt: e.copy(
                            out=Vd[:, tt, :, 0:64], in_=pv[:, 0:128].rearrange("p (a b) -> p a b", a=2)),
                            r=[pvk], w=['Vd'])
                    QTm = SB(sc, "dQTm", [128, 4, T], BF16)
                    for c in range(2):
                        P.op('dve', lambda e, c=c: e.memset(QTm[64:128, 2 * c, :], 0.0), w=['dQTm'])
                        P.op('dve', lambda e, c=c: e.memset(QTm[0:64, 2 * c + 1, :], 0.0), w=['dQTm'])
                    for c in range(2):
                        rope_norm_chunk(wD, c, 2 + c, gq[:, 0:1], gq[:, 1:2], blk1b[:], 64,
                                        (QTm[:, 2 * c, :], QTm[:, 2 * c + 1, :]), 'dQTm', tmp)
                        rope_norm_chunk(wD, 4 + c, 6 + c, gq[:, 2:3], gq[:, 3:4], blk1b[:], 64, KT[:, c, :], 'dKT', tmp)
                    precast(l, 1)
                    attention(sc, "d", 4, 64,
                              lambda h: QTm[:, h, :],
                              lambda h: KT[:, h // 2, :],
                              lambda h, kt: Vd[:, kt, h // 2, :],
                              ['dQTm', 'dKT', 'Vd'], yT, 64 ** -0.5)
                    P.flush()
                merge(3, l, yT, mergedT, False, pre=lambda: ld_B(l))

                with ExitStack() as sc:
                    ld_M(l, 1)
                    wB = VB['wB']
                    wuq = VB['wuq']
                    wkk = VB['wkk']
                    wkv = VB['wkv']
                    qg = SB(sc, "qg", [128, 2])
                    kvg = SB(sc, "kvg", [128, 1])
                    mqnR = Rot("mqn", [SB(sc, f"mqn{i}", [128, 2, 512], BF16) for i in range(2)])
                    mkvnR = Rot("mkvn", [SB(sc, f"mkvn{i}", [128, 512], BF16) for i in range(2)])
                    QT = SB(sc, "bQT", [128, 4, T], BF16)
                    KT = SB(sc, "bKT", [128, 4, T], BF16)
                    Vm = SB(sc, "Vm", [128, NT, 4, 128], BF16)
                    tbC = Rot("mbC", [SB(sc, f"mbC{i}", [128, 512]) for i in range(1)])
                    tbS = Rot("mbS", [SB(sc, f"mbS{i}", [128, 512]) for i in range(1)])
                    sq = [SB(sc, f"bsq{i}", [128, 512], BF16) for i in range(2)]
                    rsbR = Rot("brsb", [SB(sc, f"brsb{i}", [128, 512]) for i in range(2)])
                    t1R = Rot("bt1", [SB(sc, f"bt1{i}", [128, 512]) for i in range(2)])
                    t2R = Rot("bt2", [SB(sc, f"bt2{i}", [128, 512]) for i in range(2)])
                    fload(qg[:], mla_qg[l], "qg")
                    fload(kvg[:], mla_kvg[l], "kvg")
                    P.op('pool', lambda e: e.memset(Vm[:, :, :, 64:128], 1.0), w=['Vm'])

                    def rsb_chain(pss, pssk, n, nfeat):
                        rsb, rk = rsbR.next()
                        P.op('dve', lambda e: e.tensor_scalar(out=rsb[:, 0:n], in0=pss[:, 0:n], scalar1=1.0 / nfeat,
                                                              scalar2=EPS, op0=ALU.mult, op1=ALU.add),
                             r=[pssk], w=[rk])
                        P.op('act', lambda e: e.activation(out=rsb[:, 0:n], in_=rsb[:, 0:n], func=AF.Sqrt),
                             r=[rk], w=[rk])
                        P.op('dve', lambda e: e.reciprocal(out=rsb[:, 0:n], in_=rsb[:, 0:n]), r=[rk], w=[rk])
                        return rsb, rk

                    def bblk(b, t0, n):
                        mqn, mqk = mqnR.next()
                        mkvn, mkk = mkvnR.next()
                        for c in range(2):
                            for kc in range(8):
                                P.op('pe', lambda e, c=c, kc=kc: e.matmul(
                                    ps[c][:, 0:n], lhsT=wB[:, kc, c * 128:(c + 1) * 128], rhs=hnT[:, kc, t0:t0 + n],
                                    start=(kc == 0), stop=(kc == 7)), r=['wB'] + hk(b), w=[PSK[c]])
                            P.op('act', lambda e, c=c: e.activation(out=sq[c][:, 0:n], in_=ps[c][:, 0:n],
                                                                    func=AF.Square), r=[PSK[c]], w=[f"bsq{c}"])
                        for c in range(2):
                            P.op('pe', lambda e, c=c: e.matmul(ps[2][:, 0:n], lhsT=onesb[:], rhs=sq[c][:, 0:n],
                                                               start=(c == 0), stop=(c == 1)),
                                 r=[f"bsq{c}", 'onesb'], w=[PSK[2]])
                        rsb, rk = rsb_chain(ps[2], PSK[2], n, 256)
                        for c in range(2):
                            P.op('dve', lambda e, c=c: e.scalar_tensor_tensor(
                                out=mqn[:, c, 0:n], in0=ps[c][:, 0:n], scalar=qg[:, c:c + 1], in1=rsb[:, 0:n],
                                op0=ALU.mult, op1=ALU.mult), r=[PSK[c], 'qg', rk], w=[mqk])
                        for kc in range(8):
                            P.op('pe', lambda e, kc=kc: e.matmul(
                                ps[3][:, 0:n], lhsT=wB[:, kc, 256:384], rhs=hnT[:, kc, t0:t0 + n],
                                start=(kc == 0), stop=(kc == 7)), r=['wB'] + hk(b), w=[PSK[3]])
                        P.op('act', lambda e: e.activation(out=sq[0][:, 0:n], in_=ps[3][:, 0:n], func=AF.Square),
                             r=[PSK[3]], w=["bsq0"])
                        P.op('pe', lambda e: e.matmul(ps[2][:, 0:n], lhsT=onesb[:], rhs=sq[0][:, 0:n], start=True,
                                                      stop=True), r=["bsq0", 'onesb'], w=[PSK[2]])
                        rsb2, rk2 = rsb_chain(ps[2], PSK[2], n, 128)
                        P.op('dve', lambda e: e.scalar_tensor_tensor(
                            out=mkvn[:, 0:n], in0=ps[3][:, 0:n], scalar=kvg[:, 0:1], in1=rsb2[:, 0:n],
                            op0=ALU.mult, op1=ALU.mult), r=[PSK[3], 'kvg', rk2], w=[mkk])
                        tc_, tck = tbC.next()
                        ts_, tsk = tbS.next()
                        fload(tc_[:, 0:n], ropeM[0, :, t0:t0 + n], tck)
                        fload(ts_[:, 0:n], ropeM[1, :, t0:t0 + n], tsk)
                        for (pi, blk) in ((4, 3), (5, 4)):
                            for kc in range(8):
                                P.op('pe', lambda e, pi=pi, blk=blk, kc=kc: e.matmul(
                                    ps[pi][:, 0:n], lhsT=wB[:, kc, blk * 128:(blk + 1) * 128],
                                    rhs=hnT[:, kc, t0:t0 + n], start=(kc == 0), stop=(kc == 7)),
                                    r=['wB'] + hk(b), w=[PSK[pi]])
                        t1, t1k = t1R.next()
                        t2, t2k = t2R.next()
                        P.op('dve', lambda e, tc_=tc_: e.tensor_tensor(out=t1[64:96, 0:n], in0=ps[4][64:96, 0:n],
                                                                       in1=tc_[64:96, 0:n], op=ALU.mult),
                             r=[PSK[4], tck], w=[t1k])
                        P.op('dve', lambda e, ts_=ts_: e.tensor_tensor(out=t2[64:96, 0:n], in0=ps[5][64:96, 0:n],
                                                                       in1=ts_[64:96, 0:n], op=ALU.mult),
                             r=[PSK[5], tsk], w=[t2k])
                        P.op('pool', lambda e: e.tensor_tensor(
                            out=t1[64:96, 0:n], in0=t1[64:96, 0:n], in1=t2[64:96, 0:n], op=ALU.add),
                            r=[t1k, t2k], w=[t1k])
                        for h in range(4):
                            P.op('act', lambda e, h=h: e.copy(out=KT[64:96, h, t0:t0 + n], in_=t1[64:96, 0:n]),
                                 r=[t1k], w=['bKT'])
                        for h in range(4):
                            pi = 6 + h % 2
                            P.op('pe', lambda e, h=h, pi=pi: e.matmul(
                                ps[pi][0:64, 0:n], lhsT=wkk[:, h * 64:(h + 1) * 64], rhs=mkvn[:, 0:n],
                                start=True, stop=True), r=['wkk', mkk], w=[PSK[pi]])
                            P.op('act', lambda e, h=h, pi=pi: e.copy(out=KT[0:64, h, t0:t0 + n],
                                                                     in_=ps[pi][0:64, 0:n]), r=[PSK[pi]], w=['bKT'])
                        for q_ in range(n // 128):
                            tt = t0 // 128 + q_
                            pv, pvk = ps[6 + tt % 2], PSK[6 + tt % 2]
                            P.op('pe', lambda e, pv=pv, q_=q_: e.matmul(
                                pv[:, 0:256], lhsT=mkvn[:, q_ * 128:(q_ + 1) * 128], rhs=wkv, start=True, stop=True),
                                r=['wkv', mkk], w=[pvk])
                            P.op('act', lambda e, pv=pv, tt=tt: e.copy(
                                out=Vm[:, tt, :, 0:64], in_=pv[:, 0:256].rearrange("p (a b) -> p a b", a=4)),
                                r=[pvk], w=['Vm'])
                        def qhead(h):
                            pa, pb_ = (h % 2) * 2, (h % 2) * 2 + 1
                            for (pi, sw) in ((pa, 0), (pb_, 1)):
                                for c in range(2):
                                    P.op('pe', lambda e, pi=pi, sw=sw, c=c: e.matmul(
                                        ps[pi][0:96, 0:n], lhsT=wuq[:, c, (h * 2 + sw) * 96:(h * 2 + sw + 1) * 96],
                                        rhs=mqn[:, c, 0:n], start=(c == 0), stop=(c == 1)),
                                        r=['wuq', mqk], w=[PSK[pi]])
                            u1, u1k = t1R.next()
                            u2, u2k = t2R.next()
                            P.op('act', lambda e: e.copy(out=QT[0:64, h, t0:t0 + n], in_=ps[pa][0:64, 0:n]),
                                 r=[PSK[pa]], w=['bQT'])
                            P.op('dve', lambda e: e.tensor_tensor(
                                out=u1[64:96, 0:n], in0=ps[pa][64:96, 0:n], in1=tc_[64:96, 0:n], op=ALU.mult),
                                r=[PSK[pa], tck], w=[u1k])
                            P.op('dve', lambda e: e.tensor_tensor(
                                out=u2[64:96, 0:n], in0=ps[pb_][64:96, 0:n], in1=ts_[64:96, 0:n], op=ALU.mult),
                                r=[PSK[pb_], tsk], w=[u2k])
                            P.op('pool', lambda e: e.tensor_tensor(
                                out=QT[64:96, h, t0:t0 + n], in0=u1[64:96, 0:n], in1=u2[64:96, 0:n], op=ALU.add),
                                r=[u1k, u2k], w=['bQT'])
                        for h in range(4):
                            qhead(h)
                    for b, (t0, n) in enumerate(BLOCKS):
                        bblk(b, t0, n)
                    precast(l, 2)
                    attention(sc, "b", 4, 96,
                              lambda h: QT[0:96, h, :], lambda h: KT[0:96, h, :],
                              lambda h, kt: Vm[:, kt, h, :],
                              ['bQT', 'bKT', 'Vm'], yT, 96 ** -0.5)
                    P.flush()
                merge(1, l, yT, mergedT, False, pre=lambda: ld_O(l))

                with ExitStack() as sc:
                    wo = VO['wo']
                    gb = gate_bcast(sc, l, 2, "g1b")
                    hin = Rot("oh", [SB(sc, f"oh{i}", [128, D]) for i in range(3)])
                    tmo = Rot("ot", [SB(sc, f"ot{i}", [128, D]) for i in range(2)])
                    for tt in range(NT):
                        s = 1 if tt < 2 else 0
                        b = [i for i, (t0, n) in enumerate(BLOCKS) if t0 <= tt * 128 < t0 + n][0]
                        h_, hk_ = hin.next()
                        P.dma('sp', lambda e, h_=h_, tt=tt: e.dma_start(out=h_[:], in_=(hs0v if l == 0 else hsv)[tt]),
                              hk_, r=[f"hs{tt}"], w=[hk_])
                        t_, tk = tmo.next()
                        for h2 in range(2):
                            pi = 4 + (tt % 2) * 2 + h2
                            for kc in range(8):
                                P.op('pe', lambda e, pi=pi, kc=kc, tt=tt, h2=h2: e.matmul(
                                    ps[pi][:, :], lhsT=mergedT[:, kc, tt * 128:(tt + 1) * 128],
                                    rhs=wo[:, kc, h2 * 512:(h2 + 1) * 512], start=(kc == 0), stop=(kc == 7)),
                                    r=[f"wo{h2}", f"mg{b}"], w=[PSK[pi]])
                            P.op('dve', lambda e, pi=pi, t_=t_, h2=h2, s=s: e.tensor_tensor(
                                out=t_[:, h2 * 512:(h2 + 1) * 512], in0=ps[pi][:, :], in1=gb[:, s, h2 * 512:(h2 + 1) * 512],
                                op=ALU.mult), r=[PSK[pi], 'g1b'], w=[tk])
                        P.op('dve', lambda e, h_=h_, t_=t_: e.tensor_tensor(out=h_[:], in0=h_[:], in1=t_[:], op=ALU.add),
                             r=[hk_, tk], w=[hk_])
                        P.dma('sp', lambda e, h_=h_, tt=tt: e.dma_start(out=hsv[tt], in_=h_[:]), 'S' + hk_,
                              r=[hk_], w=[f"hs{tt}"])
                    P.flush()

            tt0 = 2 if last else 0
            with ExitStack() as mo:
                hntok = SB(mo, "hntok", [128, NT, D], BF16)
                norm_phase(l, 3, 4, hntok=hntok)
                w12 = SB(mo, "w12", [128, 2, NT])
                desti = SB(mo, "desti", [128, NT, 2], I32)
                idn = SB(mo, "idn", [128, NB], I32)
                with ExitStack() as sc:
                    wr = SB(sc, "wr", [128, 8, 36], BF16)
                    brb = SB(sc, "brb", [128, 36])
                    Lg = SB(sc, "Lg", [128, NT, 36])
                    oh = SB(sc, "oh", [128, NT, 4])
                    lm = SB(sc, "lm", [128, NT, 32])
                    lm2 = SB(sc, "lm2", [128, NT, 32])
                    mk1 = SB(sc, "mk1", [128, NT, 32])
                    mk2 = SB(sc, "mk2", [128, NT, 32])
                    Ab = SB(sc, "Ab", [128, NT, 32], BF16)
                    RK = SB(sc, "RK", [128, NT, 32])
                    r1 = SB(sc, "r1", [128, 8, NT])
                    Cn = SB(sc, "Cn", [128, 32])
                    nbk = SB(sc, "nbk", [128, 32])
                    pend = SB(sc, "pend", [128, 32])
                    pst = SB(sc, "pst", [128, 32])
                    zer = SB(sc, "zer", [128, 32])
                    cmp1 = SB(sc, "cmp1", [128, 32, 36])
                    cmp2 = SB(sc, "cmp2", [128, NB, 32])
                    thr = SB(sc, "thr", [128, 36])
                    thri = SB(sc, "thri", [128, 36], I32)
                    bvl = SB(sc, "bvl", [128, NB])
                    bvli = SB(sc, "bvli", [128, NB], I32)
                    pidx = SB(sc, "pidx", [128, 4])
                    pidxi = SB(sc, "pidxi", [128, 4], I32)
                    pci = SB(sc, "pci", [128, 1], I32)
                    pcf = SB(sc, "pcf", [128, 1])
                    bke = SB(sc, "bke", [128, NB])
                    tmpf = SB(sc, "tmpf", [128, 2, NB])
                    destf = SB(sc, "destf", [128, NT, 2])
                    Um = SB(sc, "Um", [128, 128], BF16)
                    Uf = SB(sc, "Uf", [128, 128])
                    wload(wr[:], w_r[l].rearrange("(kc p) n -> p kc n", p=128), "wr")
                    fload(brb[:], b_r[l].partition_broadcast(128), "brb")
                    P.op('pool', lambda e: e.memset(Uf[:], 1.0), w=['Uf'])
                    P.op('pool', lambda e: e.affine_select(out=Uf[:], in_=Uf[:], pattern=[[1, 128]], base=0,
                                                            channel_multiplier=-1, compare_op=ALU.is_gt, fill=0.0),
                         r=['Uf'], w=['Uf'])
                    P.op('pool', lambda e: e.tensor_copy(out=Um[:], in_=Uf[:]), r=['Uf'], w=['Um'])
                    P.op('pool', lambda e: e.iota(thri[:], pattern=[[128, 36]], base=0, channel_multiplier=0), w=['thri'])
                    P.op('pool', lambda e: e.iota(bvli[:], pattern=[[1, NB]], base=0, channel_multiplier=0), w=['bvli'])
                    P.op('pool', lambda e: e.iota(pidxi[:], pattern=[[1, 4]], base=0, channel_multiplier=2), w=['pidxi'])
                    P.op('pool', lambda e: e.memset(zer[:], 0.0), w=['zer'])
                    P.op('pool', lambda e: e.iota(pci[:], pattern=[[0, 1]], base=0, channel_multiplier=1), w=['pci'])
                    P.op('dve', lambda e: e.tensor_copy(out=pcf[:], in_=pci[:]), r=['pci'], w=['pcf'])
                    P.op('dve', lambda e: e.tensor_copy(out=thr[:], in_=thri[:]), r=['thri'], w=['thr'])
                    P.op('dve', lambda e: e.tensor_copy(out=bvl[:], in_=bvli[:]), r=['bvli'], w=['bvl'])
                    P.op('dve', lambda e: e.tensor_copy(out=pidx[:], in_=pidxi[:]), r=['pidxi'], w=['pidx'])
                    for tt in range(NT):
                        pr, prk = ps[tt % 2], PSK[tt % 2]
                        for kc in range(8):
                            P.op('pe', lambda e, pr=pr, kc=kc, tt=tt: e.matmul(
                                pr[:, 0:36], lhsT=hnT[:, kc, tt * 128:(tt + 1) * 128], rhs=wr[:, kc, :],
                                start=(kc == 0), stop=(kc == 7)), r=['wr', f"hnT{tt}"], w=[prk])
                        P.op('dve', lambda e, pr=pr, tt=tt: e.tensor_tensor(out=Lg[:, tt, :], in0=pr[:, 0:36], in1=brb[:],
                                                                           op=ALU.add), r=[prk, 'brb'], w=['Lg'])
                    BIG = 1.0e4
                    mg_, sg_, m1, m2, dd, w1, w2, pgt = [r1[:, i, :] for i in range(8)]
                    R = lambda fn, r, w: P.op('dve', fn, r=r, w=w)
                    bc32 = lambda a: a.unsqueeze(2).broadcast_to([128, NT, 32])
                    R(lambda e: e.tensor_reduce(out=mg_, in_=Lg[:, :, 0:4], axis=AX.X, op=ALU.max), ['Lg'], ['r1a'])
                    R(lambda e: e.tensor_tensor(out=oh[:], in0=Lg[:, :, 0:4],
                                                in1=mg_.unsqueeze(2).broadcast_to([128, NT, 4]), op=ALU.is_ge),
                      ['Lg', 'r1a'], ['oh'])
                    R(lambda e: e.tensor_tensor(out=lm[:, :, 0:4], in0=Lg[:, :, 0:4],
                                                in1=mg_.unsqueeze(2).broadcast_to([128, NT, 4]), op=ALU.subtract),
                      ['Lg', 'r1a'], ['lm'])
                    P.op('act', lambda e: e.activation(out=lm[:, :, 0:4], in_=lm[:, :, 0:4], func=AF.Exp), r=['lm'], w=['lm'])
                    R(lambda e: e.tensor_reduce(out=sg_, in_=lm[:, :, 0:4], axis=AX.X, op=ALU.add), ['lm'], ['r1b'])
                    R(lambda e: e.reciprocal(out=pgt, in_=sg_), ['r1b'], ['r1c'])
                    R(lambda e: e.tensor_scalar(out=oh[:], in0=oh[:], scalar1=-1.0, scalar2=BIG, op0=ALU.add,
                                                op1=ALU.mult), ['oh'], ['oh'])
                    R(lambda e: e.tensor_tensor(
                        out=lm[:].rearrange("p t (g x) -> p t g x", g=4),
                        in0=Lg[:, :, 4:36].rearrange("p t (g x) -> p t g x", g=4),
                        in1=oh[:].unsqueeze(3).broadcast_to([128, NT, 4, 8]), op=ALU.add), ['Lg', 'oh', 'lm'], ['lm'])
                    R(lambda e: e.tensor_reduce(out=m1, in_=lm[:], axis=AX.X, op=ALU.max), ['lm'], ['r1d'])
                    R(lambda e: e.tensor_tensor(out=mk1[:], in0=lm[:], in1=bc32(m1), op=ALU.is_ge), ['lm', 'r1d'], ['mk1'])
                    R(lambda e: e.scalar_tensor_tensor(out=lm2[:], in0=mk1[:], scalar=-BIG, in1=lm[:], op0=ALU.mult,
                                                       op1=ALU.add), ['mk1', 'lm'], ['lm2'])
                    R(lambda e: e.tensor_reduce(out=m2, in_=lm2[:], axis=AX.X, op=ALU.max), ['lm2'], ['r1e'])
                    R(lambda e: e.tensor_tensor(out=mk2[:], in0=lm2[:], in1=bc32(m2), op=ALU.is_ge), ['lm2', 'r1e'], ['mk2'])
                    R(lambda e: e.tensor_tensor(out=dd, in0=m2, in1=m1, op=ALU.subtract), ['r1d', 'r1e'], ['r1f'])
                    P.op('act', lambda e: e.activation(out=dd, in_=dd, func=AF.Exp), r=['r1f'], w=['r1f'])
                    R(lambda e: e.tensor_scalar(out=w1, in0=dd, scalar1=1.0, scalar2=None, op0=ALU.add), ['r1f'], ['r1g'])
                    R(lambda e: e.reciprocal(out=w1, in_=w1), ['r1g'], ['r1g'])
                    R(lambda e: e.tensor_tensor(out=w12[:, 0, :], in0=w1, in1=pgt, op=ALU.mult), ['r1g', 'r1c'], ['w12'])
                    R(lambda e: e.tensor_tensor(out=w12[:, 1, :], in0=w12[:, 0, :], in1=dd, op=ALU.mult),
                      ['w12', 'r1f'], ['w12'])
                    if last:
                        R(lambda e: e.memset(mk1[:, 0:2, :], 0.0), ['mk1'], ['mk1'])
                        R(lambda e: e.memset(mk2[:, 0:2, :], 0.0), ['mk2'], ['mk2'])
                    R(lambda e: e.tensor_tensor(out=Ab[:], in0=mk1[:], in1=mk2[:], op=ALU.add), ['mk1', 'mk2'], ['Ab'])
                    for j in range(NT):
                        P.op('pe', lambda e, j=j: e.matmul(ps[2][:, 0:32], lhsT=onesb[:], rhs=Ab[:, j, :],
                                                           start=(j == 0), stop=(j == NT - 1)), r=['onesb', 'Ab'], w=[PSK[2]])
                    R(lambda e: e.tensor_copy(out=Cn[:], in_=ps[2][:, 0:32]), [PSK[2]], ['Cn'])
                    R(lambda e: e.tensor_tensor(out=cmp1[:], in0=Cn[:].unsqueeze(2).broadcast_to([128, 32, 36]),
                                                in1=thr[:].unsqueeze(1).broadcast_to([128, 32, 36]), op=ALU.is_gt),
                      ['Cn', 'thr'], ['cmp1'])
                    R(lambda e: e.tensor_reduce(out=nbk[:], in_=cmp1[:], axis=AX.X, op=ALU.add), ['cmp1'], ['nbk'])
                    R(lambda e: e.tensor_tensor_scan(out=pend[:], data0=nbk[:], data1=zer[:], initial=0.0,
                                                     op0=ALU.add, op1=ALU.add), ['nbk', 'zer'], ['pend'])
                    R(lambda e: e.tensor_tensor(out=pst[:], in0=pend[:], in1=nbk[:], op=ALU.subtract), ['pend', 'nbk'], ['pst'])
                    R(lambda e: e.tensor_scalar(out=pst[:], in0=pst[:], scalar1=128.0, scalar2=None, op0=ALU.mult),
                      ['pst'], ['pst'])
                    R(lambda e: e.tensor_tensor(out=cmp2[:], in0=pend[:].unsqueeze(1).broadcast_to([128, NB, 32]),
                                                in1=bvl[:].unsqueeze(2).broadcast_to([128, NB, 32]), op=ALU.is_le),
                      ['pend', 'bvl'], ['cmp2'])
                    R(lambda e: e.tensor_reduce(out=bke[:], in_=cmp2[:], axis=AX.X, op=ALU.add), ['cmp2'], ['bke'])
                    R(lambda e: e.tensor_scalar(out=bke[:], in0=bke[:], scalar1=31.0, scalar2=None, op0=ALU.min),
                      ['bke'], ['bke'])
                    R(lambda e: e.tensor_scalar(out=tmpf[:, 0, :], in0=bke[:], scalar1=128.0, scalar2=pcf[:, 0:1],
                                                op0=ALU.mult, op1=ALU.add), ['bke', 'pcf'], ['tmpf'])
                    R(lambda e: e.tensor_copy(out=idn[:], in_=tmpf[:, 0, :]), ['tmpf'], ['idn'])
                    for i in range(NT):
                        pk_ = 3 + i % 5
                        P.op('pe', lambda e, i=i, pk_=pk_: e.matmul(ps[pk_][:, 0:32], lhsT=Um[:], rhs=Ab[:, i, :],
                                                                    start=True, stop=(i == 0)),
                             r=['Um', 'Ab'], w=[PSK[pk_]])
                        for j in range(i):
                            P.op('pe', lambda e, i=i, j=j, pk_=pk_: e.matmul(
                                ps[pk_][:, 0:32], lhsT=onesb[:], rhs=Ab[:, j, :], start=False, stop=(j == i - 1)),
                                r=['onesb', 'Ab'], w=[PSK[pk_]])
                        R(lambda e, i=i, pk_=pk_: e.tensor_tensor(out=RK[:, i, :], in0=ps[pk_][:, 0:32], in1=pst[:],
                                                                  op=ALU.add), [PSK[pk_], 'pst'], ['RK'])
                    for k, mk in enumerate((mk1, mk2)):
                        R(lambda e, mk=mk: e.tensor_tensor(out=mk[:], in0=mk[:], in1=RK[:], op=ALU.mult),
                          [f"mk{k + 1}", 'RK'], [f"mk{k + 1}"])
                        R(lambda e, mk=mk, k=k: e.tensor_reduce(out=destf[:, :, k], in_=mk[:], axis=AX.X, op=ALU.add),
                          [f"mk{k + 1}"], ['destf'])
                    R(lambda e: e.tensor_copy(out=desti[:], in_=destf[:]), ['destf'], ['desti'])
                    for i in range(tt0, NT):
                        for k in range(2):
                            P.dma('pool', lambda e, i=i, k=k: e.indirect_dma_start(
                                out=xs_d, out_offset=bass.IndirectOffsetOnAxis(ap=desti[:, i, k:k + 1], axis=0),
                                in_=hntok[:, i, :], in_offset=None),
                                'xsc', r=['desti', f"hntok{i}"], w=[f"xsw{i}_{k}"])
                    P.flush()
                with ExitStack() as sc:
                    wgu = Rot("egu", [SB(sc, f"egu{i}", [128, 8, 512], BF16) for i in range(3)])
                    wdn = Rot("edn", [SB(sc, f"edn{i}", [128, 2, D], BF16) for i in range(3)])
                    xsb = Rot("exs", [SB(sc, f"exs{i}", [128, D], BF16) for i in range(3)])
                    xsT = Rot("exT", [SB(sc, f"exT{i}", [128, 8, 128], BF16) for i in range(2)])
                    sgl = Rot("esg", [SB(sc, f"esg{i}", [128, 256]) for i in range(2)])
                    hsb = Rot("ehs", [SB(sc, f"ehs{i}", [128, 256]) for i in range(2)])
                    hT = Rot("ehT", [SB(sc, f"ehT{i}", [128, 2, 128], BF16) for i in range(2)])
                    yb = Rot("eyb", [SB(sc, f"eyb{i}", [128, D]) for i in range(3)])
                    xsv = xs_d.rearrange("(n p) d -> n p d", p=128)
                    ysv = ys_d.rearrange("(n p) d -> n p d", p=128)

                    def eload(b):
                        g_, gk = wgu.next()
                        d_, dk_ = wdn.next()
                        x_, xk = xsb.next()
                        P.dma('pool', lambda e: e.indirect_dma_start(
                            out=g_[:].rearrange("p a b -> p (a b)"), out_offset=None,
                            in_=wgu_bf, in_offset=bass.IndirectOffsetOnAxis(ap=idn[:, b:b + 1], axis=0)),
                            gk, r=['idn'], w=[gk])
                        P.dma('pool', lambda e: e.indirect_dma_start(
                            out=d_[:].rearrange("p a b -> p (a b)"), out_offset=None,
                            in_=wdn_bf, in_offset=bass.IndirectOffsetOnAxis(ap=idn[:, b:b + 1], axis=0)),
                            dk_, r=['idn'], w=[dk_])
                        P.dma('sp', lambda e: e.dma_start(out=x_[:], in_=xsv[b]), xk, w=[xk])
                        return g_, gk, d_, dk_, x_, xk

                    st8 = {}

                    def f1(b):
                        g_, gk, d_, dk_, x_, xk = st8[b]['w']
                        p = b % 2
                        xT_, xTk = xsT.next()
                        for kc in range(8):
                            P.op('pe', lambda e, kc=kc: e.transpose(
                                out=psb0[:, p * 1024 + kc * 128:p * 1024 + (kc + 1) * 128],
                                in_=x_[:, kc * 128:(kc + 1) * 128], identity=identb[:]),
                                r=[xk, 'identb'], w=[PSK[p]])
                        P.op('act', lambda e: e.copy(out=xT_[:, 0:4, :].rearrange("p a b -> p (a b)"),
                                                     in_=psb0[:, p * 1024:p * 1024 + 512]), r=[PSK[p]], w=[xTk])
                        P.op('dve', lambda e: e.tensor_copy(out=xT_[:, 4:8, :].rearrange("p a b -> p (a b)"),
                                                            in_=psb0[:, p * 1024 + 512:p * 1024 + 1024]),
                             r=[PSK[p]], w=[xTk])
                        st8[b]['xT'] = (xT_, xTk)

                    def f3(b):
                        g_, gk, d_, dk_, x_, xk = st8[b]['w']
                        xT_, xTk = st8[b]['xT']
                        p = b % 2
                        pg, pgk = ps[2 + p], PSK[2 + p]
                        for kc in range(8):
                            P.op('pe', lambda e, kc=kc: e.matmul(pg[:, :], lhsT=xT_[:, kc, :], rhs=g_[:, kc, :],
                                                                 start=(kc == 0), stop=(kc == 7)), r=[xTk, gk], w=[pgk])
                        s_, sk = sgl.next()
                        h_, hkey = hsb.next()
                        P.op('act', lambda e: e.activation(out=s_[:], in_=pg[:, 0:256], func=AF.Silu), r=[pgk], w=[sk])
                        P.op('dve', lambda e: e.tensor_tensor(out=h_[:], in0=pg[:, 256:512], in1=s_[:], op=ALU.mult),
                             r=[pgk, sk], w=[hkey])
                        st8[b]['h'] = (h_, hkey)

                    def f2(b):
                        h_, hkey = st8[b]['h']
                        pi0 = 4 + (b % 2) * 2
                        hT_, hTk = hT.next()
                        for c in range(2):
                            P.op('pe', lambda e, c=c: e.transpose(out=ps[pi0][:, c * 128:(c + 1) * 128],
                                                                  in_=h_[:, c * 128:(c + 1) * 128], identity=identf[:]),
                                 r=[hkey, 'identf'], w=[PSK[pi0]])
                        P.op('act', lambda e: e.copy(out=hT_[:].rearrange("p a b -> p (a b)"), in_=ps[pi0][:, 0:256]),
                             r=[PSK[pi0]], w=[hTk])
                        st8[b]['hT'] = (hT_, hTk)

                    def f4(b):
                        g_, gk, d_, dk_, x_, xk = st8[b]['w']
                        hT_, hTk = st8[b]['hT']
                        y_, yk = yb.next()
                        for h2 in range(2):
                            pi = 4 + (b % 2) * 2 + h2
                            for c in range(2):
                                P.op('pe', lambda e, pi=pi, c=c, h2=h2: e.matmul(
                                    ps[pi][:, :], lhsT=hT_[:, c, :], rhs=d_[:, c, h2 * 512:(h2 + 1) * 512],
                                    start=(c == 0), stop=(c == 1)), r=[hTk, dk_], w=[PSK[pi]])
                            if h2 == 0:
                                P.op('act', lambda e, pi=pi: e.copy(out=y_[:, 0:512], in_=ps[pi][:, :]), r=[PSK[pi]], w=[yk])
                            else:
                                P.op('dve', lambda e, pi=pi: e.tensor_copy(out=y_[:, 512:1024], in_=ps[pi][:, :]),
                                     r=[PSK[pi]], w=[yk])
                        P.dma('sp', lambda e: e.dma_start(out=ysv[b], in_=y_[:]), 'S' + yk, r=[yk], w=[f"ys{b}"])
                        del st8[b]
                    nbl = (NB - 4) if last else NB
                    for b in range(min(3, nbl)):
                        st8[b] = dict(w=eload(b))
                    f1(0)
                    f3(0)
                    for i in range(nbl):
                        if i + 1 < nbl:
                            f1(i + 1)
                        f2(i)
                        if i + 1 < nbl:
                            f3(i + 1)
                        f4(i)
                        if i + 3 < nbl:
                            st8[i + 3] = dict(w=eload(i + 3))
                    P.flush()
                with ExitStack() as sc:
                    gb = gate_bcast(sc, l, 5, "g2b")
                    hin = Rot("fh", [SB(sc, f"fh{i}", [128, D]) for i in range(3)])
                    g1r = Rot("fg1", [SB(sc, f"fg1{i}", [128, D]) for i in range(3)])
                    g2r = Rot("fg2", [SB(sc, f"fg2{i}", [128, D]) for i in range(3)])
                    fuse = (l + 1 < L)
                    if fuse:
                        ld_A(l + 1)
                        njunk = SB(sc, "cnjunk", [128, D], BF16)
                        nxn = Rot("cnxn", [SB(sc, f"cnxn{i}", [128, D]) for i in range(2)])
                        nst = SB(sc, "cnst", [128, NT, 2])
                    if last:
                        fgb = SB(sc, "fgb", [128, D])
                        fload(fgb[:], final_gain.partition_broadcast(128), "fgb")
                        fjunk = SB(sc, "fjunk", [128, D], BF16)
                        fst = SB(sc, "fst", [128, NT, 2])
                        outv = out_d.rearrange("(n p) d -> n p d", p=128)

                    def ftile(tt):
                        s = 1 if tt < 2 else 0
                        h_, hk_ = hin.next()
                        a_, ak = g1r.next()
                        b_, bk = g2r.next()
                        P.dma('sp', lambda e: e.dma_start(out=h_[:], in_=hsv[tt]), hk_, r=[f"hs{tt}"], w=[hk_])
                        P.dma('pool', lambda e: e.indirect_dma_start(
                            out=a_[:], out_offset=None, in_=ys_d,
                            in_offset=bass.IndirectOffsetOnAxis(ap=desti[:, tt, 0:1], axis=0)), ak, r=['desti'], w=[ak])
                        P.dma('pool', lambda e: e.indirect_dma_start(
                            out=b_[:], out_offset=None, in_=ys_d,
                            in_offset=bass.IndirectOffsetOnAxis(ap=desti[:, tt, 1:2], axis=0)), bk, r=['desti'], w=[bk])
                        P.op('act', lambda e: e.activation(out=a_[:], in_=a_[:], func=AF.Identity,
                                                           scale=w12[:, 0, tt:tt + 1]), r=[ak, 'w12'], w=[ak])
                        P.op('dve', lambda e: e.scalar_tensor_tensor(out=a_[:], in0=b_[:], scalar=w12[:, 1, tt:tt + 1],
                                                                     in1=a_[:], op0=ALU.mult, op1=ALU.add),
                             r=[ak, bk, 'w12'], w=[ak])
                        P.op('dve', lambda e: e.tensor_tensor(out=a_[:], in0=a_[:], in1=gb[:, s, :], op=ALU.mult),
                             r=[ak, 'g2b'], w=[ak])
                        P.op('dve', lambda e: e.tensor_tensor(out=h_[:], in0=h_[:], in1=a_[:], op=ALU.add),
                             r=[hk_, ak], w=[hk_])
                        if not last:
                            P.dma('sp', lambda e: e.dma_start(out=hsv[tt], in_=h_[:]), 'S' + hk_, r=[hk_], w=[f"hs{tt}"])
                            if fuse:
                                P.op('act', lambda e: e.activation(out=njunk[:], in_=h_[:], func=AF.Square,
                                                                   accum_out=nst[:, tt, 0:1]),
                                     r=[hk_], w=['cnjunk', f"cnst{tt}"])
                                rstd_from_ssq(nst[:, tt, 0:1], nst[:, tt, 1:2], D, [f"cnst{tt}"], f"cnrs{tt}")
                                xo, ko = nxn.next()
                                P.op('act', lambda e: e.activation(out=xo[:], in_=h_[:], func=AF.Identity,
                                                                   scale=nst[:, tt, 1:2]),
                                     r=[hk_, f"cnrs{tt}"], w=[ko])
                                return xo, ko
                        else:
                            P.op('act', lambda e: e.activation(out=fjunk[:], in_=h_[:], func=AF.Square,
                                                               accum_out=fst[:, tt, 0:1]),
                                 r=[hk_], w=['fjunk', f"fst{tt}"])
                            rstd_from_ssq(fst[:, tt, 0:1], fst[:, tt, 1:2], D, [f"fst{tt}"], f"frs{tt}")
                            P.op('dve', lambda e: e.scalar_tensor_tensor(
                                out=h_[:], in0=h_[:], scalar=fst[:, tt, 1:2], in1=fgb[:], op0=ALU.mult, op1=ALU.mult),
                                r=[hk_, f"frs{tt}", 'fgb'], w=[hk_])
                            P.dma('sp', lambda e: e.dma_start(out=outv[tt - 2], in_=h_[:]), 'S' + hk_, r=[hk_],
                                  w=[f"out{tt}"])
                    def ftile2(tt, ctx):
                        if ctx is None:
                            return
                        xo, ko = ctx
                        s = 1 if tt < 2 else 0
                        ln = l + 1
                        for half in range(2):
                            pt = ps[(tt % 2) * 2 + half]
                            pk = PSK[(tt % 2) * 2 + half]
                            for q in range(4):
                                kc = half * 4 + q
                                P.op('pe', lambda e, pt=pt, kc=kc, q=q: e.transpose(
                                    out=pt[:, q * 128:(q + 1) * 128], in_=xo[:, kc * 128:(kc + 1) * 128],
                                    identity=identf[:]), r=[ko, 'identf'], w=[pk])
                            for q in range(4):
                                kc = half * 4 + q
                                if q % 2 == 0:
                                    P.op('act', lambda e, pt=pt, kc=kc, q=q: e.activation(
                                        out=hnT[:, kc, tt * 128:(tt + 1) * 128], in_=pt[:, q * 128:(q + 1) * 128],
                                        func=AF.Identity, scale=modc[:, ln, 1, kc, s:s + 1],
                                        bias=modc[:, ln, 0, kc, s:s + 1]), r=[pk, 'modc'], w=[f"hnT{tt}"])
                                else:
                                    P.op('dve', lambda e, pt=pt, kc=kc, q=q: e.tensor_scalar(
                                        out=hnT[:, kc, tt * 128:(tt + 1) * 128], in0=pt[:, q * 128:(q + 1) * 128],
                                        scalar1=modc[:, ln, 1, kc, s:s + 1], scalar2=modc[:, ln, 0, kc, s:s + 1],
                                        op0=ALU.mult, op1=ALU.add), r=[pk, 'modc'], w=[f"hnT{tt}"])
                    swpipe(range(tt0, NT), ftile, ftile2)
                    P.flush()
        print("ops", len(P.ops), "waits", P.nwaits, "sems", len(P.sems), flush=True)
    return nc


def _rope_tables():
    theta = 10000.0
    rows = 32
    GW = 64

    def ang(rot_dim):
        nf = rot_dim // 4
        freqs = (theta ** (-np.arange(nf, dtype=np.float32) / nf)).astype(np.float32)
        row = np.repeat(np.arange(rows, dtype=np.float32), GW)
        col = (np.arange(rows * GW) % GW).astype(np.float32)
        a = np.concatenate([row[:, None] * freqs, col[:, None] * freqs], axis=-1)
        a = np.concatenate([np.zeros((NCTX, rot_dim // 2), np.float32), a], axis=0)
        return a.astype(np.float32)
    ag = ang(64)
    cg, sg = np.cos(ag).T.astype(np.float32), np.sin(ag).T.astype(np.float32)
    G = np.zeros((2, 128, T), np.float32)
    for h in range(2):
        G[0, h * 64:h * 64 + 32] = cg
        G[0, h * 64 + 32:h * 64 + 64] = cg
        G[1, h * 64:h * 64 + 32] = -sg
        G[1, h * 64 + 32:h * 64 + 64] = sg
    am = ang(32)
    cm, sm = np.cos(am).T.astype(np.float32), np.sin(am).T.astype(np.float32)
    M = np.zeros((2, 128, T), np.float32)
    M[0, 64:80] = cm
    M[0, 80:96] = cm
    M[1, 64:80] = -sm
    M[1, 80:96] = sm
    return G, M


def _prep_weights(inp):
    f = lambda a: np.ascontiguousarray(np.asarray(a, dtype=np.float32))
    Lr = DEPTH
    w_in = f(inp['w_in'])
    sw64 = np.concatenate([np.arange(32, 64), np.arange(0, 32)])
    sw32 = np.concatenate([np.arange(16, 32), np.arange(0, 16)])
    cols = []
    cols += list(range(0, 512))
    cols += list(range(2080, 2208))
    cols += list(range(512, 768))
    cols += list(range(768, 896))
    cols += list(np.tile(np.arange(896, 928), 4))
    cols += list(np.tile(896 + sw32, 4))
    cols += list(range(928, 1696))
    cols += list(range(1696, 1952))
    for h in range(4):
        cols += list(1696 + h * 64 + sw64)
    for g in range(2):
        cols += list(range(1952 + g * 64, 1952 + (g + 1) * 64)) * 2
    for g in range(2):
        cols += list(1952 + g * 64 + sw64) * 2
    cols = np.asarray(cols)
    assert cols.shape[0] == 3072
    w_inx = f(w_in[:, :, cols])
    w_uq = f(inp['w_uq'])
    uq_cols = []
    for h in range(4):
        base = h * 96
        uq_cols += list(range(base, base + 96))
        uq_cols += list(range(base, base + 64)) + list(base + 64 + sw32)
    w_uqx = f(w_uq[:, :, np.asarray(uq_cols)])
    w_ukv = f(inp['w_ukv']).reshape(Lr, 128, 4, 128)
    w_ukvk = f(w_ukv[:, :, :, :64].reshape(Lr, 128, 256))
    w_ukvv = f(w_ukv[:, :, :, 64:].reshape(Lr, 128, 256))
    gq = f(inp['gqa_q_gain'])
    gk = f(inp['gqa_k_gain'])
    gqag = np.stack([np.tile(gq, (1, 2)), np.tile(gq[:, sw64], (1, 2)), np.tile(gk, (1, 2)),
                     np.tile(gk[:, sw64], (1, 2))], axis=-1)
    d = dict(
        w_ada=f(inp['w_ada']),
        b_adac=f(f(inp['b_ada']).reshape(Lr, 48, 128).transpose(0, 2, 1)),
        w_inx=w_inx,
        sgu_wT=f(f(inp['w_sgu']).transpose(0, 3, 1, 2)),
        sgu_b=f(f(inp['b_sgu']).transpose(0, 2, 1)),
        sgu_gain=f(inp['sgu_gain']),
        mla_qg=f(f(inp['mla_q_gain']).reshape(Lr, 2, 128).transpose(0, 2, 1)),
        mla_kvg=f(f(inp['mla_kv_gain']).reshape(Lr, 128, 1)),
        w_uqx=w_uqx, w_ukvk=w_ukvk, w_ukvv=w_ukvv,
        convw=f(f(inp['w_conv']).reshape(Lr, 3, 2, 128).transpose(0, 3, 2, 1)),
        gqag=f(gqag),
        w_gate=f(inp['w_gate']),
        b_gatec=f(f(inp['b_gate']).reshape(Lr, 4, 8, 128).transpose(0, 3, 1, 2)),
        w_branch=f(inp['w_branch']),
        w_out=f(inp['w_out']),
        w_r=f(np.concatenate([f(inp['w_group_router']), f(inp['w_expert_router'])], axis=-1)),
        b_r=f(np.concatenate([f(inp['b_group_router']), f(inp['b_expert_router'])], axis=-1)),
        w_gu_r=f(f(inp['w_expert_gate_up']).reshape(Lr, 32, 8, 128, 512).transpose(0, 1, 3, 2, 4)
                 .reshape(Lr, 32 * 128, 4096)),
        w_dn_r=f(f(inp['w_expert_down']).reshape(Lr, 32, 2, 128, D).transpose(0, 1, 3, 2, 4).reshape(Lr, 32 * 128, 2048)),
    )
    return d


PER_LAYER = ['w_ada', 'b_adac', 'w_inx', 'sgu_wT', 'sgu_b', 'sgu_gain', 'mla_qg', 'mla_kvg', 'w_uqx', 'w_ukvk',
             'w_ukvv', 'convw', 'gqag', 'w_gate', 'b_gatec', 'w_branch', 'w_out', 'w_r', 'b_r', 'w_gu_r', 'w_dn_r']

_CACHE = {}


def _get_prog(n_layers, final):
    key = (n_layers, final)
    if key not in _CACHE:
        _CACHE[key] = build(n_layers, final)
    return _CACHE[key]


def run_layers(hs_list, cvecs, wd, G, M, fg, l0, n_layers, final, cores):
    nc = _get_prog(n_layers, final)
    in_maps = []
    for i in range(len(cores)):
        m = {k: np.ascontiguousarray(wd[k][l0:l0 + n_layers]) for k in PER_LAYER}
        m.update(hs0=hs_list[i], cvec=cvecs[i], final_gain=fg, ropeG=G, ropeM=M)
        in_maps.append(m)
    res = run_bass_kernel_spmd(nc, in_maps, core_ids=list(cores))
    return [r["out" if final else "hs_out"] for r in res.results]


FUSED = True


def kernel(**inp):
    x = np.asarray(inp['x'], np.float32)
    c = np.asarray(inp['c'], np.float32)
    ctx = np.asarray(inp['ctx'], np.float32)
    c_ctx = np.asarray(inp['c_ctx'], np.float32)
    B = x.shape[0]
    wd = _prep_weights(inp)
    G, M = _rope_tables()
    fg = np.ascontiguousarray(np.asarray(inp['final_gain'], np.float32))
    hs = [np.ascontiguousarray(np.concatenate([ctx[b], x[b]], axis=0)) for b in range(B)]
    cvecs = [np.ascontiguousarray(np.stack([c[b].reshape(8, 128).T, c_ctx.reshape(8, 128).T], axis=-1)) for b in range(B)]
    cores = list(range(B))
    if FUSED:
        outs = run_layers(hs, cvecs, wd, G, M, fg, 0, DEPTH, True, cores)
    else:
        for l in range(DEPTH - 1):
            hs = run_layers(hs, cvecs, wd, G, M, fg, l, 1, False, cores)
        outs = run_layers(hs, cvecs, wd, G, M, fg, DEPTH - 1, 1, True, cores)
    return np.stack(outs, axis=0).astype(np.float32)
```

```python
import numpy as np
from contextlib import ExitStack
import concourse.bass as bass
import concourse.mybir as mybir
from concourse.bass_utils import run_bass_kernel_spmd

F32 = mybir.dt.float32
I32 = mybir.dt.int32
BF16 = mybir.dt.bfloat16
AF = mybir.ActivationFunctionType
ALU = mybir.AluOpType
AX = mybir.AxisListType

ENGS = ('pe', 'act', 'dve', 'pool', 'sp')
ENGOBJ = {'pe': 'tensor', 'act': 'scalar', 'dve': 'vector', 'pool': 'gpsimd', 'sp': 'sync'}

D = 1024
T = 2304
NCTX = 256
NT = 18
DEPTH = 4
EPS = 1e-6
NB = 68
CAP = NB * 128
BLOCKS = [(0, 256), (256, 512), (768, 512), (1280, 512), (1792, 512)]


STRICT = True


class Prog:
    def __init__(self, nc, st):
        self.nc = nc
        self.st = st
        self.ops = []
        self.buf = {}
        self.sems = {}
        self.cnt = {}
        self.val = {}
        self.seen = {e: {} for e in ENGS}
        self.flushed = 0
        self.nwaits = 0

    def _deps(self, eng, r, w, is_dma):
        deps = set()
        rw = set()
        for k in r:
            b = self.buf.get(k)
            if b is not None and b[0] is not None:
                deps.add(b[0])
                rw.add(b[0])
        for k in w:
            b = self.buf.get(k)
            if b is not None:
                if b[0] is not None:
                    deps.add(b[0])
                for ri in b[1].values():
                    deps.add(ri)
        out = []
        for d in deps:
            o = self.ops[d]
            if (not o['dma']) and (not is_dma) and o['eng'] == eng:
                if eng == 'pe':
                    continue
                if (not STRICT) and d not in rw:
                    continue
            out.append(d)
        return out

    def _commit(self, idx, semname, r, w):
        for k in r:
            b = self.buf.setdefault(k, [None, {}])
            b[1][semname] = idx
        for k in w:
            self.buf[k] = [idx, {}]

    def op(self, eng, fn, r=(), w=()):
        deps = self._deps(eng, r, w, False)
        idx = len(self.ops)
        self.ops.append(dict(eng=eng, fn=fn, deps=deps, dma=False, sem='E' + eng, sig=False))
        for d in deps:
            self.ops[d]['sig'] = True
        self._commit(idx, 'E' + eng, r, w)
        return idx

    def dma(self, q, fn, semkey, r=(), w=()):
        deps = self._deps(q, r, w, True)
        idx = len(self.ops)
        self.ops.append(dict(eng=q, fn=fn, deps=deps, dma=True, sem='D' + str(semkey), sig=True))
        for d in deps:
            self.ops[d]['sig'] = True
        self._commit(idx, 'D' + str(semkey), r, w)
        return idx

    def flush(self, final_keys=()):
        nc = self.nc
        ops = self.ops
        lo = self.flushed
        hi = len(ops)
        if lo == hi:
            return
        dma_last = {}
        for i in range(lo, hi):
            if ops[i]['dma']:
                dma_last[ops[i]['sem']] = i
        ops.append(dict(eng='sp', fn=None, deps=list(dma_last.values()), dma=False, sem='Esp', sig=False))
        hi = len(ops)
        for i in range(lo, hi):
            o = ops[i]
            s = o['sem']
            if s not in self.sems:
                self.sems[s] = self.st.enter_context(nc.semaphore(s))
                self.cnt[s] = 0
            if o['fn'] is None:
                continue
            if o['dma']:
                self.cnt[s] += 16
                self.val[i] = self.cnt[s]
            else:
                if o['sig']:
                    self.cnt[s] += 1
                    self.val[i] = self.cnt[s]
        per = {e: [] for e in ENGS}
        for i in range(lo, hi):
            per[ops[i]['eng']].append(i)
        prog = self

        def body(ename):
            def f(e):
                seen = prog.seen[ename]
                for i in per[ename]:
                    o = ops[i]
                    need = {}
                    for d in o['deps']:
                        if d < lo and not ops[d]['dma'] and d not in prog.val:
                            continue
                        if d not in prog.val:
                            continue
                        s = ops[d]['sem']
                        need[s] = max(need.get(s, 0), prog.val[d])
                    for s, v in need.items():
                        if seen.get(s, 0) < v:
                            e.wait_ge(prog.sems[s], v)
                            seen[s] = v
                            prog.nwaits += 1
                    if o['fn'] is None:
                        continue
                    ins = o['fn'](e)
                    if o['dma']:
                        ins.then_inc(prog.sems[o['sem']], 16)
                    elif i in prog.val:
                        ins.then_inc(prog.sems[o['sem']], 1)
            return f

        with nc.Block() as block:
            for ename in ENGS:
                if per[ename]:
                    getattr(block, ENGOBJ[ename])(body(ename))
        self.flushed = hi
        self.buf = {}


class Rot:
    def __init__(self, name, tiles):
        self.name = name
        self.tiles = tiles
        self.i = 0

    def next(self):
        k = self.i % len(self.tiles)
        self.i += 1
        return self.tiles[k], f"{self.name}{k}"


def build(n_layers, final):
    nc = bass.Bass("TRN2", target_bir_lowering=False)
    L = n_layers

    def din(name, shape):
        return nc.dram_tensor(name, list(shape), F32, kind="ExternalInput").ap()

    hs0 = din("hs0", [T, D])
    cvec = din("cvec", [128, 8, 2])
    w_ada = din("w_ada", [L, D, 6 * D])
    b_adac = din("b_adac", [L, 128, 48])
    w_inx = din("w_inx", [L, D, 3072])
    sgu_wT = din("sgu_wT", [L, 128, 4, 128])
    sgu_b = din("sgu_b", [L, 128, 4])
    sgu_gain = din("sgu_gain", [L, 256])
    mla_qg = din("mla_qg", [L, 128, 2])
    mla_kvg = din("mla_kvg", [L, 128, 1])
    w_uqx = din("w_uqx", [L, 256, 768])
    w_ukvk = din("w_ukvk", [L, 128, 256])
    w_ukvv = din("w_ukvv", [L, 128, 256])
    convw = din("convw", [L, 128, 2, 3])
    gqag = din("gqag", [L, 128, 4])
    w_gate = din("w_gate", [L, 4, D, D])
    b_gatec = din("b_gatec", [L, 128, 4, 8])
    w_branch = din("w_branch", [L, 4, 256, D])
    w_out = din("w_out", [L, D, D])
    w_r = din("w_r", [L, D, 36])
    b_r = din("b_r", [L, 36])
    w_gu_r = din("w_gu_r", [L, 32 * 128, 4096])
    w_dn_r = din("w_dn_r", [L, 32 * 128, 2048])
    wgu_bf = nc.dram_tensor("wgu_bf", [32 * 128, 4096], BF16).ap()
    wdn_bf = nc.dram_tensor("wdn_bf", [32 * 128, 2048], BF16).ap()
    final_gain = din("final_gain", [D])
    ropeG = din("ropeG", [2, 128, T])
    ropeM = din("ropeM", [2, 128, T])
    xs_d = nc.dram_tensor("xs_scr", [CAP, D], BF16).ap()
    ys_d = nc.dram_tensor("ys_scr", [CAP, D], F32).ap()
    if final:
        out_d = nc.dram_tensor("out", [T - NCTX, D], F32, kind="ExternalOutput").ap()
        hs_d = nc.dram_tensor("hs_scr", [T, D], F32).ap()
    else:
        hs_d = nc.dram_tensor("hs_out", [T, D], F32, kind="ExternalOutput").ap()

    with ExitStack() as st:
        P = Prog(nc, st)

        uid = [0]

        def SB(scope, name, shape, dt=F32):
            uid[0] += 1
            return scope.enter_context(nc.sbuf_tensor(f"{name}_{uid[0]}", list(shape), dt))

        psbig = [st.enter_context(nc.psum_tensor(f"psb{i}", [128, 1024], F32)) for i in range(4)]
        ps = [psbig[i // 2][:, (i % 2) * 512:(i % 2 + 1) * 512] for i in range(8)]
        PSK = [f"ps{i}" for i in range(8)]
        hnT = SB(st, "hnT", [128, 8, T], BF16)
        identf = SB(st, "identf", [128, 128], F32)
        identb = SB(st, "identb", [128, 128], BF16)
        psb0 = psbig[0][:, :].bitcast(BF16)
        onesb = SB(st, "onesb", [128, 128], BF16)
        blk1b = SB(st, "blk1b", [128, 128], BF16)
        onesf = SB(st, "onesf", [128, 128], F32)
        modc = SB(st, "modc", [128, L, 6, 8, 2], F32)
        sT = SB(st, "sT", [128, 8, 2], F32)

        def hk(b):
            t0, n = BLOCKS[b]
            return [f"hnT{t}" for t in range(t0 // 128, (t0 + n) // 128)]

        def bsl(b):
            t0, n = BLOCKS[b]
            return slice(t0, t0 + n)

        P.op('pool', lambda e: e.memset(identf[:], 0.0), w=['identf'])
        P.op('pool', lambda e: e.affine_select(out=identf[:], in_=identf[:], pattern=[[-1, 128]], base=0,
                                                channel_multiplier=1, compare_op=ALU.not_equal, fill=1.0),
             r=['identf'], w=['identf'])
        P.op('pool', lambda e: e.tensor_copy(out=identb[:], in_=identf[:]), r=['identf'], w=['identb'])
        P.op('pool', lambda e: e.memset(onesb[:], 1.0), w=['onesb'])
        P.op('pool', lambda e: e.memset(onesf[:], 1.0), w=['onesf'])
        P.op('pool', lambda e: e.memset(blk1b[:], 0.0), w=['blk1b'])
        P.op('pool', lambda e: e.memset(blk1b[0:64, 0:64], 1.0), r=['blk1b'], w=['blk1b'])
        P.op('pool', lambda e: e.memset(blk1b[64:128, 64:128], 1.0), r=['blk1b'], w=['blk1b'])

        if True:
            with ExitStack() as sc:
                hs0v = hs0.rearrange("(n p) d -> n p d", p=128)
                hsv = hs_d.rearrange("(n p) d -> n p d", p=128)
                zt = SB(sc, "zt", [128, D], BF16)
                P.op('pool', lambda e: e.memset(zt[:], 0.0), w=['zt'])
                xsv0 = xs_d.rearrange("(n p) d -> n p d", p=128)
                for b in range(NB):
                    P.dma('sp', lambda e, b=b: e.dma_start(out=xsv0[b], in_=zt[:]), 'zinit', r=['zt'], w=[f"xsz{b}"])
                P.dma('sp', lambda e: e.dma_start(out=sT[:], in_=cvec), 'sT', w=['sT'])
                P.op('act', lambda e: e.activation(out=sT[:], in_=sT[:], func=AF.Silu), r=['sT'], w=['sT'])
                wa = Rot("wa", [SB(sc, f"wa{i}", [128, 8, 1024], BF16) for i in range(3)])
                sTb = SB(sc, "sTb", [128, 8, 2], BF16)
                P.op('dve', lambda e: e.tensor_copy(out=sTb[:], in_=sT[:]), r=['sT'], w=['sTb'])
                bad = SB(sc, "bad", [128, L, 48])
                P.dma('sp', lambda e: e.dma_start(out=bad[:], in_=b_adac.rearrange("l p n -> p l n")), 'bad',
                      w=['bad'])
                for l in range(L):
                    for j in range(6):
                        wt, k = wa.next()
                        src = w_ada[l, :, j * 1024:(j + 1) * 1024].rearrange("(kc p) n -> p kc n", p=128)
                        P.dma('pool', lambda e, wt=wt, src=src: e.dma_start(out=wt[:], in_=src), k, w=[k])
                        pk = PSK[j % 2]
                        pt = ps[j % 2]
                        for fo in range(8):
                            for kc in range(8):
                                P.op('pe', lambda e, pt=pt, wt=wt, fo=fo, kc=kc: e.matmul(
                                    pt[:, fo * 2:fo * 2 + 2], lhsT=wt[:, kc, fo * 128:(fo + 1) * 128],
                                    rhs=sTb[:, kc, :], start=(kc == 0), stop=(kc == 7)),
                                    r=[k, 'sTb'], w=[pk])
                        for s in range(2):
                            P.op('dve', lambda e, pt=pt, l=l, j=j, s=s: e.tensor_tensor(
                                out=modc[:, l, j, :, s], in0=pt[:, s:16:2], in1=bad[:, l, j * 8:(j + 1) * 8],
                                op=ALU.add), r=[pk, 'bad'], w=['modc'])
                    for j in (1, 4):
                        P.op('dve', lambda e, l=l, j=j: e.tensor_scalar(
                            out=modc[:, l, j], in0=modc[:, l, j], scalar1=1.0, scalar2=None, op0=ALU.add),
                            r=['modc'], w=['modc'])
                P.flush()

        rawX = SB(st, "rawX", [128, 9216], BF16)
        rawY = SB(st, "rawY", [128, 10240], BF16)
        v8 = lambda buf, a, n: buf[:, a:a + 8 * n].rearrange("p (k n) -> p k n", k=8)
        VA = dict(wA=v8(rawX, 0, 512), wsT=rawX[:, 4096:4608].rearrange("p (k n) -> p k n", k=4))
        VM = dict(wg=v8(rawY, 0, 1024), wb=rawY[:, 8192:10240].rearrange("p (k n) -> p k n", k=2))
        VC = dict(wC=v8(rawX, 0, 768))
        VD = dict(wD=v8(rawX, 0, 1024), wV=v8(rawX, 8192, 128))
        VB = dict(wB=v8(rawX, 0, 640), wuq=rawX[:, 5120:6656].rearrange("p (k n) -> p k n", k=2),
                  wkk=rawX[:, 6656:6912], wkv=rawX[:, 6912:7168])
        VO = dict(wo=v8(rawX, 0, 1024))

        def pl(dst, src, key):
            P.dma('pool', lambda e: e.dma_start(out=dst, in_=src), key, w=[key])

        def winv(l, c0, c1):
            return w_inx[l, :, c0:c1].rearrange("(kc p) n -> p kc n", p=128)

        def ld_A(l):
            pl(VA['wA'], winv(l, 0, 512), "wA")
            pl(VA['wsT'], sgu_wT[l], "wsT")

        def ld_M(l, i):
            for h2 in range(2):
                pl(VM['wg'][:, :, h2 * 512:(h2 + 1) * 512],
                   w_gate[l, i, :, h2 * 512:(h2 + 1) * 512].rearrange("(kc p) n -> p kc n", p=128), f"wg{h2}")
            pl(VM['wb'], w_branch[l, i].rearrange("(kc p) n -> p kc n", p=128), "wb")

        def ld_C(l):
            pl(VC['wC'], winv(l, 1280, 2048), "wC")

        def ld_D(l):
            pl(VD['wD'], winv(l, 2048, 3072), "wD")
            pl(VD['wV'], winv(l, 512, 640), "wV")

        def ld_B(l):
            pl(VB['wB'], winv(l, 640, 1280), "wB")
            pl(VB['wuq'], w_uqx[l].rearrange("(c p) n -> p c n", p=128), "wuq")
            pl(VB['wkk'], w_ukvk[l], "wkk")
            pl(VB['wkv'], w_ukvv[l], "wkv")

        def ld_O(l):
            for h2 in range(2):
                pl(VO['wo'][:, :, h2 * 512:(h2 + 1) * 512],
                   w_out[l, :, h2 * 512:(h2 + 1) * 512].rearrange("(kc p) n -> p kc n", p=128), f"wo{h2}")

        def precast(l, part):
            gsrc = w_gu_r[l].rearrange("r (h c) -> (r h) c", h=2)
            gdst = wgu_bf.rearrange("r (h c) -> (r h) c", h=2)
            jobs = [(gsrc, gdst, i) for i in range(16)] + [(w_dn_r[l], wdn_bf, i) for i in range(8)]
            lo_, hi_ = [(0, 4), (4, 14), (14, 24)][part]
            for j, (src, dst, i) in enumerate(jobs[lo_:hi_]):
                P.dma('pool', lambda e, src=src, dst=dst, i=i: e.dma_start(
                    out=dst[i * 512:(i + 1) * 512, :], in_=src[i * 512:(i + 1) * 512, :]), 'pc', w=[f"pc{part}_{j}"])

        def swpipe(items, stage1, stage2):
            prev = None
            for it in items:
                ctx = stage1(it)
                if prev is not None:
                    stage2(*prev)
                prev = (it, ctx)
            if prev is not None:
                stage2(*prev)

        def rstd_from_ssq(ssq_ap, out_ap, n, keys_r, key_w):
            P.op('dve', lambda e: e.tensor_scalar(out=out_ap, in0=ssq_ap, scalar1=1.0 / n, scalar2=EPS,
                                                  op0=ALU.mult, op1=ALU.add), r=keys_r, w=[key_w])
            P.op('act', lambda e: e.activation(out=out_ap, in_=out_ap, func=AF.Sqrt), r=[key_w], w=[key_w])
            P.op('dve', lambda e: e.reciprocal(out=out_ap, in_=out_ap), r=[key_w], w=[key_w])

        def gate_bcast(sc, l, j, name):
            gb = SB(sc, name, [128, 2, D])
            dgs = [SB(sc, f"{name}dg{i}", [128, 4, 128]) for i in range(2)]
            it = 0
            for s in range(2):
                for half in range(2):
                    dg, dk = dgs[it % 2], f"{name}dg{it % 2}"
                    pt, pk = ps[it % 2], PSK[it % 2]
                    it += 1
                    P.op('dve', lambda e, dg=dg, half=half, s=s: e.tensor_tensor(
                        out=dg[:], in0=identf[:].unsqueeze(1).broadcast_to([128, 4, 128]),
                        in1=modc[:, l, j, half * 4:(half + 1) * 4, s].unsqueeze(2).broadcast_to([128, 4, 128]),
                        op=ALU.mult), r=['identf', 'modc'], w=[dk])
                    P.op('pe', lambda e, pt=pt, dg=dg: e.matmul(pt[:, :], lhsT=onesf[:],
                                                                rhs=dg[:].rearrange("p a b -> p (a b)"),
                                                                start=True, stop=True), r=['onesf', dk], w=[pk])
                    P.op('act', lambda e, pt=pt, half=half, s=s: e.copy(out=gb[:, s, half * 512:(half + 1) * 512],
                                                                        in_=pt[:, :]), r=[pk], w=[name])
            return gb

        def norm_phase(l, jsh, jsc, hntok=None, pre=None, src=None):
            with ExitStack() as sc:
                if pre is not None:
                    pre()
                if hntok is not None:
                    scb = gate_bcast(sc, l, jsc, "nscb")
                    shb = gate_bcast(sc, l, jsh, "nshb")
                xin = Rot("nx", [SB(sc, f"nx{i}", [128, D]) for i in range(3)])
                junk = SB(sc, "njunk", [128, D], BF16)
                xn = Rot("nxn", [SB(sc, f"nxn{i}", [128, D]) for i in range(2)])
                if hntok is not None:
                    htmp = Rot("nht", [SB(sc, f"nht{i}", [128, D]) for i in range(2)])
                st_ = SB(sc, "nst", [128, NT, 2])
                def n1(tt):
                    xt, kx = xin.next()
                    P.dma('sp', lambda e: e.dma_start(out=xt[:], in_=(hsv if src is None else src)[tt]), kx,
                          r=[f"hs{tt}"], w=[kx])
                    P.op('act', lambda e: e.activation(out=junk[:], in_=xt[:], func=AF.Square,
                                                       accum_out=st_[:, tt, 0:1]),
                         r=[kx], w=['njunk', f"nst{tt}"])
                    rstd_from_ssq(st_[:, tt, 0:1], st_[:, tt, 1:2], D, [f"nst{tt}"], f"nrs{tt}")
                    xo, ko = xn.next()
                    P.op('dve', lambda e: e.tensor_scalar(
                        out=xo[:], in0=xt[:], scalar1=st_[:, tt, 1:2], scalar2=None, op0=ALU.mult),
                        r=[kx, f"nrs{tt}"], w=[ko])
                    s = 1 if tt < 2 else 0
                    if hntok is not None:
                        ht_, htk = htmp.next()
                        P.op('dve', lambda e: e.tensor_tensor(
                            out=ht_[:], in0=xo[:], in1=scb[:, s, :], op=ALU.mult),
                            r=[ko, 'nscb'], w=[htk])
                        P.op('dve', lambda e: e.tensor_tensor(
                            out=hntok[:, tt, :], in0=ht_[:], in1=shb[:, s, :], op=ALU.add),
                            r=[htk, 'nshb'], w=[f"hntok{tt}"])
                    return xo, ko

                def n2(tt, ctx):
                    xo, ko = ctx
                    s = 1 if tt < 2 else 0
                    for half in range(2):
                        pt = ps[(tt % 2) * 2 + half]
                        pk = PSK[(tt % 2) * 2 + half]
                        for q in range(4):
                            kc = half * 4 + q
                            P.op('pe', lambda e, pt=pt, kc=kc, q=q: e.transpose(
                                out=pt[:, q * 128:(q + 1) * 128], in_=xo[:, kc * 128:(kc + 1) * 128],
                                identity=identf[:]), r=[ko, 'identf'], w=[pk])
                        for q in range(4):
                            kc = half * 4 + q
                            if q % 2 == 0:
                                P.op('act', lambda e, pt=pt, kc=kc, q=q: e.activation(
                                    out=hnT[:, kc, tt * 128:(tt + 1) * 128], in_=pt[:, q * 128:(q + 1) * 128],
                                    func=AF.Identity, scale=modc[:, l, jsc, kc, s:s + 1],
                                    bias=modc[:, l, jsh, kc, s:s + 1]), r=[pk, 'modc'], w=[f"hnT{tt}"])
                            else:
                                P.op('dve', lambda e, pt=pt, kc=kc, q=q: e.tensor_scalar(
                                    out=hnT[:, kc, tt * 128:(tt + 1) * 128], in0=pt[:, q * 128:(q + 1) * 128],
                                    scalar1=modc[:, l, jsc, kc, s:s + 1], scalar2=modc[:, l, jsh, kc, s:s + 1],
                                    op0=ALU.mult, op1=ALU.add), r=[pk, 'modc'], w=[f"hnT{tt}"])
                swpipe(range(NT), n1, n2)
                P.flush()

        def wload(dst, src, key):
            P.dma('pool', lambda e: e.dma_start(out=dst, in_=src), key, w=[key])

        def fload(dst, src, key):
            P.dma('sp', lambda e: e.dma_start(out=dst, in_=src), key, w=[key])

        def win_view(l, c0, c1):
            return w_inx[l, :, c0:c1].rearrange("(kc p) n -> p kc n", p=128)

        def attention(sc, nm, nheads, dk, QTf, KTf, Vf, qk_keys, yT, scale):
            Pt = Rot(nm + "Pt", [SB(sc, f"{nm}Pt{i}", [128, 2, 512], BF16) for i in range(2)])
            rec = Rot(nm + "rc", [SB(sc, f"{nm}rc{i}", [128, 512]) for i in range(1)])
            def do_blk(h, b, t0, n, po, pok):
                nkt = 2 if b == 0 else NT
                npair = nkt // 2
                qs = QTf(h)[:, t0:t0 + n]

                def smm(j):
                    for a_ in range(2):
                        kt = 2 * j + a_
                        pt, pk = ps[(j % 2) * 2 + a_], PSK[(j % 2) * 2 + a_]
                        P.op('pe', lambda e, pt=pt, kt=kt: e.matmul(
                            pt[:, 0:n], lhsT=KTf(h)[:, kt * 128:(kt + 1) * 128], rhs=qs,
                            start=True, stop=True), r=qk_keys, w=[pk])

                def pv(j):
                    big = psbig[j % 2]
                    pe_, pek = Pt.next()
                    P.op('act', lambda e: e.activation(
                        out=pe_[:, :, 0:n], in_=big[:, :].rearrange("p (a b) -> p a b", a=2)[:, :, 0:n],
                        func=AF.Exp, scale=scale), r=[PSK[(j % 2) * 2], PSK[(j % 2) * 2 + 1]], w=[pek])
                    for a_ in range(2):
                        kt = 2 * j + a_
                        P.op('pe', lambda e, a_=a_, kt=kt: e.matmul(
                            po[:, 0:n], lhsT=Vf(h, kt), rhs=pe_[:, a_, 0:n],
                            start=(kt == 0), stop=(kt == nkt - 1)), r=[pek] + qk_keys, w=[pok])
                smm(0)
                for j in range(npair):
                    if j + 1 < npair:
                        smm(j + 1)
                    pv(j)
                rc, rck = rec.next()
                P.op('dve', lambda e: e.reciprocal(out=rc[64:128, 0:n], in_=po[64:128, 0:n]), r=[pok], w=[rck])
                p0 = (h % 2) * 64
                P.op('dve', lambda e: e.tensor_tensor(
                    out=yT[p0:p0 + 64, h // 2, t0:t0 + n], in0=po[0:64, 0:n], in1=rc[64:128, 0:n],
                    op=ALU.mult), r=[pok, rck], w=[f"yT{b}"])
            it = 0
            for h in range(nheads):
                for b, (t0, n) in enumerate(BLOCKS):
                    do_blk(h, b, t0, n, ps[4 + (it % 2)], PSK[4 + (it % 2)])
                    it += 1

        def merge(i, l, yT, mergedT, first, pre=None):
            with ExitStack() as sc:
                if pre is not None:
                    pre()
                wg = VM['wg']
                wb = VM['wb']
                bg = SB(sc, "bg", [128, 8])
                sg = Rot("sg", [SB(sc, f"sg{i_}", [128, 512]) for i_ in range(2)])
                tm = Rot("tm", [SB(sc, f"tm{i_}", [128, 512]) for i_ in range(2)])
                fload(bg[:], b_gatec[l, :, i, :], "bg")
                def mblk(b, t0, n, fo, pg, pgk, pb, pbk):
                    for kc in range(8):
                        P.op('pe', lambda e, kc=kc: e.matmul(
                            pg[:, 0:n], lhsT=wg[:, kc, fo * 128:(fo + 1) * 128], rhs=hnT[:, kc, t0:t0 + n],
                            start=(kc == 0), stop=(kc == 7)), r=[f"wg{fo // 4}"] + hk(b), w=[pgk])
                    for c in range(2):
                        P.op('pe', lambda e, c=c: e.matmul(
                            pb[:, 0:n], lhsT=wb[:, c, fo * 128:(fo + 1) * 128], rhs=yT[:, c, t0:t0 + n],
                            start=(c == 0), stop=(c == 1)), r=["wb", f"yT{b}"], w=[pbk])
                    s_, sk = sg.next()
                    P.op('act', lambda e: e.activation(
                        out=s_[:, 0:n], in_=pg[:, 0:n], func=AF.Sigmoid, bias=bg[:, fo:fo + 1]),
                        r=[pgk, 'bg'], w=[sk])
                    if first:
                        P.op('dve', lambda e: e.tensor_tensor(
                            out=mergedT[:, fo, t0:t0 + n], in0=pb[:, 0:n], in1=s_[:, 0:n], op=ALU.mult),
                            r=[pbk, sk], w=[f"mg{b}"])
                    else:
                        t_, tk = tm.next()
                        P.op('dve', lambda e: e.tensor_tensor(
                            out=t_[:, 0:n], in0=pb[:, 0:n], in1=s_[:, 0:n], op=ALU.mult),
                            r=[pbk, sk], w=[tk])
                        P.op('pool', lambda e: e.tensor_tensor(
                            out=mergedT[:, fo, t0:t0 + n], in0=mergedT[:, fo, t0:t0 + n], in1=t_[:, 0:n],
                            op=ALU.add), r=[tk, f"mg{b}"], w=[f"mg{b}"])
                it = 0
                for b, (t0, n) in enumerate(BLOCKS):
                    for fo in range(8):
                        mblk(b, t0, n, fo, ps[it % 4], PSK[it % 4], ps[4 + it % 4], PSK[4 + it % 4])
                        it += 1
                P.flush()

        rnc = [0]

        def rope_norm_chunk(wq, blkq, blksw, gcol, gswcol, ssq_lhsT, nfeat, dst, dstkey, tmp):
            (tabC, tabS, sqR, rsbR, t1R, t2R, t3R) = tmp

            def r1(it):
                b, (t0, n) = it
                st_ = (rnc[0] % 2) * 3
                rnc[0] += 1
                pz, pzk = ps[st_], PSK[st_]
                pw, pwk = ps[st_ + 1], PSK[st_ + 1]
                pss, pssk = ps[st_ + 2], PSK[st_ + 2]
                sq, sqk = sqR.next()
                rsb, rsk = rsbR.next()
                for kc in range(8):
                    P.op('pe', lambda e, kc=kc: e.matmul(pz[:, 0:n], lhsT=wq[:, kc, blkq * 128:(blkq + 1) * 128],
                                                         rhs=hnT[:, kc, t0:t0 + n], start=(kc == 0), stop=(kc == 7)),
                         r=['wD'] + hk(b), w=[pzk])
                for kc in range(8):
                    P.op('pe', lambda e, kc=kc: e.matmul(pw[:, 0:n], lhsT=wq[:, kc, blksw * 128:(blksw + 1) * 128],
                                                         rhs=hnT[:, kc, t0:t0 + n], start=(kc == 0), stop=(kc == 7)),
                         r=['wD'] + hk(b), w=[pwk])
                tc_, tck = tabC.next()
                ts_, tsk = tabS.next()
                fload(tc_[:, 0:n], ropeG[0, :, t0:t0 + n], tck)
                fload(ts_[:, 0:n], ropeG[1, :, t0:t0 + n], tsk)
                P.op('act', lambda e: e.activation(out=sq[:, 0:n], in_=pz[:, 0:n], func=AF.Square), r=[pzk], w=[sqk])
                P.op('pe', lambda e: e.matmul(pss[:, 0:n], lhsT=ssq_lhsT, rhs=sq[:, 0:n], start=True, stop=True),
                     r=[sqk, 'blk1b'], w=[pssk])
                P.op('dve', lambda e: e.tensor_scalar(out=rsb[:, 0:n], in0=pss[:, 0:n], scalar1=1.0 / nfeat,
                                                      scalar2=EPS, op0=ALU.mult, op1=ALU.add), r=[pssk], w=[rsk])
                P.op('act', lambda e: e.activation(out=rsb[:, 0:n], in_=rsb[:, 0:n], func=AF.Sqrt), r=[rsk], w=[rsk])
                P.op('dve', lambda e: e.reciprocal(out=rsb[:, 0:n], in_=rsb[:, 0:n]), r=[rsk], w=[rsk])
                return (pz, pzk, pw, pwk, rsb, rsk, tc_, tck, ts_, tsk)

            def r2(it, ctx):
                b, (t0, n) = it
                (pz, pzk, pw, pwk, rsb, rsk, tc_, tck, ts_, tsk) = ctx
                t1, t1k = t1R.next()
                t2, t2k = t2R.next()
                t3, t3k = t3R.next()
                P.op('dve', lambda e: e.scalar_tensor_tensor(out=t1[:, 0:n], in0=pz[:, 0:n], scalar=gcol,
                                                             in1=tc_[:, 0:n], op0=ALU.mult, op1=ALU.mult),
                     r=[pzk, tck, 'gq'], w=[t1k])
                P.op('dve', lambda e: e.scalar_tensor_tensor(out=t2[:, 0:n], in0=pw[:, 0:n], scalar=gswcol,
                                                             in1=ts_[:, 0:n], op0=ALU.mult, op1=ALU.mult),
                     r=[pwk, tsk, 'gq'], w=[t2k])
                P.op('pool', lambda e: e.tensor_tensor(out=t3[:, 0:n], in0=t1[:, 0:n], in1=t2[:, 0:n], op=ALU.add),
                     r=[t1k, t2k], w=[t3k])
                if isinstance(dst, tuple):
                    for hh, d_ in enumerate(dst):
                        P.op('dve', lambda e, hh=hh, d_=d_: e.tensor_tensor(
                            out=d_[hh * 64:(hh + 1) * 64, t0:t0 + n], in0=t3[hh * 64:(hh + 1) * 64, 0:n],
                            in1=rsb[hh * 64:(hh + 1) * 64, 0:n], op=ALU.mult), r=[t3k, rsk], w=[dstkey])
                else:
                    P.op('dve', lambda e: e.tensor_tensor(out=dst[:, t0:t0 + n], in0=t3[:, 0:n], in1=rsb[:, 0:n],
                                                          op=ALU.mult), r=[t3k, rsk], w=[dstkey])
            for it in enumerate(BLOCKS):
                r2(it, r1(it))

        for l in range(L):
            last = final and (l == L - 1)
            if l == 0:
                norm_phase(l, 0, 1, pre=lambda: ld_A(l), src=hs0v)
            with ExitStack() as mx:
                mergedT = SB(mx, "mergedT", [128, 8, T], BF16)
                yT = SB(mx, "yT", [128, 2, T], BF16)

                with ExitStack() as sc:
                    ld_M(l, 0)
                    precast(l, 0)
                    wA = VA['wA']
                    wsT = VA['wsT']
                    bs = SB(sc, "bs", [128, 4])
                    gnb = SB(sc, "gnb", [128, 256])
                    g = Rot("ag", [SB(sc, f"ag{i}", [128, 512]) for i in range(2)])
                    junk = SB(sc, "ajunk", [128, 256], BF16)
                    stA = SB(sc, "stA", [128, NT, 2])
                    vn = Rot("avn", [SB(sc, f"avn{i}", [128, 256], BF16) for i in range(2)])
                    ya = Rot("aya", [SB(sc, f"aya{i}", [128, 256]) for i in range(2)])
                    fload(bs[:], sgu_b[l], "bs")
                    fload(gnb[:], sgu_gain[l].partition_broadcast(128), "gnb")
                    def a1(tt):
                        pa, pak = ps[tt % 2], PSK[tt % 2]
                        for kc in range(8):
                            P.op('pe', lambda e, kc=kc: e.matmul(
                                pa[:, :], lhsT=hnT[:, kc, tt * 128:(tt + 1) * 128], rhs=wA[:, kc, :],
                                start=(kc == 0), stop=(kc == 7)), r=['wA', f"hnT{tt}"], w=[pak])
                        g_, gk = g.next()
                        P.op('act', lambda e: e.activation(out=g_[:], in_=pa[:], func=AF.Gelu_apprx_tanh),
                             r=[pak], w=[gk])
                        P.op('act', lambda e: e.activation(out=junk[:], in_=g_[:, 256:512], func=AF.Square,
                                                           accum_out=stA[:, tt, 0:1]),
                             r=[gk], w=['ajunk', f"stA{tt}"])
                        rstd_from_ssq(stA[:, tt, 0:1], stA[:, tt, 1:2], 256, [f"stA{tt}"], f"rsA{tt}")
                        v_, vk = vn.next()
                        P.op('dve', lambda e: e.scalar_tensor_tensor(
                            out=v_[:], in0=g_[:, 256:512], scalar=stA[:, tt, 1:2], in1=gnb[:], op0=ALU.mult,
                            op1=ALU.mult), r=[gk, f"rsA{tt}", 'gnb'], w=[vk])
                        return g_, gk, v_, vk

                    def a2(tt, ctx):
                        g_, gk, v_, vk = ctx
                        pm, pmk = ps[2 + tt % 2], PSK[2 + tt % 2]
                        pT_, pTk = ps[4 + tt % 2], PSK[4 + tt % 2]
                        for gg in range(4):
                            P.op('pe', lambda e, gg=gg: e.matmul(
                                pm[:, gg * 64:(gg + 1) * 64], lhsT=wsT[:, gg, :], rhs=v_[:, gg * 64:(gg + 1) * 64],
                                start=True, stop=True), r=['wsT', vk], w=[pmk])
                        y_, yk = ya.next()
                        for gg in range(4):
                            P.op('dve', lambda e, gg=gg: e.scalar_tensor_tensor(
                                out=y_[:, gg * 64:(gg + 1) * 64], in0=pm[:, gg * 64:(gg + 1) * 64],
                                scalar=bs[:, gg:gg + 1], in1=g_[:, gg * 64:(gg + 1) * 64], op0=ALU.add, op1=ALU.mult),
                                r=[pmk, gk, 'bs'], w=[yk])
                        for c in range(2):
                            P.op('pe', lambda e, c=c: e.transpose(
                                out=pT_[:, c * 128:(c + 1) * 128], in_=y_[:, c * 128:(c + 1) * 128],
                                identity=identf[:]), r=[yk, 'identf'], w=[pTk])
                        bi = [i for i, (t0, n) in enumerate(BLOCKS) if t0 <= tt * 128 < t0 + n][0]
                        for c in range(2):
                            P.op('act', lambda e, c=c: e.copy(
                                out=yT[:, c, tt * 128:(tt + 1) * 128], in_=pT_[:, c * 128:(c + 1) * 128]),
                                r=[pTk], w=[f"yT{bi}"])
                    swpipe(range(NT), a1, a2)
                    P.flush()
                merge(0, l, yT, mergedT, True, pre=lambda: ld_C(l))

                with ExitStack() as sc:
                    ld_M(l, 2)
                    wC = VC['wC']
                    cw = SB(sc, "cw", [128, 2, 3])
                    U = SB(sc, "cU", [128, T + 3])
                    acc = SB(sc, "cacc", [128, T + 3])
                    CB = SB(sc, "cCB", [128, T + 3], BF16)
                    tx = Rot("ctx", [SB(sc, f"ctx{i}", [128, 512]) for i in range(2)])
                    fload(cw[:], convw[l], "cw")
                    off = lambda t0: t0 + 1 if t0 < NCTX else t0 + 2
                    def cblk(c, b, t0, n):
                        pb_, pc_, px_ = ps[(b % 2) * 3], ps[(b % 2) * 3 + 1], ps[(b % 2) * 3 + 2]
                        kb_, kc_, kx_ = PSK[(b % 2) * 3], PSK[(b % 2) * 3 + 1], PSK[(b % 2) * 3 + 2]
                        for (pp, kk, blk) in ((pb_, kb_, c), (pc_, kc_, 2 + c), (px_, kx_, 4 + c)):
                            for kc in range(8):
                                P.op('pe', lambda e, pp=pp, kc=kc, blk=blk: e.matmul(
                                    pp[:, 0:n], lhsT=wC[:, kc, blk * 128:(blk + 1) * 128],
                                    rhs=hnT[:, kc, t0:t0 + n], start=(kc == 0), stop=(kc == 7)),
                                    r=['wC'] + hk(b), w=[kk])
                        o = off(t0)
                        t_, tk = tx.next()
                        P.op('act', lambda e: e.copy(out=t_[:, 0:n], in_=px_[:, 0:n]), r=[kx_], w=[tk])
                        P.op('act', lambda e: e.copy(out=CB[:, o:o + n], in_=pb_[:, 0:n]), r=[kb_], w=['cCB'])
                        P.op('dve', lambda e: e.tensor_tensor(
                            out=U[:, o:o + n], in0=pc_[:, 0:n], in1=t_[:, 0:n], op=ALU.mult), r=[kc_, tk], w=['cU'])

                    def cout(c, b, t0, n):
                        o = off(t0)
                        P.op('pool', lambda e: e.tensor_tensor(
                            out=yT[:, c, t0:t0 + n], in0=CB[:, o:o + n], in1=acc[:, o:o + n], op=ALU.mult),
                            r=['cCB', 'cacc'], w=[f"yT{b}"])

                    def cchunk(c):
                        P.op('pool', lambda e: e.memset(U[:], 0.0), w=['cU'])
                        for b, (t0, n) in enumerate(BLOCKS):
                            cblk(c, b, t0, n)
                        NN = T + 1
                        P.op('dve', lambda e: e.tensor_scalar(out=acc[:, 1:1 + NN], in0=U[:, 0:NN],
                                                              scalar1=cw[:, c, 0:1], scalar2=None, op0=ALU.mult),
                             r=['cU', 'cw'], w=['cacc'])
                        P.op('dve', lambda e: e.scalar_tensor_tensor(
                            out=acc[:, 1:1 + NN], in0=U[:, 1:1 + NN], scalar=cw[:, c, 1:2], in1=acc[:, 1:1 + NN],
                            op0=ALU.mult, op1=ALU.add), r=['cU', 'cw', 'cacc'], w=['cacc'])
                        P.op('dve', lambda e: e.scalar_tensor_tensor(
                            out=acc[:, 1:1 + NN], in0=U[:, 2:2 + NN], scalar=cw[:, c, 2:3], in1=acc[:, 1:1 + NN],
                            op0=ALU.mult, op1=ALU.add), r=['cU', 'cw', 'cacc'], w=['cacc'])
                        for b, (t0, n) in enumerate(BLOCKS):
                            cout(c, b, t0, n)
                    for c in range(2):
                        cchunk(c)
                    P.flush()
                merge(2, l, yT, mergedT, False, pre=lambda: ld_D(l))

                with ExitStack() as sc:
                    ld_M(l, 3)
                    wD = VD['wD']
                    wV = VD['wV']
                    gq = SB(sc, "gq", [128, 4])
                    Vd = SB(sc, "Vd", [128, NT, 2, 128], BF16)
                    KT = SB(sc, "dKT", [128, 2, T], BF16)
                    tmp = (Rot("tbC", [SB(sc, f"tbC{i}", [128, 512]) for i in range(2)]),
                           Rot("tbS", [SB(sc, f"tbS{i}", [128, 512]) for i in range(2)]),
                           Rot("sq", [SB(sc, f"sq{i}", [128, 512], BF16) for i in range(2)]),
                           Rot("rsb", [SB(sc, f"rsb{i}", [128, 512]) for i in range(2)]),
                           Rot("t1", [SB(sc, f"t1{i}", [128, 512]) for i in range(2)]),
                           Rot("t2", [SB(sc, f"t2{i}", [128, 512]) for i in range(2)]),
                           Rot("t3", [SB(sc, f"t3{i}", [128, 512]) for i in range(2)]))
                    fload(gq[:], gqag[l], "gq")
                    P.op('pool', lambda e: e.memset(Vd[:, :, :, 64:128], 1.0), w=['Vd'])
                    for tt in range(NT):
                        pv, pvk = ps[6 + tt % 2], PSK[6 + tt % 2]
                        for kc in range(8):
                            P.op('pe', lambda e, pv=pv, kc=kc, tt=tt: e.matmul(
                                pv[:, 0:128], lhsT=hnT[:, kc, tt * 128:(tt + 1) * 128], rhs=wV[:, kc, :],
                                start=(kc == 0), stop=(kc == 7)), r=['wV', f"hnT{tt}"], w=[pvk])
                        P.op('act', lambda e, pv=pv, tt=tt: e.copy(
                            out=Vd[:, tt, :, 0:64], in_=pv[:, 0:128].rearrange("p (a b) -> p a b", a=2)),
                            r=[pvk], w=['Vd'])
                    QTm = SB(sc, "dQTm", [128, 4, T], BF16)
                    for c in range(2):
                        P.op('dve', lambda e, c=c: e.memset(QTm[64:128, 2 * c, :], 0.0), w=['dQTm'])
                        P.op('dve', lambda e, c=c: e.memset(QTm[0:64, 2 * c + 1, :], 0.0), w=['dQTm'])
                    for c in range(2):
                        rope_norm_chunk(wD, c, 2 + c, gq[:, 0:1], gq[:, 1:2], blk1b[:], 64,
                                        (QTm[:, 2 * c, :], QTm[:, 2 * c + 1, :]), 'dQTm', tmp)
                        rope_norm_chunk(wD, 4 + c, 6 + c, gq[:, 2:3], gq[:, 3:4], blk1b[:], 64, KT[:, c, :], 'dKT', tmp)
                    precast(l, 1)
                    attention(sc, "d", 4, 64,
                              lambda h: QTm[:, h, :],
                              lambda h: KT[:, h // 2, :],
                              lambda h, kt: Vd[:, kt, h // 2, :],
                              ['dQTm', 'dKT', 'Vd'], yT, 64 ** -0.5)
                    P.flush()
                merge(3, l, yT, mergedT, False, pre=lambda: ld_B(l))

                with ExitStack() as sc:
                    ld_M(l, 1)
                    wB = VB['wB']
                    wuq = VB['wuq']
                    wkk = VB['wkk']
                    wkv = VB['wkv']
                    qg = SB(sc, "qg", [128, 2])
                    kvg = SB(sc, "kvg", [128, 1])
                    mqnR = Rot("mqn", [SB(sc, f"mqn{i}", [128, 2, 512], BF16) for i in range(2)])
                    mkvnR = Rot("mkvn", [SB(sc, f"mkvn{i}", [128, 512], BF16) for i in range(2)])
                    QT = SB(sc, "bQT", [128, 4, T], BF16)
                    KT = SB(sc, "bKT", [128, 4, T], BF16)
                    Vm = SB(sc, "Vm", [128, NT, 4, 128], BF16)
                    tbC = Rot("mbC", [SB(sc, f"mbC{i}", [128, 512]) for i in range(1)])
                    tbS = Rot("mbS", [SB(sc, f"mbS{i}", [128, 512]) for i in range(1)])
                    sq = [SB(sc, f"bsq{i}", [128, 512], BF16) for i in range(2)]
                    rsbR = Rot("brsb", [SB(sc, f"brsb{i}", [128, 512]) for i in range(2)])
                    t1R = Rot("bt1", [SB(sc, f"bt1{i}", [128, 512]) for i in range(2)])
                    t2R = Rot("bt2", [SB(sc, f"bt2{i}", [128, 512]) for i in range(2)])
                    fload(qg[:], mla_qg[l], "qg")
                    fload(kvg[:], mla_kvg[l], "kvg")
                    P.op('pool', lambda e: e.memset(Vm[:, :, :, 64:128], 1.0), w=['Vm'])

                    def rsb_chain(pss, pssk, n, nfeat):
                        rsb, rk = rsbR.next()
                        P.op('dve', lambda e: e.tensor_scalar(out=rsb[:, 0:n], in0=pss[:, 0:n], scalar1=1.0 / nfeat,
                                                              scalar2=EPS, op0=ALU.mult, op1=ALU.add),
                             r=[pssk], w=[rk])
                        P.op('act', lambda e: e.activation(out=rsb[:, 0:n], in_=rsb[:, 0:n], func=AF.Sqrt),
                             r=[rk], w=[rk])
                        P.op('dve', lambda e: e.reciprocal(out=rsb[:, 0:n], in_=rsb[:, 0:n]), r=[rk], w=[rk])
                        return rsb, rk

                    def bblk(b, t0, n):
                        mqn, mqk = mqnR.next()
                        mkvn, mkk = mkvnR.next()
                        for c in range(2):
                            for kc in range(8):
                                P.op('pe', lambda e, c=c, kc=kc: e.matmul(
                                    ps[c][:, 0:n], lhsT=wB[:, kc, c * 128:(c + 1) * 128], rhs=hnT[:, kc, t0:t0 + n],
                                    start=(kc == 0), stop=(kc == 7)), r=['wB'] + hk(b), w=[PSK[c]])
                            P.op('act', lambda e, c=c: e.activation(out=sq[c][:, 0:n], in_=ps[c][:, 0:n],
                                                                    func=AF.Square), r=[PSK[c]], w=[f"bsq{c}"])
                        for c in range(2):
                            P.op('pe', lambda e, c=c: e.matmul(ps[2][:, 0:n], lhsT=onesb[:], rhs=sq[c][:, 0:n],
                                                               start=(c == 0), stop=(c == 1)),
                                 r=[f"bsq{c}", 'onesb'], w=[PSK[2]])
                        rsb, rk = rsb_chain(ps[2], PSK[2], n, 256)
                        for c in range(2):
                            P.op('dve', lambda e, c=c: e.scalar_tensor_tensor(
                                out=mqn[:, c, 0:n], in0=ps[c][:, 0:n], scalar=qg[:, c:c + 1], in1=rsb[:, 0:n],
                                op0=ALU.mult, op1=ALU.mult), r=[PSK[c], 'qg', rk], w=[mqk])
                        for kc in range(8):
                            P.op('pe', lambda e, kc=kc: e.matmul(
                                ps[3][:, 0:n], lhsT=wB[:, kc, 256:384], rhs=hnT[:, kc, t0:t0 + n],
                                start=(kc == 0), stop=(kc == 7)), r=['wB'] + hk(b), w=[PSK[3]])
                        P.op('act', lambda e: e.activation(out=sq[0][:, 0:n], in_=ps[3][:, 0:n], func=AF.Square),
                             r=[PSK[3]], w=["bsq0"])
                        P.op('pe', lambda e: e.matmul(ps[2][:, 0:n], lhsT=onesb[:], rhs=sq[0][:, 0:n], start=True,
                                                      stop=True), r=["bsq0", 'onesb'], w=[PSK[2]])
                        rsb2, rk2 = rsb_chain(ps[2], PSK[2], n, 128)
                        P.op('dve', lambda e: e.scalar_tensor_tensor(
                            out=mkvn[:, 0:n], in0=ps[3][:, 0:n], scalar=kvg[:, 0:1], in1=rsb2[:, 0:n],
                            op0=ALU.mult, op1=ALU.mult), r=[PSK[3], 'kvg', rk2], w=[mkk])
                        tc_, tck = tbC.next()
                        ts_, tsk = tbS.next()
                        fload(tc_[:, 0:n], ropeM[0, :, t0:t0 + n], tck)
                        fload(ts_[:, 0:n], ropeM[1, :, t0:t0 + n], tsk)
                        for (pi, blk) in ((4, 3), (5, 4)):
                            for kc in range(8):
                                P.op('pe', lambda e, pi=pi, blk=blk, kc=kc: e.matmul(
                                    ps[pi][:, 0:n], lhsT=wB[:, kc, blk * 128:(blk + 1) * 128],
                                    rhs=hnT[:, kc, t0:t0 + n], start=(kc == 0), stop=(kc == 7)),
                                    r=['wB'] + hk(b), w=[PSK[pi]])
                        t1, t1k = t1R.next()
                        t2, t2k = t2R.next()
                        P.op('dve', lambda e, tc_=tc_: e.tensor_tensor(out=t1[64:96, 0:n], in0=ps[4][64:96, 0:n],
                                                                       in1=tc_[64:96, 0:n], op=ALU.mult),
                             r=[PSK[4], tck], w=[t1k])
                        P.op('dve', lambda e, ts_=ts_: e.tensor_tensor(out=t2[64:96, 0:n], in0=ps[5][64:96, 0:n],
                                                                       in1=ts_[64:96, 0:n], op=ALU.mult),
                             r=[PSK[5], tsk], w=[t2k])
                        P.op('pool', lambda e: e.tensor_tensor(
                            out=t1[64:96, 0:n], in0=t1[64:96, 0:n], in1=t2[64:96, 0:n], op=ALU.add),
                            r=[t1k, t2k], w=[t1k])
                        for h in range(4):
                            P.op('act', lambda e, h=h: e.copy(out=KT[64:96, h, t0:t0 + n], in_=t1[64:96, 0:n]),
                                 r=[t1k], w=['bKT'])
                        for h in range(4):
                            pi = 6 + h % 2
                            P.op('pe', lambda e, h=h, pi=pi: e.matmul(
                                ps[pi][0:64, 0:n], lhsT=wkk[:, h * 64:(h + 1) * 64], rhs=mkvn[:, 0:n],
                                start=True, stop=True), r=['wkk', mkk], w=[PSK[pi]])
                            P.op('act', lambda e, h=h, pi=pi: e.copy(out=KT[0:64, h, t0:t0 + n],
                                                                     in_=ps[pi][0:64, 0:n]), r=[PSK[pi]], w=['bKT'])
                        for q_ in range(n // 128):
                            tt = t0 // 128 + q_
                            pv, pvk = ps[6 + tt % 2], PSK[6 + tt % 2]
                            P.op('pe', lambda e, pv=pv, q_=q_: e.matmul(
                                pv[:, 0:256], lhsT=mkvn[:, q_ * 128:(q_ + 1) * 128], rhs=wkv, start=True, stop=True),
                                r=['wkv', mkk], w=[pvk])
                            P.op('act', lambda e, pv=pv, tt=tt: e.copy(
                                out=Vm[:, tt, :, 0:64], in_=pv[:, 0:256].rearrange("p (a b) -> p a b", a=4)),
                                r=[pvk], w=['Vm'])
                        def qhead(h):
                            pa, pb_ = (h % 2) * 2, (h % 2) * 2 + 1
                            for (pi, sw) in ((pa, 0), (pb_, 1)):
                                for c in range(2):
                                    P.op('pe', lambda e, pi=pi, sw=sw, c=c: e.matmul(
                                        ps[pi][0:96, 0:n], lhsT=wuq[:, c, (h * 2 + sw) * 96:(h * 2 + sw + 1) * 96],
                                        rhs=mqn[:, c, 0:n], start=(c == 0), stop=(c == 1)),
                                        r=['wuq', mqk], w=[PSK[pi]])
                            u1, u1k = t1R.next()
                            u2, u2k = t2R.next()
                            P.op('act', lambda e: e.copy(out=QT[0:64, h, t0:t0 + n], in_=ps[pa][0:64, 0:n]),
                                 r=[PSK[pa]], w=['bQT'])
                            P.op('dve', lambda e: e.tensor_tensor(
                                out=u1[64:96, 0:n], in0=ps[pa][64:96, 0:n], in1=tc_[64:96, 0:n], op=ALU.mult),
                                r=[PSK[pa], tck], w=[u1k])
                            P.op('dve', lambda e: e.tensor_tensor(
                                out=u2[64:96, 0:n], in0=ps[pb_][64:96, 0:n], in1=ts_[64:96, 0:n], op=ALU.mult),
                                r=[PSK[pb_], tsk], w=[u2k])
                            P.op('pool', lambda e: e.tensor_tensor(
                                out=QT[64:96, h, t0:t0 + n], in0=u1[64:96, 0:n], in1=u2[64:96, 0:n], op=ALU.add),
                                r=[u1k, u2k], w=['bQT'])
                        for h in range(4):
                            qhead(h)
                    for b, (t0, n) in enumerate(BLOCKS):
                        bblk(b, t0, n)
                    precast(l, 2)
                    attention(sc, "b", 4, 96,
                              lambda h: QT[0:96, h, :], lambda h: KT[0:96, h, :],
                              lambda h, kt: Vm[:, kt, h, :],
                              ['bQT', 'bKT', 'Vm'], yT, 96 ** -0.5)
                    P.flush()
                merge(1, l, yT, mergedT, False, pre=lambda: ld_O(l))

                with ExitStack() as sc:
                    wo = VO['wo']
                    gb = gate_bcast(sc, l, 2, "g1b")
                    hin = Rot("oh", [SB(sc, f"oh{i}", [128, D]) for i in range(3)])
                    tmo = Rot("ot", [SB(sc, f"ot{i}", [128, D]) for i in range(2)])
                    for tt in range(NT):
                        s = 1 if tt < 2 else 0
                        b = [i for i, (t0, n) in enumerate(BLOCKS) if t0 <= tt * 128 < t0 + n][0]
                        h_, hk_ = hin.next()
                        P.dma('sp', lambda e, h_=h_, tt=tt: e.dma_start(out=h_[:], in_=(hs0v if l == 0 else hsv)[tt]),
                              hk_, r=[f"hs{tt}"], w=[hk_])
                        t_, tk = tmo.next()
                        for h2 in range(2):
                            pi = 4 + (tt % 2) * 2 + h2
                            for kc in range(8):
                                P.op('pe', lambda e, pi=pi, kc=kc, tt=tt, h2=h2: e.matmul(
                                    ps[pi][:, :], lhsT=mergedT[:, kc, tt * 128:(tt + 1) * 128],
                                    rhs=wo[:, kc, h2 * 512:(h2 + 1) * 512], start=(kc == 0), stop=(kc == 7)),
                                    r=[f"wo{h2}", f"mg{b}"], w=[PSK[pi]])
                            P.op('dve', lambda e, pi=pi, t_=t_, h2=h2, s=s: e.tensor_tensor(
                                out=t_[:, h2 * 512:(h2 + 1) * 512], in0=ps[pi][:, :], in1=gb[:, s, h2 * 512:(h2 + 1) * 512],
                                op=ALU.mult), r=[PSK[pi], 'g1b'], w=[tk])
                        P.op('dve', lambda e, h_=h_, t_=t_: e.tensor_tensor(out=h_[:], in0=h_[:], in1=t_[:], op=ALU.add),
                             r=[hk_, tk], w=[hk_])
                        P.dma('sp', lambda e, h_=h_, tt=tt: e.dma_start(out=hsv[tt], in_=h_[:]), 'S' + hk_,
                              r=[hk_], w=[f"hs{tt}"])
                    P.flush()

            tt0 = 2 if last else 0
            with ExitStack() as mo:
                hntok = SB(mo, "hntok", [128, NT, D], BF16)
                norm_phase(l, 3, 4, hntok=hntok)
                w12 = SB(mo, "w12", [128, 2, NT])
                desti = SB(mo, "desti", [128, NT, 2], I32)
                idn = SB(mo, "idn", [128, NB], I32)
                with ExitStack() as sc:
                    wr = SB(sc, "wr", [128, 8, 36], BF16)
                    brb = SB(sc, "brb", [128, 36])
                    Lg = SB(sc, "Lg", [128, NT, 36])
                    oh = SB(sc, "oh", [128, NT, 4])
                    lm = SB(sc, "lm", [128, NT, 32])
                    lm2 = SB(sc, "lm2", [128, NT, 32])
                    mk1 = SB(sc, "mk1", [128, NT, 32])
                    mk2 = SB(sc, "mk2", [128, NT, 32])
                    Ab = SB(sc, "Ab", [128, NT, 32], BF16)
                    RK = SB(sc, "RK", [128, NT, 32])
                    r1 = SB(sc, "r1", [128, 8, NT])
                    Cn = SB(sc, "Cn", [128, 32])
                    nbk = SB(sc, "nbk", [128, 32])
                    pend = SB(sc, "pend", [128, 32])
                    pst = SB(sc, "pst", [128, 32])
                    zer = SB(sc, "zer", [128, 32])
                    cmp1 = SB(sc, "cmp1", [128, 32, 36])
                    cmp2 = SB(sc, "cmp2", [128, NB, 32])
                    thr = SB(sc, "thr", [128, 36])
                    thri = SB(sc, "thri", [128, 36], I32)
                    bvl = SB(sc, "bvl", [128, NB])
                    bvli = SB(sc, "bvli", [128, NB], I32)
                    pidx = SB(sc, "pidx", [128, 4])
                    pidxi = SB(sc, "pidxi", [128, 4], I32)
                    pci = SB(sc, "pci", [128, 1], I32)
                    pcf = SB(sc, "pcf", [128, 1])
                    bke = SB(sc, "bke", [128, NB])
                    tmpf = SB(sc, "tmpf", [128, 2, NB])
                    destf = SB(sc, "destf", [128, NT, 2])
                    Um = SB(sc, "Um", [128, 128], BF16)
                    Uf = SB(sc, "Uf", [128, 128])
                    wload(wr[:], w_r[l].rearrange("(kc p) n -> p kc n", p=128), "wr")
                    fload(brb[:], b_r[l].partition_broadcast(128), "brb")
                    P.op('pool', lambda e: e.memset(Uf[:], 1.0), w=['Uf'])
                    P.op('pool', lambda e: e.affine_select(out=Uf[:], in_=Uf[:], pattern=[[1, 128]], base=0,
                                                            channel_multiplier=-1, compare_op=ALU.is_gt, fill=0.0),
                         r=['Uf'], w=['Uf'])
                    P.op('pool', lambda e: e.tensor_copy(out=Um[:], in_=Uf[:]), r=['Uf'], w=['Um'])
                    P.op('pool', lambda e: e.iota(thri[:], pattern=[[128, 36]], base=0, channel_multiplier=0), w=['thri'])
                    P.op('pool', lambda e: e.iota(bvli[:], pattern=[[1, NB]], base=0, channel_multiplier=0), w=['bvli'])
                    P.op('pool', lambda e: e.iota(pidxi[:], pattern=[[1, 4]], base=0, channel_multiplier=2), w=['pidxi'])
                    P.op('pool', lambda e: e.memset(zer[:], 0.0), w=['zer'])
                    P.op('pool', lambda e: e.iota(pci[:], pattern=[[0, 1]], base=0, channel_multiplier=1), w=['pci'])
                    P.op('dve', lambda e: e.tensor_copy(out=pcf[:], in_=pci[:]), r=['pci'], w=['pcf'])
                    P.op('dve', lambda e: e.tensor_copy(out=thr[:], in_=thri[:]), r=['thri'], w=['thr'])
                    P.op('dve', lambda e: e.tensor_copy(out=bvl[:], in_=bvli[:]), r=['bvli'], w=['bvl'])
                    P.op('dve', lambda e: e.tensor_copy(out=pidx[:], in_=pidxi[:]), r=['pidxi'], w=['pidx'])
                    for tt in range(NT):
                        pr, prk = ps[tt % 2], PSK[tt % 2]
                        for kc in range(8):
                            P.op('pe', lambda e, pr=pr, kc=kc, tt=tt: e.matmul(
                                pr[:, 0:36], lhsT=hnT[:, kc, tt * 128:(tt + 1) * 128], rhs=wr[:, kc, :],
                                start=(kc == 0), stop=(kc == 7)), r=['wr', f"hnT{tt}"], w=[prk])
                        P.op('dve', lambda e, pr=pr, tt=tt: e.tensor_tensor(out=Lg[:, tt, :], in0=pr[:, 0:36], in1=brb[:],
                                                                           op=ALU.add), r=[prk, 'brb'], w=['Lg'])
                    BIG = 1.0e4
                    mg_, sg_, m1, m2, dd, w1, w2, pgt = [r1[:, i, :] for i in range(8)]
                    R = lambda fn, r, w: P.op('dve', fn, r=r, w=w)
                    bc32 = lambda a: a.unsqueeze(2).broadcast_to([128, NT, 32])
                    R(lambda e: e.tensor_reduce(out=mg_, in_=Lg[:, :, 0:4], axis=AX.X, op=ALU.max), ['Lg'], ['r1a'])
                    R(lambda e: e.tensor_tensor(out=oh[:], in0=Lg[:, :, 0:4],
                                                in1=mg_.unsqueeze(2).broadcast_to([128, NT, 4]), op=ALU.is_ge),
                      ['Lg', 'r1a'], ['oh'])
                    R(lambda e: e.tensor_tensor(out=lm[:, :, 0:4], in0=Lg[:, :, 0:4],
                                                in1=mg_.unsqueeze(2).broadcast_to([128, NT, 4]), op=ALU.subtract),
                      ['Lg', 'r1a'], ['lm'])
                    P.op('act', lambda e: e.activation(out=lm[:, :, 0:4], in_=lm[:, :, 0:4], func=AF.Exp), r=['lm'], w=['lm'])
                    R(lambda e: e.tensor_reduce(out=sg_, in_=lm[:, :, 0:4], axis=AX.X, op=ALU.add), ['lm'], ['r1b'])
                    R(lambda e: e.reciprocal(out=pgt, in_=sg_), ['r1b'], ['r1c'])
                    R(lambda e: e.tensor_scalar(out=oh[:], in0=oh[:], scalar1=-1.0, scalar2=BIG, op0=ALU.add,
                                                op1=ALU.mult), ['oh'], ['oh'])
                    R(lambda e: e.tensor_tensor(
                        out=lm[:].rearrange("p t (g x) -> p t g x", g=4),
                        in0=Lg[:, :, 4:36].rearrange("p t (g x) -> p t g x", g=4),
                        in1=oh[:].unsqueeze(3).broadcast_to([128, NT, 4, 8]), op=ALU.add), ['Lg', 'oh', 'lm'], ['lm'])
                    R(lambda e: e.tensor_reduce(out=m1, in_=lm[:], axis=AX.X, op=ALU.max), ['lm'], ['r1d'])
                    R(lambda e: e.tensor_tensor(out=mk1[:], in0=lm[:], in1=bc32(m1), op=ALU.is_ge), ['lm', 'r1d'], ['mk1'])
                    R(lambda e: e.scalar_tensor_tensor(out=lm2[:], in0=mk1[:], scalar=-BIG, in1=lm[:], op0=ALU.mult,
                                                       op1=ALU.add), ['mk1', 'lm'], ['lm2'])
                    R(lambda e: e.tensor_reduce(out=m2, in_=lm2[:], axis=AX.X, op=ALU.max), ['lm2'], ['r1e'])
                    R(lambda e: e.tensor_tensor(out=mk2[:], in0=lm2[:], in1=bc32(m2), op=ALU.is_ge), ['lm2', 'r1e'], ['mk2'])
                    R(lambda e: e.tensor_tensor(out=dd, in0=m2, in1=m1, op=ALU.subtract), ['r1d', 'r1e'], ['r1f'])
                    P.op('act', lambda e: e.activation(out=dd, in_=dd, func=AF.Exp), r=['r1f'], w=['r1f'])
                    R(lambda e: e.tensor_scalar(out=w1, in0=dd, scalar1=1.0, scalar2=None, op0=ALU.add), ['r1f'], ['r1g'])
                    R(lambda e: e.reciprocal(out=w1, in_=w1), ['r1g'], ['r1g'])
                    R(lambda e: e.tensor_tensor(out=w12[:, 0, :], in0=w1, in1=pgt, op=ALU.mult), ['r1g', 'r1c'], ['w12'])
                    R(lambda e: e.tensor_tensor(out=w12[:, 1, :], in0=w12[:, 0, :], in1=dd, op=ALU.mult),
                      ['w12', 'r1f'], ['w12'])
                    if last:
                        R(lambda e: e.memset(mk1[:, 0:2, :], 0.0), ['mk1'], ['mk1'])
                        R(lambda e: e.memset(mk2[:, 0:2, :], 0.0), ['mk2'], ['mk2'])
                    R(lambda e: e.tensor_tensor(out=Ab[:], in0=mk1[:], in1=mk2[:], op=ALU.add), ['mk1', 'mk2'], ['Ab'])
                    for j in range(NT):
                        P.op('pe', lambda e, j=j: e.matmul(ps[2][:, 0:32], lhsT=onesb[:], rhs=Ab[:, j, :],
                                                           start=(j == 0), stop=(j == NT - 1)), r=['onesb', 'Ab'], w=[PSK[2]])
                    R(lambda e: e.tensor_copy(out=Cn[:], in_=ps[2][:, 0:32]), [PSK[2]], ['Cn'])
                    R(lambda e: e.tensor_tensor(out=cmp1[:], in0=Cn[:].unsqueeze(2).broadcast_to([128, 32, 36]),
                                                in1=thr[:].unsqueeze(1).broadcast_to([128, 32, 36]), op=ALU.is_gt),
                      ['Cn', 'thr'], ['cmp1'])
                    R(lambda e: e.tensor_reduce(out=nbk[:], in_=cmp1[:], axis=AX.X, op=ALU.add), ['cmp1'], ['nbk'])
                    R(lambda e: e.tensor_tensor_scan(out=pend[:], data0=nbk[:], data1=zer[:], initial=0.0,
                                                     op0=ALU.add, op1=ALU.add), ['nbk', 'zer'], ['pend'])
                    R(lambda e: e.tensor_tensor(out=pst[:], in0=pend[:], in1=nbk[:], op=ALU.subtract), ['pend', 'nbk'], ['pst'])
                    R(lambda e: e.tensor_scalar(out=pst[:], in0=pst[:], scalar1=128.0, scalar2=None, op0=ALU.mult),
                      ['pst'], ['pst'])
                    R(lambda e: e.tensor_tensor(out=cmp2[:], in0=pend[:].unsqueeze(1).broadcast_to([128, NB, 32]),
                                                in1=bvl[:].unsqueeze(2).broadcast_to([128, NB, 32]), op=ALU.is_le),
                      ['pend', 'bvl'], ['cmp2'])
                    R(lambda e: e.tensor_reduce(out=bke[:], in_=cmp2[:], axis=AX.X, op=ALU.add), ['cmp2'], ['bke'])
                    R(lambda e: e.tensor_scalar(out=bke[:], in0=bke[:], scalar1=31.0, scalar2=None, op0=ALU.min),
                      ['bke'], ['bke'])
                    R(lambda e: e.tensor_scalar(out=tmpf[:, 0, :], in0=bke[:], scalar1=128.0, scalar2=pcf[:, 0:1],
                                                op0=ALU.mult, op1=ALU.add), ['bke', 'pcf'], ['tmpf'])
                    R(lambda e: e.tensor_copy(out=idn[:], in_=tmpf[:, 0, :]), ['tmpf'], ['idn'])
                    for i in range(NT):
                        pk_ = 3 + i % 5
                        P.op('pe', lambda e, i=i, pk_=pk_: e.matmul(ps[pk_][:, 0:32], lhsT=Um[:], rhs=Ab[:, i, :],
                                                                    start=True, stop=(i == 0)),
                             r=['Um', 'Ab'], w=[PSK[pk_]])
                        for j in range(i):
                            P.op('pe', lambda e, i=i, j=j, pk_=pk_: e.matmul(
                                ps[pk_][:, 0:32], lhsT=onesb[:], rhs=Ab[:, j, :], start=False, stop=(j == i - 1)),
                                r=['onesb', 'Ab'], w=[PSK[pk_]])
                        R(lambda e, i=i, pk_=pk_: e.tensor_tensor(out=RK[:, i, :], in0=ps[pk_][:, 0:32], in1=pst[:],
                                                                  op=ALU.add), [PSK[pk_], 'pst'], ['RK'])
                    for k, mk in enumerate((mk1, mk2)):
                        R(lambda e, mk=mk: e.tensor_tensor(out=mk[:], in0=mk[:], in1=RK[:], op=ALU.mult),
                          [f"mk{k + 1}", 'RK'], [f"mk{k + 1}"])
                        R(lambda e, mk=mk, k=k: e.tensor_reduce(out=destf[:, :, k], in_=mk[:], axis=AX.X, op=ALU.add),
                          [f"mk{k + 1}"], ['destf'])
                    R(lambda e: e.tensor_copy(out=desti[:], in_=destf[:]), ['destf'], ['desti'])
                    for i in range(tt0, NT):
                        for k in range(2):
                            P.dma('pool', lambda e, i=i, k=k: e.indirect_dma_start(
                                out=xs_d, out_offset=bass.IndirectOffsetOnAxis(ap=desti[:, i, k:k + 1], axis=0),
                                in_=hntok[:, i, :], in_offset=None),
                                'xsc', r=['desti', f"hntok{i}"], w=[f"xsw{i}_{k}"])
                    P.flush()
                with ExitStack() as sc:
                    wgu = Rot("egu", [SB(sc, f"egu{i}", [128, 8, 512], BF16) for i in range(3)])
                    wdn = Rot("edn", [SB(sc, f"edn{i}", [128, 2, D], BF16) for i in range(3)])
                    xsb = Rot("exs", [SB(sc, f"exs{i}", [128, D], BF16) for i in range(3)])
                    xsT = Rot("exT", [SB(sc, f"exT{i}", [128, 8, 128], BF16) for i in range(2)])
                    sgl = Rot("esg", [SB(sc, f"esg{i}", [128, 256]) for i in range(2)])
                    hsb = Rot("ehs", [SB(sc, f"ehs{i}", [128, 256]) for i in range(2)])
                    hT = Rot("ehT", [SB(sc, f"ehT{i}", [128, 2, 128], BF16) for i in range(2)])
                    yb = Rot("eyb", [SB(sc, f"eyb{i}", [128, D]) for i in range(3)])
                    xsv = xs_d.rearrange("(n p) d -> n p d", p=128)
                    ysv = ys_d.rearrange("(n p) d -> n p d", p=128)

                    def eload(b):
                        g_, gk = wgu.next()
                        d_, dk_ = wdn.next()
                        x_, xk = xsb.next()
                        P.dma('pool', lambda e: e.indirect_dma_start(
                            out=g_[:].rearrange("p a b -> p (a b)"), out_offset=None,
                            in_=wgu_bf, in_offset=bass.IndirectOffsetOnAxis(ap=idn[:, b:b + 1], axis=0)),
                            gk, r=['idn'], w=[gk])
                        P.dma('pool', lambda e: e.indirect_dma_start(
                            out=d_[:].rearrange("p a b -> p (a b)"), out_offset=None,
                            in_=wdn_bf, in_offset=bass.IndirectOffsetOnAxis(ap=idn[:, b:b + 1], axis=0)),
                            dk_, r=['idn'], w=[dk_])
                        P.dma('act', lambda e: e.dma_start(out=x_[:], in_=xsv[b]), xk, w=[xk])
                        return g_, gk, d_, dk_, x_, xk

                    st8 = {}

                    def f1(b):
                        g_, gk, d_, dk_, x_, xk = st8[b]['w']
                        p = b % 2
                        xT_, xTk = xsT.next()
                        for kc in range(8):
                            P.op('pe', lambda e, kc=kc: e.transpose(
                                out=psb0[:, p * 1024 + kc * 128:p * 1024 + (kc + 1) * 128],
                                in_=x_[:, kc * 128:(kc + 1) * 128], identity=identb[:]),
                                r=[xk, 'identb'], w=[PSK[p]])
                        P.op('act', lambda e: e.copy(out=xT_[:, 0:4, :].rearrange("p a b -> p (a b)"),
                                                     in_=psb0[:, p * 1024:p * 1024 + 512]), r=[PSK[p]], w=[xTk])
                        P.op('dve', lambda e: e.tensor_copy(out=xT_[:, 4:8, :].rearrange("p a b -> p (a b)"),
                                                            in_=psb0[:, p * 1024 + 512:p * 1024 + 1024]),
                             r=[PSK[p]], w=[xTk])
                        st8[b]['xT'] = (xT_, xTk)

                    def f3(b):
                        g_, gk, d_, dk_, x_, xk = st8[b]['w']
                        xT_, xTk = st8[b]['xT']
                        p = b % 2
                        pg, pgk = ps[2 + p], PSK[2 + p]
                        for kc in range(8):
                            P.op('pe', lambda e, kc=kc: e.matmul(pg[:, :], lhsT=xT_[:, kc, :], rhs=g_[:, kc, :],
                                                                 start=(kc == 0), stop=(kc == 7)), r=[xTk, gk], w=[pgk])
                        s_, sk = sgl.next()
                        h_, hkey = hsb.next()
                        P.op('act', lambda e: e.activation(out=s_[:], in_=pg[:, 0:256], func=AF.Silu), r=[pgk], w=[sk])
                        P.op('dve', lambda e: e.tensor_tensor(out=h_[:], in0=pg[:, 256:512], in1=s_[:], op=ALU.mult),
                             r=[pgk, sk], w=[hkey])
                        st8[b]['h'] = (h_, hkey)

                    def f2(b):
                        h_, hkey = st8[b]['h']
                        pi0 = 4 + (b % 2) * 2
                        hT_, hTk = hT.next()
                        for c in range(2):
                            P.op('pe', lambda e, c=c: e.transpose(out=ps[pi0][:, c * 128:(c + 1) * 128],
                                                                  in_=h_[:, c * 128:(c + 1) * 128], identity=identf[:]),
                                 r=[hkey, 'identf'], w=[PSK[pi0]])
                        P.op('act', lambda e: e.copy(out=hT_[:].rearrange("p a b -> p (a b)"), in_=ps[pi0][:, 0:256]),
                             r=[PSK[pi0]], w=[hTk])
                        st8[b]['hT'] = (hT_, hTk)

                    def f4(b):
                        g_, gk, d_, dk_, x_, xk = st8[b]['w']
                        hT_, hTk = st8[b]['hT']
                        y_, yk = yb.next()
                        for h2 in range(2):
                            pi = 4 + (b % 2) * 2 + h2
                            for c in range(2):
                                P.op('pe', lambda e, pi=pi, c=c, h2=h2: e.matmul(
                                    ps[pi][:, :], lhsT=hT_[:, c, :], rhs=d_[:, c, h2 * 512:(h2 + 1) * 512],
                                    start=(c == 0), stop=(c == 1)), r=[hTk, dk_], w=[PSK[pi]])
                            if h2 == 0:
                                P.op('act', lambda e, pi=pi: e.copy(out=y_[:, 0:512], in_=ps[pi][:, :]), r=[PSK[pi]], w=[yk])
                            else:
                                P.op('dve', lambda e, pi=pi: e.tensor_copy(out=y_[:, 512:1024], in_=ps[pi][:, :]),
                                     r=[PSK[pi]], w=[yk])
                        P.dma('sp', lambda e: e.dma_start(out=ysv[b], in_=y_[:]), 'S' + yk, r=[yk], w=[f"ys{b}"])
                        del st8[b]
                    nbl = (NB - 4) if last else NB
                    for b in range(min(3, nbl)):
                        st8[b] = dict(w=eload(b))
                    f1(0)
                    f3(0)
                    for i in range(nbl):
                        if i + 1 < nbl:
                            f1(i + 1)
                        f2(i)
                        if i + 1 < nbl:
                            f3(i + 1)
                        f4(i)
                        if i + 3 < nbl:
                            st8[i + 3] = dict(w=eload(i + 3))
                    P.flush()
                with ExitStack() as sc:
                    gb = gate_bcast(sc, l, 5, "g2b")
                    hin = Rot("fh", [SB(sc, f"fh{i}", [128, D]) for i in range(3)])
                    g1r = Rot("fg1", [SB(sc, f"fg1{i}", [128, D]) for i in range(3)])
                    g2r = Rot("fg2", [SB(sc, f"fg2{i}", [128, D]) for i in range(3)])
                    fuse = (l + 1 < L)
                    if fuse:
                        ld_A(l + 1)
                        njunk = SB(sc, "cnjunk", [128, D], BF16)
                        nxn = Rot("cnxn", [SB(sc, f"cnxn{i}", [128, D]) for i in range(2)])
                        nst = SB(sc, "cnst", [128, NT, 2])
                    if last:
                        fgb = SB(sc, "fgb", [128, D])
                        fload(fgb[:], final_gain.partition_broadcast(128), "fgb")
                        fjunk = SB(sc, "fjunk", [128, D], BF16)
                        fst = SB(sc, "fst", [128, NT, 2])
                        outv = out_d.rearrange("(n p) d -> n p d", p=128)

                    def ftile(tt):
                        s = 1 if tt < 2 else 0
                        h_, hk_ = hin.next()
                        a_, ak = g1r.next()
                        b_, bk = g2r.next()
                        P.dma('sp', lambda e: e.dma_start(out=h_[:], in_=hsv[tt]), hk_, r=[f"hs{tt}"], w=[hk_])
                        P.dma('pool', lambda e: e.indirect_dma_start(
                            out=a_[:], out_offset=None, in_=ys_d,
                            in_offset=bass.IndirectOffsetOnAxis(ap=desti[:, tt, 0:1], axis=0)), ak, r=['desti'], w=[ak])
                        P.dma('pool', lambda e: e.indirect_dma_start(
                            out=b_[:], out_offset=None, in_=ys_d,
                            in_offset=bass.IndirectOffsetOnAxis(ap=desti[:, tt, 1:2], axis=0)), bk, r=['desti'], w=[bk])
                        P.op('act', lambda e: e.activation(out=a_[:], in_=a_[:], func=AF.Identity,
                                                           scale=w12[:, 0, tt:tt + 1]), r=[ak, 'w12'], w=[ak])
                        P.op('dve', lambda e: e.scalar_tensor_tensor(out=a_[:], in0=b_[:], scalar=w12[:, 1, tt:tt + 1],
                                                                     in1=a_[:], op0=ALU.mult, op1=ALU.add),
                             r=[ak, bk, 'w12'], w=[ak])
                        P.op('dve', lambda e: e.tensor_tensor(out=a_[:], in0=a_[:], in1=gb[:, s, :], op=ALU.mult),
                             r=[ak, 'g2b'], w=[ak])
                        P.op('dve', lambda e: e.tensor_tensor(out=h_[:], in0=h_[:], in1=a_[:], op=ALU.add),
                             r=[hk_, ak], w=[hk_])
                        if not last:
                            P.dma('sp', lambda e: e.dma_start(out=hsv[tt], in_=h_[:]), 'S' + hk_, r=[hk_], w=[f"hs{tt}"])
                            if fuse:
                                P.op('act', lambda e: e.activation(out=njunk[:], in_=h_[:], func=AF.Square,
                                                                   accum_out=nst[:, tt, 0:1]),
                                     r=[hk_], w=['cnjunk', f"cnst{tt}"])
                                rstd_from_ssq(nst[:, tt, 0:1], nst[:, tt, 1:2], D, [f"cnst{tt}"], f"cnrs{tt}")
                                xo, ko = nxn.next()
                                P.op('dve', lambda e: e.tensor_scalar(
                                    out=xo[:], in0=h_[:], scalar1=nst[:, tt, 1:2], scalar2=None, op0=ALU.mult),
                                    r=[hk_, f"cnrs{tt}"], w=[ko])
                                return xo, ko
                        else:
                            P.op('act', lambda e: e.activation(out=fjunk[:], in_=h_[:], func=AF.Square,
                                                               accum_out=fst[:, tt, 0:1]),
                                 r=[hk_], w=['fjunk', f"fst{tt}"])
                            rstd_from_ssq(fst[:, tt, 0:1], fst[:, tt, 1:2], D, [f"fst{tt}"], f"frs{tt}")
                            P.op('dve', lambda e: e.scalar_tensor_tensor(
                                out=h_[:], in0=h_[:], scalar=fst[:, tt, 1:2], in1=fgb[:], op0=ALU.mult, op1=ALU.mult),
                                r=[hk_, f"frs{tt}", 'fgb'], w=[hk_])
                            P.dma('sp', lambda e: e.dma_start(out=outv[tt - 2], in_=h_[:]), 'S' + hk_, r=[hk_],
                                  w=[f"out{tt}"])
                    def ftile2(tt, ctx):
                        if ctx is None:
                            return
                        xo, ko = ctx
                        s = 1 if tt < 2 else 0
                        ln = l + 1
                        for half in range(2):
                            pt = ps[(tt % 2) * 2 + half]
                            pk = PSK[(tt % 2) * 2 + half]
                            for q in range(4):
                                kc = half * 4 + q
                                P.op('pe', lambda e, pt=pt, kc=kc, q=q: e.transpose(
                                    out=pt[:, q * 128:(q + 1) * 128], in_=xo[:, kc * 128:(kc + 1) * 128],
                                    identity=identf[:]), r=[ko, 'identf'], w=[pk])
                            for q in range(4):
                                kc = half * 4 + q
                                if q % 2 == 0:
                                    P.op('act', lambda e, pt=pt, kc=kc, q=q: e.activation(
                                        out=hnT[:, kc, tt * 128:(tt + 1) * 128], in_=pt[:, q * 128:(q + 1) * 128],
                                        func=AF.Identity, scale=modc[:, ln, 1, kc, s:s + 1],
                                        bias=modc[:, ln, 0, kc, s:s + 1]), r=[pk, 'modc'], w=[f"hnT{tt}"])
                                else:
                                    P.op('dve', lambda e, pt=pt, kc=kc, q=q: e.tensor_scalar(
                                        out=hnT[:, kc, tt * 128:(tt + 1) * 128], in0=pt[:, q * 128:(q + 1) * 128],
                                        scalar1=modc[:, ln, 1, kc, s:s + 1], scalar2=modc[:, ln, 0, kc, s:s + 1],
                                        op0=ALU.mult, op1=ALU.add), r=[pk, 'modc'], w=[f"hnT{tt}"])
                    swpipe(range(tt0, NT), ftile, ftile2)
                    P.flush()
        print("ops", len(P.ops), "waits", P.nwaits, "sems", len(P.sems), flush=True)
    return nc


def _rope_tables():
    theta = 10000.0
    rows = 32
    GW = 64

    def ang(rot_dim):
        nf = rot_dim // 4
        freqs = (theta ** (-np.arange(nf, dtype=np.float32) / nf)).astype(np.float32)
        row = np.repeat(np.arange(rows, dtype=np.float32), GW)
        col = (np.arange(rows * GW) % GW).astype(np.float32)
        a = np.concatenate([row[:, None] * freqs, col[:, None] * freqs], axis=-1)
        a = np.concatenate([np.zeros((NCTX, rot_dim // 2), np.float32), a], axis=0)
        return a.astype(np.float32)
    ag = ang(64)
    cg, sg = np.cos(ag).T.astype(np.float32), np.sin(ag).T.astype(np.float32)
    G = np.zeros((2, 128, T), np.float32)
    for h in range(2):
        G[0, h * 64:h * 64 + 32] = cg
        G[0, h * 64 + 32:h * 64 + 64] = cg
        G[1, h * 64:h * 64 + 32] = -sg
        G[1, h * 64 + 32:h * 64 + 64] = sg
    am = ang(32)
    cm, sm = np.cos(am).T.astype(np.float32), np.sin(am).T.astype(np.float32)
    M = np.zeros((2, 128, T), np.float32)
    M[0, 64:80] = cm
    M[0, 80:96] = cm
    M[1, 64:80] = -sm
    M[1, 80:96] = sm
    return G, M


def _prep_weights(inp):
    f = lambda a: np.ascontiguousarray(np.asarray(a, dtype=np.float32))
    Lr = DEPTH
    w_in = f(inp['w_in'])
    sw64 = np.concatenate([np.arange(32, 64), np.arange(0, 32)])
    sw32 = np.concatenate([np.arange(16, 32), np.arange(0, 16)])
    cols = []
    cols += list(range(0, 512))
    cols += list(range(2080, 2208))
    cols += list(range(512, 768))
    cols += list(range(768, 896))
    cols += list(np.tile(np.arange(896, 928), 4))
    cols += list(np.tile(896 + sw32, 4))
    cols += list(range(928, 1696))
    cols += list(range(1696, 1952))
    for h in range(4):
        cols += list(1696 + h * 64 + sw64)
    for g in range(2):
        cols += list(range(1952 + g * 64, 1952 + (g + 1) * 64)) * 2
    for g in range(2):
        cols += list(1952 + g * 64 + sw64) * 2
    cols = np.asarray(cols)
    assert cols.shape[0] == 3072
    w_inx = f(w_in[:, :, cols])
    w_uq = f(inp['w_uq'])
    uq_cols = []
    for h in range(4):
        base = h * 96
        uq_cols += list(range(base, base + 96))
        uq_cols += list(range(base, base + 64)) + list(base + 64 + sw32)
    w_uqx = f(w_uq[:, :, np.asarray(uq_cols)])
    w_ukv = f(inp['w_ukv']).reshape(Lr, 128, 4, 128)
    w_ukvk = f(w_ukv[:, :, :, :64].reshape(Lr, 128, 256))
    w_ukvv = f(w_ukv[:, :, :, 64:].reshape(Lr, 128, 256))
    gq = f(inp['gqa_q_gain'])
    gk = f(inp['gqa_k_gain'])
    gqag = np.stack([np.tile(gq, (1, 2)), np.tile(gq[:, sw64], (1, 2)), np.tile(gk, (1, 2)),
                     np.tile(gk[:, sw64], (1, 2))], axis=-1)
    d = dict(
        w_ada=f(inp['w_ada']),
        b_adac=f(f(inp['b_ada']).reshape(Lr, 48, 128).transpose(0, 2, 1)),
        w_inx=w_inx,
        sgu_wT=f(f(inp['w_sgu']).transpose(0, 3, 1, 2)),
        sgu_b=f(f(inp['b_sgu']).transpose(0, 2, 1)),
        sgu_gain=f(inp['sgu_gain']),
        mla_qg=f(f(inp['mla_q_gain']).reshape(Lr, 2, 128).transpose(0, 2, 1)),
        mla_kvg=f(f(inp['mla_kv_gain']).reshape(Lr, 128, 1)),
        w_uqx=w_uqx, w_ukvk=w_ukvk, w_ukvv=w_ukvv,
        convw=f(f(inp['w_conv']).reshape(Lr, 3, 2, 128).transpose(0, 3, 2, 1)),
        gqag=f(gqag),
        w_gate=f(inp['w_gate']),
        b_gatec=f(f(inp['b_gate']).reshape(Lr, 4, 8, 128).transpose(0, 3, 1, 2)),
        w_branch=f(inp['w_branch']),
        w_out=f(inp['w_out']),
        w_r=f(np.concatenate([f(inp['w_group_router']), f(inp['w_expert_router'])], axis=-1)),
        b_r=f(np.concatenate([f(inp['b_group_router']), f(inp['b_expert_router'])], axis=-1)),
        w_gu_r=f(f(inp['w_expert_gate_up']).reshape(Lr, 32, 8, 128, 512).transpose(0, 1, 3, 2, 4)
                 .reshape(Lr, 32 * 128, 4096)),
        w_dn_r=f(f(inp['w_expert_down']).reshape(Lr, 32, 2, 128, D).transpose(0, 1, 3, 2, 4).reshape(Lr, 32 * 128, 2048)),
    )
    return d


PER_LAYER = ['w_ada', 'b_adac', 'w_inx', 'sgu_wT', 'sgu_b', 'sgu_gain', 'mla_qg', 'mla_kvg', 'w_uqx', 'w_ukvk',
             'w_ukvv', 'convw', 'gqag', 'w_gate', 'b_gatec', 'w_branch', 'w_out', 'w_r', 'b_r', 'w_gu_r', 'w_dn_r']

_CACHE = {}


def _get_prog(n_layers, final):
    key = (n_layers, final)
    if key not in _CACHE:
        _CACHE[key] = build(n_layers, final)
    return _CACHE[key]


def run_layers(hs_list, cvecs, wd, G, M, fg, l0, n_layers, final, cores):
    nc = _get_prog(n_layers, final)
    in_maps = []
    for i in range(len(cores)):
        m = {k: np.ascontiguousarray(wd[k][l0:l0 + n_layers]) for k in PER_LAYER}
        m.update(hs0=hs_list[i], cvec=cvecs[i], final_gain=fg, ropeG=G, ropeM=M)
        in_maps.append(m)
    res = run_bass_kernel_spmd(nc, in_maps, core_ids=list(cores))
    return [r["out" if final else "hs_out"] for r in res.results]


FUSED = True


def kernel(**inp):
    x = np.asarray(inp['x'], np.float32)
    c = np.asarray(inp['c'], np.float32)
    ctx = np.asarray(inp['ctx'], np.float32)
    c_ctx = np.asarray(inp['c_ctx'], np.float32)
    B = x.shape[0]
    wd = _prep_weights(inp)
    G, M = _rope_tables()
    fg = np.ascontiguousarray(np.asarray(inp['final_gain'], np.float32))
    hs = [np.ascontiguousarray(np.concatenate([ctx[b], x[b]], axis=0)) for b in range(B)]
    cvecs = [np.ascontiguousarray(np.stack([c[b].reshape(8, 128).T, c_ctx.reshape(8, 128).T], axis=-1)) for b in range(B)]
    cores = list(range(B))
    if FUSED:
        outs = run_layers(hs, cvecs, wd, G, M, fg, 0, DEPTH, True, cores)
    else:
        for l in range(DEPTH - 1):
            hs = run_layers(hs, cvecs, wd, G, M, fg, l, 1, False, cores)
        outs = run_layers(hs, cvecs, wd, G, M, fg, DEPTH - 1, 1, True, cores)
    return np.stack(outs, axis=0).astype(np.float32)
```

```python
import numpy as np
from contextlib import ExitStack
import concourse.bass as bass
import concourse.mybir as mybir
from concourse.bass_utils import run_bass_kernel_spmd

F32 = mybir.dt.float32
I32 = mybir.dt.int32
BF16 = mybir.dt.bfloat16
AF = mybir.ActivationFunctionType
ALU = mybir.AluOpType
AX = mybir.AxisListType

ENGS = ('pe', 'act', 'dve', 'pool', 'sp')
ENGOBJ = {'pe': 'tensor', 'act': 'scalar', 'dve': 'vector', 'pool': 'gpsimd', 'sp': 'sync'}

D = 1024
T = 2304
NCTX = 256
NT = 18
DEPTH = 4
EPS = 1e-6
NB = 68
CAP = NB * 128
BLOCKS = [(0, 256), (256, 512), (768, 512), (1280, 512), (1792, 512)]


STRICT = True


class Prog:
    def __init__(self, nc, st):
        self.nc = nc
        self.st = st
        self.ops = []
        self.buf = {}
        self.sems = {}
        self.cnt = {}
        self.val = {}
        self.seen = {e: {} for e in ENGS}
        self.flushed = 0
        self.nwaits = 0

    def _deps(self, eng, r, w, is_dma):
        deps = set()
        rw = set()
        for k in r:
            b = self.buf.get(k)
            if b is not None and b[0] is not None:
                deps.add(b[0])
                rw.add(b[0])
        for k in w:
            b = self.buf.get(k)
            if b is not None:
                if b[0] is not None:
                    deps.add(b[0])
                for ri in b[1].values():
                    deps.add(ri)
        out = []
        for d in deps:
            o = self.ops[d]
            if (not o['dma']) and (not is_dma) and o['eng'] == eng:
                if eng == 'pe':
                    continue
                if (not STRICT) and d not in rw:
                    continue
            out.append(d)
        return out

    def _commit(self, idx, semname, r, w):
        for k in r:
            b = self.buf.setdefault(k, [None, {}])
            b[1][semname] = idx
        for k in w:
            self.buf[k] = [idx, {}]

    def op(self, eng, fn, r=(), w=()):
        deps = self._deps(eng, r, w, False)
        idx = len(self.ops)
        self.ops.append(dict(eng=eng, fn=fn, deps=deps, dma=False, sem='E' + eng, sig=False))
        for d in deps:
            self.ops[d]['sig'] = True
        self._commit(idx, 'E' + eng, r, w)
        return idx

    def dma(self, q, fn, semkey, r=(), w=()):
        deps = self._deps(q, r, w, True)
        idx = len(self.ops)
        self.ops.append(dict(eng=q, fn=fn, deps=deps, dma=True, sem='D' + str(semkey), sig=True))
        for d in deps:
            self.ops[d]['sig'] = True
        self._commit(idx, 'D' + str(semkey), r, w)
        return idx

    def flush(self, final_keys=()):
        nc = self.nc
        ops = self.ops
        lo = self.flushed
        hi = len(ops)
        if lo == hi:
            return
        dma_last = {}
        for i in range(lo, hi):
            if ops[i]['dma']:
                dma_last[ops[i]['sem']] = i
        ops.append(dict(eng='sp', fn=None, deps=list(dma_last.values()), dma=False, sem='Esp', sig=False))
        hi = len(ops)
        for i in range(lo, hi):
            o = ops[i]
            s = o['sem']
            if s not in self.sems:
                self.sems[s] = self.st.enter_context(nc.semaphore(s))
                self.cnt[s] = 0
            if o['fn'] is None:
                continue
            if o['dma']:
                self.cnt[s] += 16
                self.val[i] = self.cnt[s]
            else:
                if o['sig']:
                    self.cnt[s] += 1
                    self.val[i] = self.cnt[s]
        per = {e: [] for e in ENGS}
        for i in range(lo, hi):
            per[ops[i]['eng']].append(i)
        prog = self

        def body(ename):
            def f(e):
                seen = prog.seen[ename]
                for i in per[ename]:
                    o = ops[i]
                    need = {}
                    for d in o['deps']:
                        if d < lo and not ops[d]['dma'] and d not in prog.val:
                            continue
                        if d not in prog.val:
                            continue
                        s = ops[d]['sem']
                        need[s] = max(need.get(s, 0), prog.val[d])
                    for s, v in need.items():
                        if seen.get(s, 0) < v:
                            e.wait_ge(prog.sems[s], v)
                            seen[s] = v
                            prog.nwaits += 1
                    if o['fn'] is None:
                        continue
                    ins = o['fn'](e)
                    if o['dma']:
                        ins.then_inc(prog.sems[o['sem']], 16)
                    elif i in prog.val:
                        ins.then_inc(prog.sems[o['sem']], 1)
            return f

        with nc.Block() as block:
            for ename in ENGS:
                if per[ename]:
                    getattr(block, ENGOBJ[ename])(body(ename))
        self.flushed = hi
        self.buf = {}


class Rot:
    def __init__(self, name, tiles):
        self.name = name
        self.tiles = tiles
        self.i = 0

    def next(self):
        k = self.i % len(self.tiles)
        self.i += 1
        return self.tiles[k], f"{self.name}{k}"


def build(n_layers, final):
    nc = bass.Bass("TRN2", target_bir_lowering=False)
    L = n_layers

    def din(name, shape):
        return nc.dram_tensor(name, list(shape), F32, kind="ExternalInput").ap()

    hs0 = din("hs0", [T, D])
    cvec = din("cvec", [128, 8, 2])
    w_ada = din("w_ada", [L, D, 6 * D])
    b_adac = din("b_adac", [L, 128, 48])
    w_inx = din("w_inx", [L, D, 3072])
    sgu_wT = din("sgu_wT", [L, 128, 4, 128])
    sgu_b = din("sgu_b", [L, 128, 4])
    sgu_gain = din("sgu_gain", [L, 256])
    mla_qg = din("mla_qg", [L, 128, 2])
    mla_kvg = din("mla_kvg", [L, 128, 1])
    w_uqx = din("w_uqx", [L, 256, 768])
    w_ukvk = din("w_ukvk", [L, 128, 256])
    w_ukvv = din("w_ukvv", [L, 128, 256])
    convw = din("convw", [L, 128, 2, 3])
    gqag = din("gqag", [L, 128, 4])
    w_gate = din("w_gate", [L, 4, D, D])
    b_gatec = din("b_gatec", [L, 128, 4, 8])
    w_branch = din("w_branch", [L, 4, 256, D])
    w_out = din("w_out", [L, D, D])
    w_r = din("w_r", [L, D, 36])
    b_r = din("b_r", [L, 36])
    w_gu_r = din("w_gu_r", [L, 32 * 128, 4096])
    w_dn_r = din("w_dn_r", [L, 32 * 128, 2048])
    wgu_bf = nc.dram_tensor("wgu_bf", [32 * 128, 4096], BF16).ap()
    wdn_bf = nc.dram_tensor("wdn_bf", [32 * 128, 2048], BF16).ap()
    final_gain = din("final_gain", [D])
    ropeG = din("ropeG", [2, 128, T])
    ropeM = din("ropeM", [2, 128, T])
    xs_d = nc.dram_tensor("xs_scr", [CAP, D], BF16).ap()
    ys_d = nc.dram_tensor("ys_scr", [CAP, D], F32).ap()
    if final:
        out_d = nc.dram_tensor("out", [T - NCTX, D], F32, kind="ExternalOutput").ap()
        hs_d = nc.dram_tensor("hs_scr", [T, D], F32).ap()
    else:
        hs_d = nc.dram_tensor("hs_out", [T, D], F32, kind="ExternalOutput").ap()

    with ExitStack() as st:
        P = Prog(nc, st)

        uid = [0]

        def SB(scope, name, shape, dt=F32):
            uid[0] += 1
            return scope.enter_context(nc.sbuf_tensor(f"{name}_{uid[0]}", list(shape), dt))

        psbig = [st.enter_context(nc.psum_tensor(f"psb{i}", [128, 1024], F32)) for i in range(4)]
        ps = [psbig[i // 2][:, (i % 2) * 512:(i % 2 + 1) * 512] for i in range(8)]
        PSK = [f"ps{i}" for i in range(8)]
        hnT = SB(st, "hnT", [128, 8, T], BF16)
        identf = SB(st, "identf", [128, 128], F32)
        identb = SB(st, "identb", [128, 128], BF16)
        psb0 = psbig[0][:, :].bitcast(BF16)
        onesb = SB(st, "onesb", [128, 128], BF16)
        blk1b = SB(st, "blk1b", [128, 128], BF16)
        onesf = SB(st, "onesf", [128, 128], F32)
        modc = SB(st, "modc", [128, L, 6, 8, 2], F32)
        sT = SB(st, "sT", [128, 8, 2], F32)

        def hk(b):
            t0, n = BLOCKS[b]
            return [f"hnT{t}" for t in range(t0 // 128, (t0 + n) // 128)]

        def bsl(b):
            t0, n = BLOCKS[b]
            return slice(t0, t0 + n)

        P.op('pool', lambda e: e.memset(identf[:], 0.0), w=['identf'])
        P.op('pool', lambda e: e.affine_select(out=identf[:], in_=identf[:], pattern=[[-1, 128]], base=0,
                                                channel_multiplier=1, compare_op=ALU.not_equal, fill=1.0),
             r=['identf'], w=['identf'])
        P.op('pool', lambda e: e.tensor_copy(out=identb[:], in_=identf[:]), r=['identf'], w=['identb'])
        P.op('pool', lambda e: e.memset(onesb[:], 1.0), w=['onesb'])
        P.op('pool', lambda e: e.memset(onesf[:], 1.0), w=['onesf'])
        P.op('pool', lambda e: e.memset(blk1b[:], 0.0), w=['blk1b'])
        P.op('pool', lambda e: e.memset(blk1b[0:64, 0:64], 1.0), r=['blk1b'], w=['blk1b'])
        P.op('pool', lambda e: e.memset(blk1b[64:128, 64:128], 1.0), r=['blk1b'], w=['blk1b'])

        if True:
            with ExitStack() as sc:
                hs0v = hs0.rearrange("(n p) d -> n p d", p=128)
                hsv = hs_d.rearrange("(n p) d -> n p d", p=128)
                zt = SB(sc, "zt", [128, D], BF16)
                P.op('pool', lambda e: e.memset(zt[:], 0.0), w=['zt'])
                xsv0 = xs_d.rearrange("(n p) d -> n p d", p=128)
                for b in range(NB):
                    P.dma('sp', lambda e, b=b: e.dma_start(out=xsv0[b], in_=zt[:]), 'zinit', r=['zt'], w=[f"xsz{b}"])
                P.dma('sp', lambda e: e.dma_start(out=sT[:], in_=cvec), 'sT', w=['sT'])
                P.op('act', lambda e: e.activation(out=sT[:], in_=sT[:], func=AF.Silu), r=['sT'], w=['sT'])
                wa = Rot("wa", [SB(sc, f"wa{i}", [128, 8, 1024], BF16) for i in range(3)])
                sTb = SB(sc, "sTb", [128, 8, 2], BF16)
                P.op('dve', lambda e: e.tensor_copy(out=sTb[:], in_=sT[:]), r=['sT'], w=['sTb'])
                bad = SB(sc, "bad", [128, L, 48])
                P.dma('sp', lambda e: e.dma_start(out=bad[:], in_=b_adac.rearrange("l p n -> p l n")), 'bad',
                      w=['bad'])
                for l in range(L):
                    for j in range(6):
                        wt, k = wa.next()
                        src = w_ada[l, :, j * 1024:(j + 1) * 1024].rearrange("(kc p) n -> p kc n", p=128)
                        P.dma('pool', lambda e, wt=wt, src=src: e.dma_start(out=wt[:], in_=src), k, w=[k])
                        pk = PSK[j % 2]
                        pt = ps[j % 2]
                        for fo in range(8):
                            for kc in range(8):
                                P.op('pe', lambda e, pt=pt, wt=wt, fo=fo, kc=kc: e.matmul(
                                    pt[:, fo * 2:fo * 2 + 2], lhsT=wt[:, kc, fo * 128:(fo + 1) * 128],
                                    rhs=sTb[:, kc, :], start=(kc == 0), stop=(kc == 7)),
                                    r=[k, 'sTb'], w=[pk])
                        for s in range(2):
                            P.op('dve', lambda e, pt=pt, l=l, j=j, s=s: e.tensor_tensor(
                                out=modc[:, l, j, :, s], in0=pt[:, s:16:2], in1=bad[:, l, j * 8:(j + 1) * 8],
                                op=ALU.add), r=[pk, 'bad'], w=['modc'])
                    for j in (1, 4):
                        P.op('dve', lambda e, l=l, j=j: e.tensor_scalar(
                            out=modc[:, l, j], in0=modc[:, l, j], scalar1=1.0, scalar2=None, op0=ALU.add),
                            r=['modc'], w=['modc'])
                P.flush()

        rawX = SB(st, "rawX", [128, 9216], BF16)
        rawY = SB(st, "rawY", [128, 10240], BF16)
        v8 = lambda buf, a, n: buf[:, a:a + 8 * n].rearrange("p (k n) -> p k n", k=8)
        VA = dict(wA=v8(rawX, 0, 512), wsT=rawX[:, 4096:4608].rearrange("p (k n) -> p k n", k=4))
        VM = dict(wg=v8(rawY, 0, 1024), wb=rawY[:, 8192:10240].rearrange("p (k n) -> p k n", k=2))
        VC = dict(wC=v8(rawX, 0, 768))
        VD = dict(wD=v8(rawX, 0, 1024), wV=v8(rawX, 8192, 128))
        VB = dict(wB=v8(rawX, 0, 640), wuq=rawX[:, 5120:6656].rearrange("p (k n) -> p k n", k=2),
                  wkk=rawX[:, 6656:6912], wkv=rawX[:, 6912:7168])
        VO = dict(wo=v8(rawX, 0, 1024))

        def pl(dst, src, key):
            P.dma('pool', lambda e: e.dma_start(out=dst, in_=src), key, w=[key])

        def winv(l, c0, c1):
            return w_inx[l, :, c0:c1].rearrange("(kc p) n -> p kc n", p=128)

        def ld_A(l):
            pl(VA['wA'], winv(l, 0, 512), "wA")
            pl(VA['wsT'], sgu_wT[l], "wsT")

        def ld_M(l, i):
            for h2 in range(2):
                pl(VM['wg'][:, :, h2 * 512:(h2 + 1) * 512],
                   w_gate[l, i, :, h2 * 512:(h2 + 1) * 512].rearrange("(kc p) n -> p kc n", p=128), f"wg{h2}")
            pl(VM['wb'], w_branch[l, i].rearrange("(kc p) n -> p kc n", p=128), "wb")

        def ld_C(l):
            pl(VC['wC'], winv(l, 1280, 2048), "wC")

        def ld_D(l):
            pl(VD['wD'], winv(l, 2048, 3072), "wD")
            pl(VD['wV'], winv(l, 512, 640), "wV")

        def ld_B(l):
            pl(VB['wB'], winv(l, 640, 1280), "wB")
            pl(VB['wuq'], w_uqx[l].rearrange("(c p) n -> p c n", p=128), "wuq")
            pl(VB['wkk'], w_ukvk[l], "wkk")
            pl(VB['wkv'], w_ukvv[l], "wkv")

        def ld_O(l):
            for h2 in range(2):
                pl(VO['wo'][:, :, h2 * 512:(h2 + 1) * 512],
                   w_out[l, :, h2 * 512:(h2 + 1) * 512].rearrange("(kc p) n -> p kc n", p=128), f"wo{h2}")

        def precast(l, part):
            gsrc = w_gu_r[l].rearrange("r (h c) -> (r h) c", h=2)
            gdst = wgu_bf.rearrange("r (h c) -> (r h) c", h=2)
            jobs = [(gsrc, gdst, i) for i in range(16)] + [(w_dn_r[l], wdn_bf, i) for i in range(8)]
            lo_, hi_ = [(0, 4), (4, 14), (14, 24)][part]
            for j, (src, dst, i) in enumerate(jobs[lo_:hi_]):
                P.dma('pool', lambda e, src=src, dst=dst, i=i: e.dma_start(
                    out=dst[i * 512:(i + 1) * 512, :], in_=src[i * 512:(i + 1) * 512, :]), 'pc', w=[f"pc{part}_{j}"])

        def swpipe(items, stage1, stage2):
            prev = None
            for it in items:
                ctx = stage1(it)
                if prev is not None:
                    stage2(*prev)
                prev = (it, ctx)
            if prev is not None:
                stage2(*prev)

        def rstd_from_ssq(ssq_ap, out_ap, n, keys_r, key_w):
            P.op('dve', lambda e: e.tensor_scalar(out=out_ap, in0=ssq_ap, scalar1=1.0 / n, scalar2=EPS,
                                                  op0=ALU.mult, op1=ALU.add), r=keys_r, w=[key_w])
            P.op('act', lambda e: e.activation(out=out_ap, in_=out_ap, func=AF.Sqrt), r=[key_w], w=[key_w])
            P.op('dve', lambda e: e.reciprocal(out=out_ap, in_=out_ap), r=[key_w], w=[key_w])

        def gate_bcast(sc, l, j, name):
            gb = SB(sc, name, [128, 2, D])
            dgs = [SB(sc, f"{name}dg{i}", [128, 4, 128]) for i in range(2)]
            it = 0
            for s in range(2):
                for half in range(2):
                    dg, dk = dgs[it % 2], f"{name}dg{it % 2}"
                    pt, pk = ps[it % 2], PSK[it % 2]
                    it += 1
                    P.op('dve', lambda e, dg=dg, half=half, s=s: e.tensor_tensor(
                        out=dg[:], in0=identf[:].unsqueeze(1).broadcast_to([128, 4, 128]),
                        in1=modc[:, l, j, half * 4:(half + 1) * 4, s].unsqueeze(2).broadcast_to([128, 4, 128]),
                        op=ALU.mult), r=['identf', 'modc'], w=[dk])
                    P.op('pe', lambda e, pt=pt, dg=dg: e.matmul(pt[:, :], lhsT=onesf[:],
                                                                rhs=dg[:].rearrange("p a b -> p (a b)"),
                                                                start=True, stop=True), r=['onesf', dk], w=[pk])
                    P.op('act', lambda e, pt=pt, half=half, s=s: e.copy(out=gb[:, s, half * 512:(half + 1) * 512],
                                                                        in_=pt[:, :]), r=[pk], w=[name])
            return gb

        def norm_phase(l, jsh, jsc, hntok=None, pre=None, src=None):
            with ExitStack() as sc:
                if pre is not None:
                    pre()
                if hntok is not None:
                    scb = gate_bcast(sc, l, jsc, "nscb")
                    shb = gate_bcast(sc, l, jsh, "nshb")
                xin = Rot("nx", [SB(sc, f"nx{i}", [128, D]) for i in range(3)])
                junk = SB(sc, "njunk", [128, D], BF16)
                xn = Rot("nxn", [SB(sc, f"nxn{i}", [128, D]) for i in range(2)])
                if hntok is not None:
                    htmp = Rot("nht", [SB(sc, f"nht{i}", [128, D]) for i in range(2)])
                st_ = SB(sc, "nst", [128, NT, 2])
                def n1(tt):
                    xt, kx = xin.next()
                    P.dma('sp', lambda e: e.dma_start(out=xt[:], in_=(hsv if src is None else src)[tt]), kx,
                          r=[f"hs{tt}"], w=[kx])
                    P.op('act', lambda e: e.activation(out=junk[:], in_=xt[:], func=AF.Square,
                                                       accum_out=st_[:, tt, 0:1]),
                         r=[kx], w=['njunk', f"nst{tt}"])
                    rstd_from_ssq(st_[:, tt, 0:1], st_[:, tt, 1:2], D, [f"nst{tt}"], f"nrs{tt}")
                    xo, ko = xn.next()
                    P.op('dve', lambda e: e.tensor_scalar(
                        out=xo[:], in0=xt[:], scalar1=st_[:, tt, 1:2], scalar2=None, op0=ALU.mult),
                        r=[kx, f"nrs{tt}"], w=[ko])
                    s = 1 if tt < 2 else 0
                    if hntok is not None:
                        ht_, htk = htmp.next()
                        P.op('dve', lambda e: e.tensor_tensor(
                            out=ht_[:], in0=xo[:], in1=scb[:, s, :], op=ALU.mult),
                            r=[ko, 'nscb'], w=[htk])
                        P.op('dve', lambda e: e.tensor_tensor(
                            out=hntok[:, tt, :], in0=ht_[:], in1=shb[:, s, :], op=ALU.add),
                            r=[htk, 'nshb'], w=[f"hntok{tt}"])
                    return xo, ko

                def n2(tt, ctx):
                    xo, ko = ctx
                    s = 1 if tt < 2 else 0
                    for half in range(2):
                        pt = ps[(tt % 2) * 2 + half]
                        pk = PSK[(tt % 2) * 2 + half]
                        for q in range(4):
                            kc = half * 4 + q
                            P.op('pe', lambda e, pt=pt, kc=kc, q=q: e.transpose(
                                out=pt[:, q * 128:(q + 1) * 128], in_=xo[:, kc * 128:(kc + 1) * 128],
                                identity=identf[:]), r=[ko, 'identf'], w=[pk])
                        for q in range(4):
                            kc = half * 4 + q
                            if q % 2 == 0:
                                P.op('act', lambda e, pt=pt, kc=kc, q=q: e.activation(
                                    out=hnT[:, kc, tt * 128:(tt + 1) * 128], in_=pt[:, q * 128:(q + 1) * 128],
                                    func=AF.Identity, scale=modc[:, l, jsc, kc, s:s + 1],
                                    bias=modc[:, l, jsh, kc, s:s + 1]), r=[pk, 'modc'], w=[f"hnT{tt}"])
                            else:
                                P.op('dve', lambda e, pt=pt, kc=kc, q=q: e.tensor_scalar(
                                    out=hnT[:, kc, tt * 128:(tt + 1) * 128], in0=pt[:, q * 128:(q + 1) * 128],
                                    scalar1=modc[:, l, jsc, kc, s:s + 1], scalar2=modc[:, l, jsh, kc, s:s + 1],
                                    op0=ALU.mult, op1=ALU.add), r=[pk, 'modc'], w=[f"hnT{tt}"])
                swpipe(range(NT), n1, n2)
                P.flush()

        def wload(dst, src, key):
            P.dma('pool', lambda e: e.dma_start(out=dst, in_=src), key, w=[key])

        def fload(dst, src, key):
            P.dma('sp', lambda e: e.dma_start(out=dst, in_=src), key, w=[key])

        def win_view(l, c0, c1):
            return w_inx[l, :, c0:c1].rearrange("(kc p) n -> p kc n", p=128)

        def attention(sc, nm, nheads, dk, QTf, KTf, Vf, qk_keys, yT, scale):
            Pt = Rot(nm + "Pt", [SB(sc, f"{nm}Pt{i}", [128, 2, 512], BF16) for i in range(2)])
            rec = Rot(nm + "rc", [SB(sc, f"{nm}rc{i}", [128, 512]) for i in range(1)])
            def do_blk(h, b, t0, n, po, pok):
                nkt = 2 if b == 0 else NT
                npair = nkt // 2
                qs = QTf(h)[:, t0:t0 + n]

                def smm(j):
                    for a_ in range(2):
                        kt = 2 * j + a_
                        pt, pk = ps[(j % 2) * 2 + a_], PSK[(j % 2) * 2 + a_]
                        P.op('pe', lambda e, pt=pt, kt=kt: e.matmul(
                            pt[:, 0:n], lhsT=KTf(h)[:, kt * 128:(kt + 1) * 128], rhs=qs,
                            start=True, stop=True), r=qk_keys, w=[pk])

                def pv(j):
                    big = psbig[j % 2]
                    pe_, pek = Pt.next()
                    P.op('act', lambda e: e.activation(
                        out=pe_[:, :, 0:n], in_=big[:, :].rearrange("p (a b) -> p a b", a=2)[:, :, 0:n],
                        func=AF.Exp, scale=scale), r=[PSK[(j % 2) * 2], PSK[(j % 2) * 2 + 1]], w=[pek])
                    for a_ in range(2):
                        kt = 2 * j + a_
                        P.op('pe', lambda e, a_=a_, kt=kt: e.matmul(
                            po[:, 0:n], lhsT=Vf(h, kt), rhs=pe_[:, a_, 0:n],
                            start=(kt == 0), stop=(kt == nkt - 1)), r=[pek] + qk_keys, w=[pok])
                smm(0)
                for j in range(npair):
                    if j + 1 < npair:
                        smm(j + 1)
                    pv(j)
                rc, rck = rec.next()
                P.op('dve', lambda e: e.reciprocal(out=rc[64:128, 0:n], in_=po[64:128, 0:n]), r=[pok], w=[rck])
                p0 = (h % 2) * 64
                P.op('dve', lambda e: e.tensor_tensor(
                    out=yT[p0:p0 + 64, h // 2, t0:t0 + n], in0=po[0:64, 0:n], in1=rc[64:128, 0:n],
                    op=ALU.mult), r=[pok, rck], w=[f"yT{b}"])
            it = 0
            for h in range(nheads):
                for b, (t0, n) in enumerate(BLOCKS):
                    do_blk(h, b, t0, n, ps[4 + (it % 2)], PSK[4 + (it % 2)])
                    it += 1

        def merge(i, l, yT, mergedT, first, pre=None):
            with ExitStack() as sc:
                if pre is not None:
                    pre()
                wg = VM['wg']
                wb = VM['wb']
                bg = SB(sc, "bg", [128, 8])
                sg = Rot("sg", [SB(sc, f"sg{i_}", [128, 512]) for i_ in range(2)])
                tm = Rot("tm", [SB(sc, f"tm{i_}", [128, 512]) for i_ in range(2)])
                fload(bg[:], b_gatec[l, :, i, :], "bg")
                def mblk(b, t0, n, fo, pg, pgk, pb, pbk):
                    for kc in range(8):
                        P.op('pe', lambda e, kc=kc: e.matmul(
                            pg[:, 0:n], lhsT=wg[:, kc, fo * 128:(fo + 1) * 128], rhs=hnT[:, kc, t0:t0 + n],
                            start=(kc == 0), stop=(kc == 7)), r=[f"wg{fo // 4}"] + hk(b), w=[pgk])
                    for c in range(2):
                        P.op('pe', lambda e, c=c: e.matmul(
                            pb[:, 0:n], lhsT=wb[:, c, fo * 128:(fo + 1) * 128], rhs=yT[:, c, t0:t0 + n],
                            start=(c == 0), stop=(c == 1)), r=["wb", f"yT{b}"], w=[pbk])
                    s_, sk = sg.next()
                    P.op('act', lambda e: e.activation(
                        out=s_[:, 0:n], in_=pg[:, 0:n], func=AF.Sigmoid, bias=bg[:, fo:fo + 1]),
                        r=[pgk, 'bg'], w=[sk])
                    if first:
                        P.op('dve', lambda e: e.tensor_tensor(
                            out=mergedT[:, fo, t0:t0 + n], in0=pb[:, 0:n], in1=s_[:, 0:n], op=ALU.mult),
                            r=[pbk, sk], w=[f"mg{b}"])
                    else:
                        t_, tk = tm.next()
                        P.op('dve', lambda e: e.tensor_tensor(
                            out=t_[:, 0:n], in0=pb[:, 0:n], in1=s_[:, 0:n], op=ALU.mult),
                            r=[pbk, sk], w=[tk])
                        P.op('pool', lambda e: e.tensor_tensor(
                            out=mergedT[:, fo, t0:t0 + n], in0=mergedT[:, fo, t0:t0 + n], in1=t_[:, 0:n],
                            op=ALU.add), r=[tk, f"mg{b}"], w=[f"mg{b}"])
                it = 0
                for b, (t0, n) in enumerate(BLOCKS):
                    for fo in range(8):
                        mblk(b, t0, n, fo, ps[it % 4], PSK[it % 4], ps[4 + it % 4], PSK[4 + it % 4])
                        it += 1
                P.flush()

        rnc = [0]

        def rope_norm_chunk(wq, blkq, blksw, gcol, gswcol, ssq_lhsT, nfeat, dst, dstkey, tmp):
            (tabC, tabS, sqR, rsbR, t1R, t2R, t3R) = tmp

            def r1(it):
                b, (t0, n) = it
                st_ = (rnc[0] % 2) * 3
                rnc[0] += 1
                pz, pzk = ps[st_], PSK[st_]
                pw, pwk = ps[st_ + 1], PSK[st_ + 1]
                pss, pssk = ps[st_ + 2], PSK[st_ + 2]
                sq, sqk = sqR.next()
                rsb, rsk = rsbR.next()
                for kc in range(8):
                    P.op('pe', lambda e, kc=kc: e.matmul(pz[:, 0:n], lhsT=wq[:, kc, blkq * 128:(blkq + 1) * 128],
                                                         rhs=hnT[:, kc, t0:t0 + n], start=(kc == 0), stop=(kc == 7)),
                         r=['wD'] + hk(b), w=[pzk])
                for kc in range(8):
                    P.op('pe', lambda e, kc=kc: e.matmul(pw[:, 0:n], lhsT=wq[:, kc, blksw * 128:(blksw + 1) * 128],
                                                         rhs=hnT[:, kc, t0:t0 + n], start=(kc == 0), stop=(kc == 7)),
                         r=['wD'] + hk(b), w=[pwk])
                tc_, tck = tabC.next()
                ts_, tsk = tabS.next()
                fload(tc_[:, 0:n], ropeG[0, :, t0:t0 + n], tck)
                fload(ts_[:, 0:n], ropeG[1, :, t0:t0 + n], tsk)
                P.op('act', lambda e: e.activation(out=sq[:, 0:n], in_=pz[:, 0:n], func=AF.Square), r=[pzk], w=[sqk])
                P.op('pe', lambda e: e.matmul(pss[:, 0:n], lhsT=ssq_lhsT, rhs=sq[:, 0:n], start=True, stop=True),
                     r=[sqk, 'blk1b'], w=[pssk])
                P.op('dve', lambda e: e.tensor_scalar(out=rsb[:, 0:n], in0=pss[:, 0:n], scalar1=1.0 / nfeat,
                                                      scalar2=EPS, op0=ALU.mult, op1=ALU.add), r=[pssk], w=[rsk])
                P.op('act', lambda e: e.activation(out=rsb[:, 0:n], in_=rsb[:, 0:n], func=AF.Sqrt), r=[rsk], w=[rsk])
                P.op('dve', lambda e: e.reciprocal(out=rsb[:, 0:n], in_=rsb[:, 0:n]), r=[rsk], w=[rsk])
                return (pz, pzk, pw, pwk, rsb, rsk, tc_, tck, ts_, tsk)

            def r2(it, ctx):
                b, (t0, n) = it
                (pz, pzk, pw, pwk, rsb, rsk, tc_, tck, ts_, tsk) = ctx
                t1, t1k = t1R.next()
                t2, t2k = t2R.next()
                t3, t3k = t3R.next()
                P.op('dve', lambda e: e.scalar_tensor_tensor(out=t1[:, 0:n], in0=pz[:, 0:n], scalar=gcol,
                                                             in1=tc_[:, 0:n], op0=ALU.mult, op1=ALU.mult),
                     r=[pzk, tck, 'gq'], w=[t1k])
                P.op('dve', lambda e: e.scalar_tensor_tensor(out=t2[:, 0:n], in0=pw[:, 0:n], scalar=gswcol,
                                                             in1=ts_[:, 0:n], op0=ALU.mult, op1=ALU.mult),
                     r=[pwk, tsk, 'gq'], w=[t2k])
                P.op('pool', lambda e: e.tensor_tensor(out=t3[:, 0:n], in0=t1[:, 0:n], in1=t2[:, 0:n], op=ALU.add),
                     r=[t1k, t2k], w=[t3k])
                if isinstance(dst, tuple):
                    for hh, d_ in enumerate(dst):
                        P.op('dve', lambda e, hh=hh, d_=d_: e.tensor_tensor(
                            out=d_[hh * 64:(hh + 1) * 64, t0:t0 + n], in0=t3[hh * 64:(hh + 1) * 64, 0:n],
                            in1=rsb[hh * 64:(hh + 1) * 64, 0:n], op=ALU.mult), r=[t3k, rsk], w=[dstkey])
                else:
                    P.op('dve', lambda e: e.tensor_tensor(out=dst[:, t0:t0 + n], in0=t3[:, 0:n], in1=rsb[:, 0:n],
                                                          op=ALU.mult), r=[t3k, rsk], w=[dstkey])
            for it in enumerate(BLOCKS):
                r2(it, r1(it))

        for l in range(L):
            last = final and (l == L - 1)
            if l == 0:
                norm_phase(l, 0, 1, pre=lambda: ld_A(l), src=hs0v)
            with ExitStack() as mx:
                mergedT = SB(mx, "mergedT", [128, 8, T], BF16)
                yT = SB(mx, "yT", [128, 2, T], BF16)

                with ExitStack() as sc:
                    ld_M(l, 0)
                    precast(l, 0)
                    wA = VA['wA']
                    wsT = VA['wsT']
                    bs = SB(sc, "bs", [128, 4])
                    gnb = SB(sc, "gnb", [128, 256])
                    g = Rot("ag", [SB(sc, f"ag{i}", [128, 512]) for i in range(2)])
                    junk = SB(sc, "ajunk", [128, 256], BF16)
                    stA = SB(sc, "stA", [128, NT, 2])
                    vn = Rot("avn", [SB(sc, f"avn{i}", [128, 256], BF16) for i in range(2)])
                    ya = Rot("aya", [SB(sc, f"aya{i}", [128, 256]) for i in range(2)])
                    fload(bs[:], sgu_b[l], "bs")
                    fload(gnb[:], sgu_gain[l].partition_broadcast(128), "gnb")
                    def a1(tt):
                        pa, pak = ps[tt % 2], PSK[tt % 2]
                        for kc in range(8):
                            P.op('pe', lambda e, kc=kc: e.matmul(
                                pa[:, :], lhsT=hnT[:, kc, tt * 128:(tt + 1) * 128], rhs=wA[:, kc, :],
                                start=(kc == 0), stop=(kc == 7)), r=['wA', f"hnT{tt}"], w=[pak])
                        g_, gk = g.next()
                        P.op('act', lambda e: e.activation(out=g_[:], in_=pa[:], func=AF.Gelu_apprx_tanh),
                             r=[pak], w=[gk])
                        P.op('act', lambda e: e.activation(out=junk[:], in_=g_[:, 256:512], func=AF.Square,
                                                           accum_out=stA[:, tt, 0:1]),
                             r=[gk], w=['ajunk', f"stA{tt}"])
                        rstd_from_ssq(stA[:, tt, 0:1], stA[:, tt, 1:2], 256, [f"stA{tt}"], f"rsA{tt}")
                        v_, vk = vn.next()
                        P.op('dve', lambda e: e.scalar_tensor_tensor(
                            out=v_[:], in0=g_[:, 256:512], scalar=stA[:, tt, 1:2], in1=gnb[:], op0=ALU.mult,
                            op1=ALU.mult), r=[gk, f"rsA{tt}", 'gnb'], w=[vk])
                        return g_, gk, v_, vk

                    def a2(tt, ctx):
                        g_, gk, v_, vk = ctx
                        pm, pmk = ps[2 + tt % 2], PSK[2 + tt % 2]
                        pT_, pTk = ps[4 + tt % 2], PSK[4 + tt % 2]
                        for gg in range(4):
                            P.op('pe', lambda e, gg=gg: e.matmul(
                                pm[:, gg * 64:(gg + 1) * 64], lhsT=wsT[:, gg, :], rhs=v_[:, gg * 64:(gg + 1) * 64],
                                start=True, stop=True), r=['wsT', vk], w=[pmk])
                        y_, yk = ya.next()
                        for gg in range(4):
                            P.op('dve', lambda e, gg=gg: e.scalar_tensor_tensor(
                                out=y_[:, gg * 64:(gg + 1) * 64], in0=pm[:, gg * 64:(gg + 1) * 64],
                                scalar=bs[:, gg:gg + 1], in1=g_[:, gg * 64:(gg + 1) * 64], op0=ALU.add, op1=ALU.mult),
                                r=[pmk, gk, 'bs'], w=[yk])
                        for c in range(2):
                            P.op('pe', lambda e, c=c: e.transpose(
                                out=pT_[:, c * 128:(c + 1) * 128], in_=y_[:, c * 128:(c + 1) * 128],
                                identity=identf[:]), r=[yk, 'identf'], w=[pTk])
                        bi = [i for i, (t0, n) in enumerate(BLOCKS) if t0 <= tt * 128 < t0 + n][0]
                        for c in range(2):
                            P.op('act', lambda e, c=c: e.copy(
                                out=yT[:, c, tt * 128:(tt + 1) * 128], in_=pT_[:, c * 128:(c + 1) * 128]),
                                r=[pTk], w=[f"yT{bi}"])
                    swpipe(range(NT), a1, a2)
                    P.flush()
                merge(0, l, yT, mergedT, True, pre=lambda: ld_C(l))

                with ExitStack() as sc:
                    ld_M(l, 2)
                    wC = VC['wC']
                    cw = SB(sc, "cw", [128, 2, 3])
                    U = SB(sc, "cU", [128, T + 3])
                    acc = SB(sc, "cacc", [128, T + 3])
                    CB = SB(sc, "cCB", [128, T + 3], BF16)
                    tx = Rot("ctx", [SB(sc, f"ctx{i}", [128, 512]) for i in range(2)])
                    fload(cw[:], convw[l], "cw")
                    off = lambda t0: t0 + 1 if t0 < NCTX else t0 + 2
                    def cblk(c, b, t0, n):
                        pb_, pc_, px_ = ps[(b % 2) * 3], ps[(b % 2) * 3 + 1], ps[(b % 2) * 3 + 2]
                        kb_, kc_, kx_ = PSK[(b % 2) * 3], PSK[(b % 2) * 3 + 1], PSK[(b % 2) * 3 + 2]
                        for (pp, kk, blk) in ((pb_, kb_, c), (pc_, kc_, 2 + c), (px_, kx_, 4 + c)):
                            for kc in range(8):
                                P.op('pe', lambda e, pp=pp, kc=kc, blk=blk: e.matmul(
                                    pp[:, 0:n], lhsT=wC[:, kc, blk * 128:(blk + 1) * 128],
                                    rhs=hnT[:, kc, t0:t0 + n], start=(kc == 0), stop=(kc == 7)),
                                    r=['wC'] + hk(b), w=[kk])
                        o = off(t0)
                        t_, tk = tx.next()
                        P.op('act', lambda e: e.copy(out=t_[:, 0:n], in_=px_[:, 0:n]), r=[kx_], w=[tk])
                        P.op('act', lambda e: e.copy(out=CB[:, o:o + n], in_=pb_[:, 0:n]), r=[kb_], w=['cCB'])
                        P.op('dve', lambda e: e.tensor_tensor(
                            out=U[:, o:o + n], in0=pc_[:, 0:n], in1=t_[:, 0:n], op=ALU.mult), r=[kc_, tk], w=['cU'])

                    def cout(c, b, t0, n):
                        o = off(t0)
                        P.op('pool', lambda e: e.tensor_tensor(
                            out=yT[:, c, t0:t0 + n], in0=CB[:, o:o + n], in1=acc[:, o:o + n], op=ALU.mult),
                            r=['cCB', 'cacc'], w=[f"yT{b}"])

                    def cchunk(c):
                        P.op('pool', lambda e: e.memset(U[:], 0.0), w=['cU'])
                        for b, (t0, n) in enumerate(BLOCKS):
                            cblk(c, b, t0, n)
                        NN = T + 1
                        P.op('dve', lambda e: e.tensor_scalar(out=acc[:, 1:1 + NN], in0=U[:, 0:NN],
                                                              scalar1=cw[:, c, 0:1], scalar2=None, op0=ALU.mult),
                             r=['cU', 'cw'], w=['cacc'])
                        P.op('dve', lambda e: e.scalar_tensor_tensor(
                            out=acc[:, 1:1 + NN], in0=U[:, 1:1 + NN], scalar=cw[:, c, 1:2], in1=acc[:, 1:1 + NN],
                            op0=ALU.mult, op1=ALU.add), r=['cU', 'cw', 'cacc'], w=['cacc'])
                        P.op('dve', lambda e: e.scalar_tensor_tensor(
                            out=acc[:, 1:1 + NN], in0=U[:, 2:2 + NN], scalar=cw[:, c, 2:3], in1=acc[:, 1:1 + NN],
                            op0=ALU.mult, op1=ALU.add), r=['cU', 'cw', 'cacc'], w=['cacc'])
                        for b, (t0, n) in enumerate(BLOCKS):
                            cout(c, b, t0, n)
                    for c in range(2):
                        cchunk(c)
                    P.flush()
                merge(2, l, yT, mergedT, False, pre=lambda: ld_D(l))

                with ExitStack() as sc:
                    ld_M(l, 3)
                    wD = VD['wD']
                    wV = VD['wV']
                    gq = SB(sc, "gq", [128, 4])
                    Vd = SB(sc, "Vd", [128, NT, 2, 128], BF16)
                    KT = SB(sc, "dKT", [128, 2, T], BF16)
                    tmp = (Rot("tbC", [SB(sc, f"tbC{i}", [128, 512]) for i in range(2)]),
                           Rot("tbS", [SB(sc, f"tbS{i}", [128, 512]) for i in range(2)]),
                           Rot("sq", [SB(sc, f"sq{i}", [128, 512], BF16) for i in range(2)]),
                           Rot("rsb", [SB(sc, f"rsb{i}", [128, 512]) for i in range(2)]),
                           Rot("t1", [SB(sc, f"t1{i}", [128, 512]) for i in range(2)]),
                           Rot("t2", [SB(sc, f"t2{i}", [128, 512]) for i in range(2)]),
                           Rot("t3", [SB(sc, f"t3{i}", [128, 512]) for i in range(2)]))
                    fload(gq[:], gqag[l], "gq")
                    P.op('pool', lambda e: e.memset(Vd[:, :, :, 64:128], 1.0), w=['Vd'])
                    for tt in range(NT):
                        pv, pvk = ps[6 + tt % 2], PSK[6 + tt % 2]
                        for kc in range(8):
                            P.op('pe', lambda e, pv=pv, kc=kc, tt=tt: e.matmul(
                                pv[:, 0:128], lhsT=hnT[:, kc, tt * 128:(tt + 1) * 128], rhs=wV[:, kc, :],
                                start=(kc == 0), stop=(kc == 7)), r=['wV', f"hnT{tt}"], w=[pvk])
                        P.op('act', lambda e, pv=pv, tt=tt: e.copy(
                            out=Vd[:, tt, :, 0:64], in_=pv[:, 0:128].rearrange("p (a b) -> p a b", a=2)),
                            r=[pvk], w=['Vd'])
                    QTm = SB(sc, "dQTm", [128, 4, T], BF16)
                    for c in range(2):
                        P.op('dve', lambda e, c=c: e.memset(QTm[64:128, 2 * c, :], 0.0), w=['dQTm'])
                        P.op('dve', lambda e, c=c: e.memset(QTm[0:64, 2 * c + 1, :], 0.0), w=['dQTm'])
                    for c in range(2):
                        rope_norm_chunk(wD, c, 2 + c, gq[:, 0:1], gq[:, 1:2], blk1b[:], 64,
                                        (QTm[:, 2 * c, :], QTm[:, 2 * c + 1, :]), 'dQTm', tmp)
                        rope_norm_chunk(wD, 4 + c, 6 + c, gq[:, 2:3], gq[:, 3:4], blk1b[:], 64, KT[:, c, :], 'dKT', tmp)
                    precast(l, 1)
                    attention(sc, "d", 4, 64,
                              lambda h: QTm[:, h, :],
                              lambda h: KT[:, h // 2, :],
                              lambda h, kt: Vd[:, kt, h // 2, :],
                              ['dQTm', 'dKT', 'Vd'], yT, 64 ** -0.5)
                    P.flush()
                merge(3, l, yT, mergedT, False, pre=lambda: ld_B(l))

                with ExitStack() as sc:
                    ld_M(l, 1)
                    wB = VB['wB']
                    wuq = VB['wuq']
                    wkk = VB['wkk']
                    wkv = VB['wkv']
                    qg = SB(sc, "qg", [128, 2])
                    kvg = SB(sc, "kvg", [128, 1])
                    mqnR = Rot("mqn", [SB(sc, f"mqn{i}", [128, 2, 512], BF16) for i in range(2)])
                    mkvnR = Rot("mkvn", [SB(sc, f"mkvn{i}", [128, 512], BF16) for i in range(2)])
                    QT = SB(sc, "bQT", [128, 4, T], BF16)
                    KT = SB(sc, "bKT", [128, 4, T], BF16)
                    Vm = SB(sc, "Vm", [128, NT, 4, 128], BF16)
                    tbC = Rot("mbC", [SB(sc, f"mbC{i}", [128, 512]) for i in range(1)])
                    tbS = Rot("mbS", [SB(sc, f"mbS{i}", [128, 512]) for i in range(1)])
                    sq = [SB(sc, f"bsq{i}", [128, 512], BF16) for i in range(2)]
                    rsbR = Rot("brsb", [SB(sc, f"brsb{i}", [128, 512]) for i in range(2)])
                    t1R = Rot("bt1", [SB(sc, f"bt1{i}", [128, 512]) for i in range(2)])
                    t2R = Rot("bt2", [SB(sc, f"bt2{i}", [128, 512]) for i in range(2)])
                    fload(qg[:], mla_qg[l], "qg")
                    fload(kvg[:], mla_kvg[l], "kvg")
                    P.op('pool', lambda e: e.memset(Vm[:, :, :, 64:128], 1.0), w=['Vm'])

                    def rsb_chain(pss, pssk, n, nfeat):
                        rsb, rk = rsbR.next()
                        P.op('dve', lambda e: e.tensor_scalar(out=rsb[:, 0:n], in0=pss[:, 0:n], scalar1=1.0 / nfeat,
                                                              scalar2=EPS, op0=ALU.mult, op1=ALU.add),
                             r=[pssk], w=[rk])
                        P.op('act', lambda e: e.activation(out=rsb[:, 0:n], in_=rsb[:, 0:n], func=AF.Sqrt),
                             r=[rk], w=[rk])
                        P.op('dve', lambda e: e.reciprocal(out=rsb[:, 0:n], in_=rsb[:, 0:n]), r=[rk], w=[rk])
                        return rsb, rk

                    def bblk(b, t0, n):
                        mqn, mqk = mqnR.next()
                        mkvn, mkk = mkvnR.next()
                        for c in range(2):
                            for kc in range(8):
                                P.op('pe', lambda e, c=c, kc=kc: e.matmul(
                                    ps[c][:, 0:n], lhsT=wB[:, kc, c * 128:(c + 1) * 128], rhs=hnT[:, kc, t0:t0 + n],
                                    start=(kc == 0), stop=(kc == 7)), r=['wB'] + hk(b), w=[PSK[c]])
                            P.op('act', lambda e, c=c: e.activation(out=sq[c][:, 0:n], in_=ps[c][:, 0:n],
                                                                    func=AF.Square), r=[PSK[c]], w=[f"bsq{c}"])
                        for c in range(2):
                            P.op('pe', lambda e, c=c: e.matmul(ps[2][:, 0:n], lhsT=onesb[:], rhs=sq[c][:, 0:n],
                                                               start=(c == 0), stop=(c == 1)),
                                 r=[f"bsq{c}", 'onesb'], w=[PSK[2]])
                        rsb, rk = rsb_chain(ps[2], PSK[2], n, 256)
                        for c in range(2):
                            P.op('dve', lambda e, c=c: e.scalar_tensor_tensor(
                                out=mqn[:, c, 0:n], in0=ps[c][:, 0:n], scalar=qg[:, c:c + 1], in1=rsb[:, 0:n],
                                op0=ALU.mult, op1=ALU.mult), r=[PSK[c], 'qg', rk], w=[mqk])
                        for kc in range(8):
                            P.op('pe', lambda e, kc=kc: e.matmul(
                                ps[3][:, 0:n], lhsT=wB[:, kc, 256:384], rhs=hnT[:, kc, t0:t0 + n],
                                start=(kc == 0), stop=(kc == 7)), r=['wB'] + hk(b), w=[PSK[3]])
                        P.op('act', lambda e: e.activation(out=sq[0][:, 0:n], in_=ps[3][:, 0:n], func=AF.Square),
                             r=[PSK[3]], w=["bsq0"])
                        P.op('pe', lambda e: e.matmul(ps[2][:, 0:n], lhsT=onesb[:], rhs=sq[0][:, 0:n], start=True,
                                                      stop=True), r=["bsq0", 'onesb'], w=[PSK[2]])
                        rsb2, rk2 = rsb_chain(ps[2], PSK[2], n, 128)
                        P.op('dve', lambda e: e.scalar_tensor_tensor(
                            out=mkvn[:, 0:n], in0=ps[3][:, 0:n], scalar=kvg[:, 0:1], in1=rsb2[:, 0:n],
                            op0=ALU.mult, op1=ALU.mult), r=[PSK[3], 'kvg', rk2], w=[mkk])
                        tc_, tck = tbC.next()
                        ts_, tsk = tbS.next()
                        fload(tc_[:, 0:n], ropeM[0, :, t0:t0 + n], tck)
                        fload(ts_[:, 0:n], ropeM[1, :, t0:t0 + n], tsk)
                        for (pi, blk) in ((4, 3), (5, 4)):
                            for kc in range(8):
                                P.op('pe', lambda e, pi=pi, blk=blk, kc=kc: e.matmul(
                                    ps[pi][:, 0:n], lhsT=wB[:, kc, blk * 128:(blk + 1) * 128],
                                    rhs=hnT[:, kc, t0:t0 + n], start=(kc == 0), stop=(kc == 7)),
                                    r=['wB'] + hk(b), w=[PSK[pi]])
                        t1, t1k = t1R.next()
                        t2, t2k = t2R.next()
                        P.op('dve', lambda e, tc_=tc_: e.tensor_tensor(out=t1[64:96, 0:n], in0=ps[4][64:96, 0:n],
                                                                       in1=tc_[64:96, 0:n], op=ALU.mult),
                             r=[PSK[4], tck], w=[t1k])
                        P.op('dve', lambda e, ts_=ts_: e.tensor_tensor(out=t2[64:96, 0:n], in0=ps[5][64:96, 0:n],
                                                                       in1=ts_[64:96, 0:n], op=ALU.mult),
                             r=[PSK[5], tsk], w=[t2k])
                        P.op('pool', lambda e: e.tensor_tensor(
                            out=t1[64:96, 0:n], in0=t1[64:96, 0:n], in1=t2[64:96, 0:n], op=ALU.add),
                            r=[t1k, t2k], w=[t1k])
                        for h in range(4):
                            P.op('act', lambda e, h=h: e.copy(out=KT[64:96, h, t0:t0 + n], in_=t1[64:96, 0:n]),
                                 r=[t1k], w=['bKT'])
                        for h in range(4):
                            pi = 6 + h % 2
                            P.op('pe', lambda e, h=h, pi=pi: e.matmul(
                                ps[pi][0:64, 0:n], lhsT=wkk[:, h * 64:(h + 1) * 64], rhs=mkvn[:, 0:n],
                                start=True, stop=True), r=['wkk', mkk], w=[PSK[pi]])
                            P.op('act', lambda e, h=h, pi=pi: e.copy(out=KT[0:64, h, t0:t0 + n],
                                                                     in_=ps[pi][0:64, 0:n]), r=[PSK[pi]], w=['bKT'])
                        for q_ in range(n // 128):
                            tt = t0 // 128 + q_
                            pv, pvk = ps[6 + tt % 2], PSK[6 + tt % 2]
                            P.op('pe', lambda e, pv=pv, q_=q_: e.matmul(
                                pv[:, 0:256], lhsT=mkvn[:, q_ * 128:(q_ + 1) * 128], rhs=wkv, start=True, stop=True),
                                r=['wkv', mkk], w=[pvk])
                            P.op('act', lambda e, pv=pv, tt=tt: e.copy(
                                out=Vm[:, tt, :, 0:64], in_=pv[:, 0:256].rearrange("p (a b) -> p a b", a=4)),
                                r=[pvk], w=['Vm'])
                        def qhead(h):
                            pa, pb_ = (h % 2) * 2, (h % 2) * 2 + 1
                            for (pi, sw) in ((pa, 0), (pb_, 1)):
                                for c in range(2):
                                    P.op('pe', lambda e, pi=pi, sw=sw, c=c: e.matmul(
                                        ps[pi][0:96, 0:n], lhsT=wuq[:, c, (h * 2 + sw) * 96:(h * 2 + sw + 1) * 96],
                                        rhs=mqn[:, c, 0:n], start=(c == 0), stop=(c == 1)),
                                        r=['wuq', mqk], w=[PSK[pi]])
                            u1, u1k = t1R.next()
                            u2, u2k = t2R.next()
                            P.op('act', lambda e: e.copy(out=QT[0:64, h, t0:t0 + n], in_=ps[pa][0:64, 0:n]),
                                 r=[PSK[pa]], w=['bQT'])
                            P.op('dve', lambda e: e.tensor_tensor(
                                out=u1[64:96, 0:n], in0=ps[pa][64:96, 0:n], in1=tc_[64:96, 0:n], op=ALU.mult),
                                r=[PSK[pa], tck], w=[u1k])
                            P.op('dve', lambda e: e.tensor_tensor(
                                out=u2[64:96, 0:n], in0=ps[pb_][64:96, 0:n], in1=ts_[64:96, 0:n], op=ALU.mult),
                                r=[PSK[pb_], tsk], w=[u2k])
                            P.op('pool', lambda e: e.tensor_tensor(
                                out=QT[64:96, h, t0:t0 + n], in0=u1[64:96, 0:n], in1=u2[64:96, 0:n], op=ALU.add),
                                r=[u1k, u2k], w=['bQT'])
                        for h in range(4):
                            qhead(h)
                    for b, (t0, n) in enumerate(BLOCKS):
                        bblk(b, t0, n)
                    precast(l, 2)
                    attention(sc, "b", 4, 96,
                              lambda h: QT[0:96, h, :], lambda h: KT[0:96, h, :],
                              lambda h, kt: Vm[:, kt, h, :],
                              ['bQT', 'bKT', 'Vm'], yT, 96 ** -0.5)
                    P.flush()
                merge(1, l, yT, mergedT, False, pre=lambda: ld_O(l))

                with ExitStack() as sc:
                    wo = VO['wo']
                    gb = gate_bcast(sc, l, 2, "g1b")
                    hin = Rot("oh", [SB(sc, f"oh{i}", [128, D]) for i in range(3)])
                    tmo = Rot("ot", [SB(sc, f"ot{i}", [128, D]) for i in range(2)])
                    for tt in range(NT):
                        s = 1 if tt < 2 else 0
                        b = [i for i, (t0, n) in enumerate(BLOCKS) if t0 <= tt * 128 < t0 + n][0]
                        h_, hk_ = hin.next()
                        P.dma('act', lambda e, h_=h_, tt=tt: e.dma_start(out=h_[:], in_=(hs0v if l == 0 else hsv)[tt]),
                              hk_, r=[f"hs{tt}"], w=[hk_])
                        t_, tk = tmo.next()
                        for h2 in range(2):
                            pi = 4 + (tt % 2) * 2 + h2
                            for kc in range(8):
                                P.op('pe', lambda e, pi=pi, kc=kc, tt=tt, h2=h2: e.matmul(
                                    ps[pi][:, :], lhsT=mergedT[:, kc, tt * 128:(tt + 1) * 128],
                                    rhs=wo[:, kc, h2 * 512:(h2 + 1) * 512], start=(kc == 0), stop=(kc == 7)),
                                    r=[f"wo{h2}", f"mg{b}"], w=[PSK[pi]])
                            P.op('dve', lambda e, pi=pi, t_=t_, h2=h2, s=s: e.tensor_tensor(
                                out=t_[:, h2 * 512:(h2 + 1) * 512], in0=ps[pi][:, :], in1=gb[:, s, h2 * 512:(h2 + 1) * 512],
                                op=ALU.mult), r=[PSK[pi], 'g1b'], w=[tk])
                        P.op('dve', lambda e, h_=h_, t_=t_: e.tensor_tensor(out=h_[:], in0=h_[:], in1=t_[:], op=ALU.add),
                             r=[hk_, tk], w=[hk_])
                        P.dma('sp', lambda e, h_=h_, tt=tt: e.dma_start(out=hsv[tt], in_=h_[:]), 'S' + hk_,
                              r=[hk_], w=[f"hs{tt}"])
                    P.flush()

            tt0 = 2 if last else 0
            with ExitStack() as mo:
                hntok = SB(mo, "hntok", [128, NT, D], BF16)
                norm_phase(l, 3, 4, hntok=hntok)
                w12 = SB(mo, "w12", [128, 2, NT])
                desti = SB(mo, "desti", [128, NT, 2], I32)
                idn = SB(mo, "idn", [128, NB], I32)
                with ExitStack() as sc:
                    wr = SB(sc, "wr", [128, 8, 36], BF16)
                    brb = SB(sc, "brb", [128, 36])
                    Lg = SB(sc, "Lg", [128, NT, 36])
                    oh = SB(sc, "oh", [128, NT, 4])
                    lm = SB(sc, "lm", [128, NT, 32])
                    lm2 = SB(sc, "lm2", [128, NT, 32])
                    mk1 = SB(sc, "mk1", [128, NT, 32])
                    mk2 = SB(sc, "mk2", [128, NT, 32])
                    Ab = SB(sc, "Ab", [128, NT, 32], BF16)
                    RK = SB(sc, "RK", [128, NT, 32])
                    r1 = SB(sc, "r1", [128, 8, NT])
                    Cn = SB(sc, "Cn", [128, 32])
                    nbk = SB(sc, "nbk", [128, 32])
                    pend = SB(sc, "pend", [128, 32])
                    pst = SB(sc, "pst", [128, 32])
                    zer = SB(sc, "zer", [128, 32])
                    cmp1 = SB(sc, "cmp1", [128, 32, 36])
                    cmp2 = SB(sc, "cmp2", [128, NB, 32])
                    thr = SB(sc, "thr", [128, 36])
                    thri = SB(sc, "thri", [128, 36], I32)
                    bvl = SB(sc, "bvl", [128, NB])
                    bvli = SB(sc, "bvli", [128, NB], I32)
                    pidx = SB(sc, "pidx", [128, 4])
                    pidxi = SB(sc, "pidxi", [128, 4], I32)
                    pci = SB(sc, "pci", [128, 1], I32)
                    pcf = SB(sc, "pcf", [128, 1])
                    bke = SB(sc, "bke", [128, NB])
                    tmpf = SB(sc, "tmpf", [128, 2, NB])
                    destf = SB(sc, "destf", [128, NT, 2])
                    Um = SB(sc, "Um", [128, 128], BF16)
                    Uf = SB(sc, "Uf", [128, 128])
                    wload(wr[:], w_r[l].rearrange("(kc p) n -> p kc n", p=128), "wr")
                    fload(brb[:], b_r[l].partition_broadcast(128), "brb")
                    P.op('pool', lambda e: e.memset(Uf[:], 1.0), w=['Uf'])
                    P.op('pool', lambda e: e.affine_select(out=Uf[:], in_=Uf[:], pattern=[[1, 128]], base=0,
                                                            channel_multiplier=-1, compare_op=ALU.is_gt, fill=0.0),
                         r=['Uf'], w=['Uf'])
                    P.op('pool', lambda e: e.tensor_copy(out=Um[:], in_=Uf[:]), r=['Uf'], w=['Um'])
                    P.op('pool', lambda e: e.iota(thri[:], pattern=[[128, 36]], base=0, channel_multiplier=0), w=['thri'])
                    P.op('pool', lambda e: e.iota(bvli[:], pattern=[[1, NB]], base=0, channel_multiplier=0), w=['bvli'])
                    P.op('pool', lambda e: e.iota(pidxi[:], pattern=[[1, 4]], base=0, channel_multiplier=2), w=['pidxi'])
                    P.op('pool', lambda e: e.memset(zer[:], 0.0), w=['zer'])
                    P.op('pool', lambda e: e.iota(pci[:], pattern=[[0, 1]], base=0, channel_multiplier=1), w=['pci'])
                    P.op('dve', lambda e: e.tensor_copy(out=pcf[:], in_=pci[:]), r=['pci'], w=['pcf'])
                    P.op('dve', lambda e: e.tensor_copy(out=thr[:], in_=thri[:]), r=['thri'], w=['thr'])
                    P.op('dve', lambda e: e.tensor_copy(out=bvl[:], in_=bvli[:]), r=['bvli'], w=['bvl'])
                    P.op('dve', lambda e: e.tensor_copy(out=pidx[:], in_=pidxi[:]), r=['pidxi'], w=['pidx'])
                    for tt in range(NT):
                        pr, prk = ps[tt % 2], PSK[tt % 2]
                        for kc in range(8):
                            P.op('pe', lambda e, pr=pr, kc=kc, tt=tt: e.matmul(
                                pr[:, 0:36], lhsT=hnT[:, kc, tt * 128:(tt + 1) * 128], rhs=wr[:, kc, :],
                                start=(kc == 0), stop=(kc == 7)), r=['wr', f"hnT{tt}"], w=[prk])
                        P.op('dve', lambda e, pr=pr, tt=tt: e.tensor_tensor(out=Lg[:, tt, :], in0=pr[:, 0:36], in1=brb[:],
                                                                           op=ALU.add), r=[prk, 'brb'], w=['Lg'])
                    BIG = 1.0e4
                    mg_, sg_, m1, m2, dd, w1, w2, pgt = [r1[:, i, :] for i in range(8)]
                    R = lambda fn, r, w: P.op('dve', fn, r=r, w=w)
                    bc32 = lambda a: a.unsqueeze(2).broadcast_to([128, NT, 32])
                    R(lambda e: e.tensor_reduce(out=mg_, in_=Lg[:, :, 0:4], axis=AX.X, op=ALU.max), ['Lg'], ['r1a'])
                    R(lambda e: e.tensor_tensor(out=oh[:], in0=Lg[:, :, 0:4],
                                                in1=mg_.unsqueeze(2).broadcast_to([128, NT, 4]), op=ALU.is_ge),
                      ['Lg', 'r1a'], ['oh'])
                    R(lambda e: e.tensor_tensor(out=lm[:, :, 0:4], in0=Lg[:, :, 0:4],
                                                in1=mg_.unsqueeze(2).broadcast_to([128, NT, 4]), op=ALU.subtract),
                      ['Lg', 'r1a'], ['lm'])
                    P.op('act', lambda e: e.activation(out=lm[:, :, 0:4], in_=lm[:, :, 0:4], func=AF.Exp), r=['lm'], w=['lm'])
                    R(lambda e: e.tensor_reduce(out=sg_, in_=lm[:, :, 0:4], axis=AX.X, op=ALU.add), ['lm'], ['r1b'])
                    R(lambda e: e.reciprocal(out=pgt, in_=sg_), ['r1b'], ['r1c'])
                    R(lambda e: e.tensor_scalar(out=oh[:], in0=oh[:], scalar1=-1.0, scalar2=BIG, op0=ALU.add,
                                                op1=ALU.mult), ['oh'], ['oh'])
                    R(lambda e: e.tensor_tensor(
                        out=lm[:].rearrange("p t (g x) -> p t g x", g=4),
                        in0=Lg[:, :, 4:36].rearrange("p t (g x) -> p t g x", g=4),
                        in1=oh[:].unsqueeze(3).broadcast_to([128, NT, 4, 8]), op=ALU.add), ['Lg', 'oh', 'lm'], ['lm'])
                    R(lambda e: e.tensor_reduce(out=m1, in_=lm[:], axis=AX.X, op=ALU.max), ['lm'], ['r1d'])
                    R(lambda e: e.tensor_tensor(out=mk1[:], in0=lm[:], in1=bc32(m1), op=ALU.is_ge), ['lm', 'r1d'], ['mk1'])
                    R(lambda e: e.scalar_tensor_tensor(out=lm2[:], in0=mk1[:], scalar=-BIG, in1=lm[:], op0=ALU.mult,
                                                       op1=ALU.add), ['mk1', 'lm'], ['lm2'])
                    R(lambda e: e.tensor_reduce(out=m2, in_=lm2[:], axis=AX.X, op=ALU.max), ['lm2'], ['r1e'])
                    R(lambda e: e.tensor_tensor(out=mk2[:], in0=lm2[:], in1=bc32(m2), op=ALU.is_ge), ['lm2', 'r1e'], ['mk2'])
                    R(lambda e: e.tensor_tensor(out=dd, in0=m2, in1=m1, op=ALU.subtract), ['r1d', 'r1e'], ['r1f'])
                    P.op('act', lambda e: e.activation(out=dd, in_=dd, func=AF.Exp), r=['r1f'], w=['r1f'])
                    R(lambda e: e.tensor_scalar(out=w1, in0=dd, scalar1=1.0, scalar2=None, op0=ALU.add), ['r1f'], ['r1g'])
                    R(lambda e: e.reciprocal(out=w1, in_=w1), ['r1g'], ['r1g'])
                    R(lambda e: e.tensor_tensor(out=w12[:, 0, :], in0=w1, in1=pgt, op=ALU.mult), ['r1g', 'r1c'], ['w12'])
                    R(lambda e: e.tensor_tensor(out=w12[:, 1, :], in0=w12[:, 0, :], in1=dd, op=ALU.mult),
                      ['w12', 'r1f'], ['w12'])
                    if last:
                        R(lambda e: e.memset(mk1[:, 0:2, :], 0.0), ['mk1'], ['mk1'])
                        R(lambda e: e.memset(mk2[:, 0:2, :], 0.0), ['mk2'], ['mk2'])
                    R(lambda e: e.tensor_tensor(out=Ab[:], in0=mk1[:], in1=mk2[:], op=ALU.add), ['mk1', 'mk2'], ['Ab'])
                    for j in range(NT):
                        P.op('pe', lambda e, j=j: e.matmul(ps[2][:, 0:32], lhsT=onesb[:], rhs=Ab[:, j, :],
                                                           start=(j == 0), stop=(j == NT - 1)), r=['onesb', 'Ab'], w=[PSK[2]])
                    R(lambda e: e.tensor_copy(out=Cn[:], in_=ps[2][:, 0:32]), [PSK[2]], ['Cn'])
                    R(lambda e: e.tensor_tensor(out=cmp1[:], in0=Cn[:].unsqueeze(2).broadcast_to([128, 32, 36]),
                                                in1=thr[:].unsqueeze(1).broadcast_to([128, 32, 36]), op=ALU.is_gt),
                      ['Cn', 'thr'], ['cmp1'])
                    R(lambda e: e.tensor_reduce(out=nbk[:], in_=cmp1[:], axis=AX.X, op=ALU.add), ['cmp1'], ['nbk'])
                    R(lambda e: e.tensor_tensor_scan(out=pend[:], data0=nbk[:], data1=zer[:], initial=0.0,
                                                     op0=ALU.add, op1=ALU.add), ['nbk', 'zer'], ['pend'])
                    R(lambda e: e.tensor_tensor(out=pst[:], in0=pend[:], in1=nbk[:], op=ALU.subtract), ['pend', 'nbk'], ['pst'])
                    R(lambda e: e.tensor_scalar(out=pst[:], in0=pst[:], scalar1=128.0, scalar2=None, op0=ALU.mult),
                      ['pst'], ['pst'])
                    R(lambda e: e.tensor_tensor(out=cmp2[:], in0=pend[:].unsqueeze(1).broadcast_to([128, NB, 32]),
                                                in1=bvl[:].unsqueeze(2).broadcast_to([128, NB, 32]), op=ALU.is_le),
                      ['pend', 'bvl'], ['cmp2'])
                    R(lambda e: e.tensor_reduce(out=bke[:], in_=cmp2[:], axis=AX.X, op=ALU.add), ['cmp2'], ['bke'])
                    R(lambda e: e.tensor_scalar(out=bke[:], in0=bke[:], scalar1=31.0, scalar2=None, op0=ALU.min),
                      ['bke'], ['bke'])
                    R(lambda e: e.tensor_scalar(out=tmpf[:, 0, :], in0=bke[:], scalar1=128.0, scalar2=pcf[:, 0:1],
                                                op0=ALU.mult, op1=ALU.add), ['bke', 'pcf'], ['tmpf'])
                    R(lambda e: e.tensor_copy(out=idn[:], in_=tmpf[:, 0, :]), ['tmpf'], ['idn'])
                    for i in range(NT):
                        pk_ = 3 + i % 5
                        P.op('pe', lambda e, i=i, pk_=pk_: e.matmul(ps[pk_][:, 0:32], lhsT=Um[:], rhs=Ab[:, i, :],
                                                                    start=True, stop=(i == 0)),
                             r=['Um', 'Ab'], w=[PSK[pk_]])
                        for j in range(i):
                            P.op('pe', lambda e, i=i, j=j, pk_=pk_: e.matmul(
                                ps[pk_][:, 0:32], lhsT=onesb[:], rhs=Ab[:, j, :], start=False, stop=(j == i - 1)),
                                r=['onesb', 'Ab'], w=[PSK[pk_]])
                        R(lambda e, i=i, pk_=pk_: e.tensor_tensor(out=RK[:, i, :], in0=ps[pk_][:, 0:32], in1=pst[:],
                                                                  op=ALU.add), [PSK[pk_], 'pst'], ['RK'])
                    for k, mk in enumerate((mk1, mk2)):
                        R(lambda e, mk=mk: e.tensor_tensor(out=mk[:], in0=mk[:], in1=RK[:], op=ALU.mult),
                          [f"mk{k + 1}", 'RK'], [f"mk{k + 1}"])
                        R(lambda e, mk=mk, k=k: e.tensor_reduce(out=destf[:, :, k], in_=mk[:], axis=AX.X, op=ALU.add),
                          [f"mk{k + 1}"], ['destf'])
                    R(lambda e: e.tensor_copy(out=desti[:], in_=destf[:]), ['destf'], ['desti'])
                    for i in range(tt0, NT):
                        for k in range(2):
                            P.dma('pool', lambda e, i=i, k=k: e.indirect_dma_start(
                                out=xs_d, out_offset=bass.IndirectOffsetOnAxis(ap=desti[:, i, k:k + 1], axis=0),
                                in_=hntok[:, i, :], in_offset=None),
                                'xsc', r=['desti', f"hntok{i}"], w=[f"xsw{i}_{k}"])
                    P.flush()
                with ExitStack() as sc:
                    wgu = Rot("egu", [SB(sc, f"egu{i}", [128, 8, 512], BF16) for i in range(3)])
                    wdn = Rot("edn", [SB(sc, f"edn{i}", [128, 2, D], BF16) for i in range(3)])
                    xsb = Rot("exs", [SB(sc, f"exs{i}", [128, D], BF16) for i in range(3)])
                    xsT = Rot("exT", [SB(sc, f"exT{i}", [128, 8, 128], BF16) for i in range(2)])
                    sgl = Rot("esg", [SB(sc, f"esg{i}", [128, 256]) for i in range(2)])
                    hsb = Rot("ehs", [SB(sc, f"ehs{i}", [128, 256]) for i in range(2)])
                    hT = Rot("ehT", [SB(sc, f"ehT{i}", [128, 2, 128], BF16) for i in range(2)])
                    yb = Rot("eyb", [SB(sc, f"eyb{i}", [128, D]) for i in range(3)])
                    xsv = xs_d.rearrange("(n p) d -> n p d", p=128)
                    ysv = ys_d.rearrange("(n p) d -> n p d", p=128)

                    def eload(b):
                        g_, gk = wgu.next()
                        d_, dk_ = wdn.next()
                        x_, xk = xsb.next()
                        P.dma('pool', lambda e: e.indirect_dma_start(
                            out=g_[:].rearrange("p a b -> p (a b)"), out_offset=None,
                            in_=wgu_bf, in_offset=bass.IndirectOffsetOnAxis(ap=idn[:, b:b + 1], axis=0)),
                            gk, r=['idn'], w=[gk])
                        P.dma('pool', lambda e: e.indirect_dma_start(
                            out=d_[:].rearrange("p a b -> p (a b)"), out_offset=None,
                            in_=wdn_bf, in_offset=bass.IndirectOffsetOnAxis(ap=idn[:, b:b + 1], axis=0)),
                            dk_, r=['idn'], w=[dk_])
                        P.dma('act', lambda e: e.dma_start(out=x_[:], in_=xsv[b]), xk, w=[xk])
                        return g_, gk, d_, dk_, x_, xk

                    st8 = {}

                    def f1(b):
                        g_, gk, d_, dk_, x_, xk = st8[b]['w']
                        p = b % 2
                        xT_, xTk = xsT.next()
                        for kc in range(8):
                            P.op('pe', lambda e, kc=kc: e.transpose(
                                out=psb0[:, p * 1024 + kc * 128:p * 1024 + (kc + 1) * 128],
                                in_=x_[:, kc * 128:(kc + 1) * 128], identity=identb[:]),
                                r=[xk, 'identb'], w=[PSK[p]])
                        P.op('act', lambda e: e.copy(out=xT_[:, 0:4, :].rearrange("p a b -> p (a b)"),
                                                     in_=psb0[:, p * 1024:p * 1024 + 512]), r=[PSK[p]], w=[xTk])
                        P.op('dve', lambda e: e.tensor_copy(out=xT_[:, 4:8, :].rearrange("p a b -> p (a b)"),
                                                            in_=psb0[:, p * 1024 + 512:p * 1024 + 1024]),
                             r=[PSK[p]], w=[xTk])
                        st8[b]['xT'] = (xT_, xTk)

                    def f3(b):
                        g_, gk, d_, dk_, x_, xk = st8[b]['w']
                        xT_, xTk = st8[b]['xT']
                        p = b % 2
                        pg, pgk = ps[2 + p], PSK[2 + p]
                        for kc in range(8):
                            P.op('pe', lambda e, kc=kc: e.matmul(pg[:, :], lhsT=xT_[:, kc, :], rhs=g_[:, kc, :],
                                                                 start=(kc == 0), stop=(kc == 7)), r=[xTk, gk], w=[pgk])
                        s_, sk = sgl.next()
                        h_, hkey = hsb.next()
                        P.op('act', lambda e: e.activation(out=s_[:], in_=pg[:, 0:256], func=AF.Silu), r=[pgk], w=[sk])
                        P.op('dve', lambda e: e.tensor_tensor(out=h_[:], in0=pg[:, 256:512], in1=s_[:], op=ALU.mult),
                             r=[pgk, sk], w=[hkey])
                        st8[b]['h'] = (h_, hkey)

                    def f2(b):
                        h_, hkey = st8[b]['h']
                        pi0 = 4 + (b % 2) * 2
                        hT_, hTk = hT.next()
                        for c in range(2):
                            P.op('pe', lambda e, c=c: e.transpose(out=ps[pi0][:, c * 128:(c + 1) * 128],
                                                                  in_=h_[:, c * 128:(c + 1) * 128], identity=identf[:]),
                                 r=[hkey, 'identf'], w=[PSK[pi0]])
                        P.op('act', lambda e: e.copy(out=hT_[:].rearrange("p a b -> p (a b)"), in_=ps[pi0][:, 0:256]),
                             r=[PSK[pi0]], w=[hTk])
                        st8[b]['hT'] = (hT_, hTk)

                    def f4(b):
                        g_, gk, d_, dk_, x_, xk = st8[b]['w']
                        hT_, hTk = st8[b]['hT']
                        y_, yk = yb.next()
                        for h2 in range(2):
                            pi = 4 + (b % 2) * 2 + h2
                            for c in range(2):
                                P.op('pe', lambda e, pi=pi, c=c, h2=h2: e.matmul(
                                    ps[pi][:, :], lhsT=hT_[:, c, :], rhs=d_[:, c, h2 * 512:(h2 + 1) * 512],
                                    start=(c == 0), stop=(c == 1)), r=[hTk, dk_], w=[PSK[pi]])
                            if h2 == 0:
                                P.op('act', lambda e, pi=pi: e.copy(out=y_[:, 0:512], in_=ps[pi][:, :]), r=[PSK[pi]], w=[yk])
                            else:
                                P.op('dve', lambda e, pi=pi: e.tensor_copy(out=y_[:, 512:1024], in_=ps[pi][:, :]),
                                     r=[PSK[pi]], w=[yk])
                        P.dma('sp', lambda e: e.dma_start(out=ysv[b], in_=y_[:]), 'S' + yk, r=[yk], w=[f"ys{b}"])
                        del st8[b]
                    nbl = (NB - 4) if last else NB
                    for b in range(min(3, nbl)):
                        st8[b] = dict(w=eload(b))
                    f1(0)
                    f3(0)
                    for i in range(nbl):
                        if i + 1 < nbl:
                            f1(i + 1)
                        f2(i)
                        if i + 1 < nbl:
                            f3(i + 1)
                        f4(i)
                        if i + 3 < nbl:
                            st8[i + 3] = dict(w=eload(i + 3))
                    P.flush()
                with ExitStack() as sc:
                    gb = gate_bcast(sc, l, 5, "g2b")
                    hin = Rot("fh", [SB(sc, f"fh{i}", [128, D]) for i in range(3)])
                    g1r = Rot("fg1", [SB(sc, f"fg1{i}", [128, D]) for i in range(3)])
                    g2r = Rot("fg2", [SB(sc, f"fg2{i}", [128, D]) for i in range(3)])
                    fuse = (l + 1 < L)
                    if fuse:
                        ld_A(l + 1)
                        njunk = SB(sc, "cnjunk", [128, D], BF16)
                        nxn = Rot("cnxn", [SB(sc, f"cnxn{i}", [128, D]) for i in range(2)])
                        nst = SB(sc, "cnst", [128, NT, 2])
                    if last:
                        fgb = SB(sc, "fgb", [128, D])
                        fload(fgb[:], final_gain.partition_broadcast(128), "fgb")
                        fjunk = SB(sc, "fjunk", [128, D], BF16)
                        fst = SB(sc, "fst", [128, NT, 2])
                        outv = out_d.rearrange("(n p) d -> n p d", p=128)

                    def ftile(tt):
                        s = 1 if tt < 2 else 0
                        h_, hk_ = hin.next()
                        a_, ak = g1r.next()
                        b_, bk = g2r.next()
                        P.dma('act', lambda e: e.dma_start(out=h_[:], in_=hsv[tt]), hk_, r=[f"hs{tt}"], w=[hk_])
                        P.dma('pool', lambda e: e.indirect_dma_start(
                            out=a_[:], out_offset=None, in_=ys_d,
                            in_offset=bass.IndirectOffsetOnAxis(ap=desti[:, tt, 0:1], axis=0)), ak, r=['desti'], w=[ak])
                        P.dma('pool', lambda e: e.indirect_dma_start(
                            out=b_[:], out_offset=None, in_=ys_d,
                            in_offset=bass.IndirectOffsetOnAxis(ap=desti[:, tt, 1:2], axis=0)), bk, r=['desti'], w=[bk])
                        P.op('act', lambda e: e.activation(out=a_[:], in_=a_[:], func=AF.Identity,
                                                           scale=w12[:, 0, tt:tt + 1]), r=[ak, 'w12'], w=[ak])
                        P.op('dve', lambda e: e.scalar_tensor_tensor(out=a_[:], in0=b_[:], scalar=w12[:, 1, tt:tt + 1],
                                                                     in1=a_[:], op0=ALU.mult, op1=ALU.add),
                             r=[ak, bk, 'w12'], w=[ak])
                        P.op('dve', lambda e: e.tensor_tensor(out=a_[:], in0=a_[:], in1=gb[:, s, :], op=ALU.mult),
                             r=[ak, 'g2b'], w=[ak])
                        P.op('dve', lambda e: e.tensor_tensor(out=h_[:], in0=h_[:], in1=a_[:], op=ALU.add),
                             r=[hk_, ak], w=[hk_])
                        if not last:
                            P.dma('sp', lambda e: e.dma_start(out=hsv[tt], in_=h_[:]), 'S' + hk_, r=[hk_], w=[f"hs{tt}"])
                            if fuse:
                                P.op('act', lambda e: e.activation(out=njunk[:], in_=h_[:], func=AF.Square,
                                                                   accum_out=nst[:, tt, 0:1]),
                                     r=[hk_], w=['cnjunk', f"cnst{tt}"])
                                rstd_from_ssq(nst[:, tt, 0:1], nst[:, tt, 1:2], D, [f"cnst{tt}"], f"cnrs{tt}")
                                xo, ko = nxn.next()
                                P.op('dve', lambda e: e.tensor_scalar(
                                    out=xo[:], in0=h_[:], scalar1=nst[:, tt, 1:2], scalar2=None, op0=ALU.mult),
                                    r=[hk_, f"cnrs{tt}"], w=[ko])
                                return xo, ko
                        else:
                            P.op('act', lambda e: e.activation(out=fjunk[:], in_=h_[:], func=AF.Square,
                                                               accum_out=fst[:, tt, 0:1]),
                                 r=[hk_], w=['fjunk', f"fst{tt}"])
                            rstd_from_ssq(fst[:, tt, 0:1], fst[:, tt, 1:2], D, [f"fst{tt}"], f"frs{tt}")
                            P.op('dve', lambda e: e.scalar_tensor_tensor(
                                out=h_[:], in0=h_[:], scalar=fst[:, tt, 1:2], in1=fgb[:], op0=ALU.mult, op1=ALU.mult),
                                r=[hk_, f"frs{tt}", 'fgb'], w=[hk_])
                            P.dma('sp', lambda e: e.dma_start(out=outv[tt - 2], in_=h_[:]), 'S' + hk_, r=[hk_],
                                  w=[f"out{tt}"])
                    def ftile2(tt, ctx):
                        if ctx is None:
                            return
                        xo, ko = ctx
                        s = 1 if tt < 2 else 0
                        ln = l + 1
                        for half in range(2):
                            pt = ps[(tt % 2) * 2 + half]
                            pk = PSK[(tt % 2) * 2 + half]
                            for q in range(4):
                                kc = half * 4 + q
                                P.op('pe', lambda e, pt=pt, kc=kc, q=q: e.transpose(
                                    out=pt[:, q * 128:(q + 1) * 128], in_=xo[:, kc * 128:(kc + 1) * 128],
                                    identity=identf[:]), r=[ko, 'identf'], w=[pk])
                            for q in range(4):
                                kc = half * 4 + q
                                if q % 2 == 0:
                                    P.op('act', lambda e, pt=pt, kc=kc, q=q: e.activation(
                                        out=hnT[:, kc, tt * 128:(tt + 1) * 128], in_=pt[:, q * 128:(q + 1) * 128],
                                        func=AF.Identity, scale=modc[:, ln, 1, kc, s:s + 1],
                                        bias=modc[:, ln, 0, kc, s:s + 1]), r=[pk, 'modc'], w=[f"hnT{tt}"])
                                else:
                                    P.op('dve', lambda e, pt=pt, kc=kc, q=q: e.tensor_scalar(
                                        out=hnT[:, kc, tt * 128:(tt + 1) * 128], in0=pt[:, q * 128:(q + 1) * 128],
                                        scalar1=modc[:, ln, 1, kc, s:s + 1], scalar2=modc[:, ln, 0, kc, s:s + 1],
                                        op0=ALU.mult, op1=ALU.add), r=[pk, 'modc'], w=[f"hnT{tt}"])
                    swpipe(range(tt0, NT), ftile, ftile2)
                    P.flush()
        print("ops", len(P.ops), "waits", P.nwaits, "sems", len(P.sems), flush=True)
    return nc


def _rope_tables():
    theta = 10000.0
    rows = 32
    GW = 64

    def ang(rot_dim):
        nf = rot_dim // 4
        freqs = (theta ** (-np.arange(nf, dtype=np.float32) / nf)).astype(np.float32)
        row = np.repeat(np.arange(rows, dtype=np.float32), GW)
        col = (np.arange(rows * GW) % GW).astype(np.float32)
        a = np.concatenate([row[:, None] * freqs, col[:, None] * freqs], axis=-1)
        a = np.concatenate([np.zeros((NCTX, rot_dim // 2), np.float32), a], axis=0)
        return a.astype(np.float32)
    ag = ang(64)
    cg, sg = np.cos(ag).T.astype(np.float32), np.sin(ag).T.astype(np.float32)
    G = np.zeros((2, 128, T), np.float32)
    for h in range(2):
        G[0, h * 64:h * 64 + 32] = cg
        G[0, h * 64 + 32:h * 64 + 64] = cg
        G[1, h * 64:h * 64 + 32] = -sg
        G[1, h * 64 + 32:h * 64 + 64] = sg
    am = ang(32)
    cm, sm = np.cos(am).T.astype(np.float32), np.sin(am).T.astype(np.float32)
    M = np.zeros((2, 128, T), np.float32)
    M[0, 64:80] = cm
    M[0, 80:96] = cm
    M[1, 64:80] = -sm
    M[1, 80:96] = sm
    return G, M


def _prep_weights(inp):
    f = lambda a: np.ascontiguousarray(np.asarray(a, dtype=np.float32))
    Lr = DEPTH
    w_in = f(inp['w_in'])
    sw64 = np.concatenate([np.arange(32, 64), np.arange(0, 32)])
    sw32 = np.concatenate([np.arange(16, 32), np.arange(0, 16)])
    cols = []
    cols += list(range(0, 512))
    cols += list(range(2080, 2208))
    cols += list(range(512, 768))
    cols += list(range(768, 896))
    cols += list(np.tile(np.arange(896, 928), 4))
    cols += list(np.tile(896 + sw32, 4))
    cols += list(range(928, 1696))
    cols += list(range(1696, 1952))
    for h in range(4):
        cols += list(1696 + h * 64 + sw64)
    for g in range(2):
        cols += list(range(1952 + g * 64, 1952 + (g + 1) * 64)) * 2
    for g in range(2):
        cols += list(1952 + g * 64 + sw64) * 2
    cols = np.asarray(cols)
    assert cols.shape[0] == 3072
    w_inx = f(w_in[:, :, cols])
    w_uq = f(inp['w_uq'])
    uq_cols = []
    for h in range(4):
        base = h * 96
        uq_cols += list(range(base, base + 96))
        uq_cols += list(range(base, base + 64)) + list(base + 64 + sw32)
    w_uqx = f(w_uq[:, :, np.asarray(uq_cols)])
    w_ukv = f(inp['w_ukv']).reshape(Lr, 128, 4, 128)
    w_ukvk = f(w_ukv[:, :, :, :64].reshape(Lr, 128, 256))
    w_ukvv = f(w_ukv[:, :, :, 64:].reshape(Lr, 128, 256))
    gq = f(inp['gqa_q_gain'])
    gk = f(inp['gqa_k_gain'])
    gqag = np.stack([np.tile(gq, (1, 2)), np.tile(gq[:, sw64], (1, 2)), np.tile(gk, (1, 2)),
                     np.tile(gk[:, sw64], (1, 2))], axis=-1)
    d = dict(
        w_ada=f(inp['w_ada']),
        b_adac=f(f(inp['b_ada']).reshape(Lr, 48, 128).transpose(0, 2, 1)),
        w_inx=w_inx,
        sgu_wT=f(f(inp['w_sgu']).transpose(0, 3, 1, 2)),
        sgu_b=f(f(inp['b_sgu']).transpose(0, 2, 1)),
        sgu_gain=f(inp['sgu_gain']),
        mla_qg=f(f(inp['mla_q_gain']).reshape(Lr, 2, 128).transpose(0, 2, 1)),
        mla_kvg=f(f(inp['mla_kv_gain']).reshape(Lr, 128, 1)),
        w_uqx=w_uqx, w_ukvk=w_ukvk, w_ukvv=w_ukvv,
        convw=f(f(inp['w_conv']).reshape(Lr, 3, 2, 128).transpose(0, 3, 2, 1)),
        gqag=f(gqag),
        w_gate=f(inp['w_gate']),
        b_gatec=f(f(inp['b_gate']).reshape(Lr, 4, 8, 128).transpose(0, 3, 1, 2)),
        w_branch=f(inp['w_branch']),
        w_out=f(inp['w_out']),
        w_r=f(np.concatenate([f(inp['w_group_router']), f(inp['w_expert_router'])], axis=-1)),
        b_r=f(np.concatenate([f(inp['b_group_router']), f(inp['b_expert_router'])], axis=-1)),
        w_gu_r=f(f(inp['w_expert_gate_up']).reshape(Lr, 32, 8, 128, 512).transpose(0, 1, 3, 2, 4)
                 .reshape(Lr, 32 * 128, 4096)),
        w_dn_r=f(f(inp['w_expert_down']).reshape(Lr, 32, 2, 128, D).transpose(0, 1, 3, 2, 4).reshape(Lr, 32 * 128, 2048)),
    )
    return d


PER_LAYER = ['w_ada', 'b_adac', 'w_inx', 'sgu_wT', 'sgu_b', 'sgu_gain', 'mla_qg', 'mla_kvg', 'w_uqx', 'w_ukvk',
             'w_ukvv', 'convw', 'gqag', 'w_gate', 'b_gatec', 'w_branch', 'w_out', 'w_r', 'b_r', 'w_gu_r', 'w_dn_r']

_CACHE = {}


def _get_prog(n_layers, final):
    key = (n_layers, final)
    if key not in _CACHE:
        _CACHE[key] = build(n_layers, final)
    return _CACHE[key]


def run_layers(hs_list, cvecs, wd, G, M, fg, l0, n_layers, final, cores):
    nc = _get_prog(n_layers, final)
    in_maps = []
    for i in range(len(cores)):
        m = {k: np.ascontiguousarray(wd[k][l0:l0 + n_layers]) for k in PER_LAYER}
        m.update(hs0=hs_list[i], cvec=cvecs[i], final_gain=fg, ropeG=G, ropeM=M)
        in_maps.append(m)
    res = run_bass_kernel_spmd(nc, in_maps, core_ids=list(cores))
    return [r["out" if final else "hs_out"] for r in res.results]


FUSED = True


def kernel(**inp):
    x = np.asarray(inp['x'], np.float32)
    c = np.asarray(inp['c'], np.float32)
    ctx = np.asarray(inp['ctx'], np.float32)
    c_ctx = np.asarray(inp['c_ctx'], np.float32)
    B = x.shape[0]
    wd = _prep_weights(inp)
    G, M = _rope_tables()
    fg = np.ascontiguousarray(np.asarray(inp['final_gain'], np.float32))
    hs = [np.ascontiguousarray(np.concatenate([ctx[b], x[b]], axis=0)) for b in range(B)]
    cvecs = [np.ascontiguousarray(np.stack([c[b].reshape(8, 128).T, c_ctx.reshape(8, 128).T], axis=-1)) for b in range(B)]
    cores = list(range(B))
    if FUSED:
        outs = run_layers(hs, cvecs, wd, G, M, fg, 0, DEPTH, True, cores)
    else:
        for l in range(DEPTH - 1):
            hs = run_layers(hs, cvecs, wd, G, M, fg, l, 1, False, cores)
        outs = run_layers(hs, cvecs, wd, G, M, fg, DEPTH - 1, 1, True, cores)
    return np.stack(outs, axis=0).astype(np.float32)
```
